# Optimizing a Trainium2 kernel written in Bass

```python
import math, functools
import jax, jax.numpy as jnp
from jax import lax
import numpy as np

D_MODEL = 1024
BATCH = 1
SEQ = 16384
DEPTH = 2

GRID_W = 64
CTX_LEN = 256
EPS = 1e-6

HY_WIDTH = 256
HY_SHORT = 3
HY_ORDER = 2
HY_BANDS = 16
HY_EMB = 1 + 2 * HY_BANDS
HY_HIDDEN = 64
HY_TARGET = 1e-2
HY_FAST_DECAY = 0.3
HY_SLOW_DECAY = 1.5
HY_MIN_DECAY = math.log(HY_TARGET) / HY_SLOW_DECAY
HY_MAX_DECAY = math.log(HY_TARGET) / HY_FAST_DECAY
HY_FILTER_STD = 0.005
HY_COLS = 3 * HY_WIDTH

SSD_HEADS = 8
SSD_HEAD_DIM = 64
SSD_WIDTH = SSD_HEADS * SSD_HEAD_DIM
SSD_GROUPS = 2
SSD_HPG = SSD_HEADS // SSD_GROUPS
SSD_STATE = 128
SSD_CONV = 3
SSD_CHUNK = 128
SSD_XBC = SSD_WIDTH + 2 * SSD_GROUPS * SSD_STATE
SSD_COLS = SSD_WIDTH + SSD_XBC + 2 * SSD_HEADS

ATTN_HEADS = 4
ATTN_KV_HEADS = 2
ATTN_REP = ATTN_HEADS // ATTN_KV_HEADS
ATTN_HEAD_DIM = 64
ATTN_WIDTH = ATTN_HEADS * ATTN_HEAD_DIM
ATTN_KV_WIDTH = ATTN_KV_HEADS * ATTN_HEAD_DIM
ATTN_BLOCK = 128
ROPE_BASE = 10000.0
ATTN_COLS = ATTN_WIDTH + 2 * ATTN_KV_WIDTH

MIX_WIDTH = HY_WIDTH + SSD_WIDTH + ATTN_WIDTH
D_IN = HY_COLS + SSD_COLS + ATTN_COLS

D_FF = 2816
FFN_CONV = 3

kernel_name = 'hybrid_hyena_ssd_gqa_dit_trunk'


def _layernorm(x, g, b):
    xf = x.astype(jnp.float32)
    mu = jnp.mean(xf, axis=-1, keepdims=True)
    var = jnp.mean(jnp.square(xf - mu), axis=-1, keepdims=True)
    return ((xf - mu) * lax.rsqrt(var + EPS) * g + b).astype(x.dtype)


def _rmsnorm(x, g):
    xf = x.astype(jnp.float32)
    return (xf * lax.rsqrt(jnp.mean(xf * xf, axis=-1, keepdims=True) + EPS) * g).astype(x.dtype)


def _dwconv(x, w, b):
    k = w.shape[0]
    y = lax.conv_general_dilated(x, w[:, None, :].astype(x.dtype), window_strides=(1,),
                                 padding=[(k // 2, k // 2)],
                                 dimension_numbers=('NWC', 'WIO', 'NWC'),
                                 feature_group_count=x.shape[-1])
    return y + b


def _flip(t):
    return jnp.flip(t, axis=1)


def _grid_rope(n_tokens):
    rows = n_tokens // GRID_W
    row = jnp.repeat(jnp.arange(rows, dtype=jnp.float32), GRID_W)
    col = jnp.tile(jnp.arange(GRID_W, dtype=jnp.float32), rows)
    n_freq = ATTN_HEAD_DIM // 4
    inv = ROPE_BASE ** (-jnp.arange(n_freq, dtype=jnp.float32) / n_freq)
    ang = jnp.concatenate([row[:, None] * inv, col[:, None] * inv], axis=-1)
    return jnp.cos(ang), jnp.sin(ang)


def _apply_rope(x, cos, sin):
    x1, x2 = jnp.split(x.astype(jnp.float32), 2, axis=-1)
    c = cos[None, :, None, :]
    s = sin[None, :, None, :]
    return jnp.concatenate([x1 * c - x2 * s, x1 * s + x2 * c], axis=-1).astype(x.dtype)


def _split_cols(p):
    return (p[..., :HY_COLS], p[..., HY_COLS:HY_COLS + SSD_COLS], p[..., HY_COLS + SSD_COLS:])


def _hyena_filters(n, w1, b1, w2, b2, w3, freq):
    t = jnp.linspace(0.0, 1.0, n, dtype=jnp.float32)[:, None]
    w = (2.0 * math.pi / n) * jnp.arange(n, dtype=jnp.float32)[:, None]
    f = jnp.linspace(1e-4, HY_BANDS - 1, HY_BANDS, dtype=jnp.float32)[None, :]
    feats = jnp.concatenate([t, jnp.cos(f * w), -jnp.sin(f * w)], axis=-1)
    h = jnp.sin(freq * (feats @ w1 + b1))
    h = jnp.sin(freq * (h @ w2 + b2))
    h = (h @ w3).astype(jnp.float32)
    deltas = jnp.abs(jnp.linspace(HY_MIN_DECAY, HY_MAX_DECAY, HY_WIDTH, dtype=jnp.float32))
    window = jnp.exp(-t * deltas)
    return h.reshape(n, HY_ORDER, 2, HY_WIDTH) * window[:, None, None, :]


def _bidir_longconv(u, h_f, h_b, skip):
    n = u.shape[1]
    k = jnp.concatenate([h_f, jnp.zeros_like(h_f[:1]), h_b[:0:-1]], axis=0)
    kf = jnp.fft.rfft(k, axis=0)
    uf = jnp.fft.rfft(u.astype(jnp.float32), n=2 * n, axis=1)
    y = jnp.fft.irfft(uf * kf[None], n=2 * n, axis=1)[:, :n]
    return (y + u.astype(jnp.float32) * skip.astype(jnp.float32)).astype(u.dtype)


def _hyena(u, filt, conv_w, conv_b, skip):
    u = _dwconv(u, conv_w, conv_b)
    v, x1, x2 = jnp.split(u, 3, axis=-1)
    z = x1 * _bidir_longconv(v, filt[:, 0, 0], filt[:, 0, 1], skip[0])
    return x2 * _bidir_longconv(z, filt[:, 1, 0], filt[:, 1, 1], skip[1])


def _ssd_prep(u, conv_w, conv_b, dt_bias):
    b, n = u.shape[:2]
    z = u[..., :SSD_WIDTH]
    xbc = jax.nn.silu(_dwconv(u[..., SSD_WIDTH:SSD_WIDTH + SSD_XBC], conv_w, conv_b))
    dt_raw = u[..., SSD_WIDTH + SSD_XBC:].astype(jnp.float32)
    xs = xbc[..., :SSD_WIDTH].reshape(b, n, SSD_GROUPS, SSD_HPG, SSD_HEAD_DIM)
    bm = xbc[..., SSD_WIDTH:SSD_WIDTH + SSD_GROUPS * SSD_STATE].reshape(b, n, SSD_GROUPS, SSD_STATE)
    cm = xbc[..., SSD_WIDTH + SSD_GROUPS * SSD_STATE:].reshape(b, n, SSD_GROUPS, SSD_STATE)
    dt = jax.nn.softplus(dt_raw.reshape(b, n, 2, SSD_GROUPS, SSD_HPG)
                         + dt_bias.astype(jnp.float32).reshape(2, SSD_GROUPS, SSD_HPG))
    return z, xs, bm, cm, dt


def _chunk(t):
    return t.reshape(t.shape[0], t.shape[1] // SSD_CHUNK, SSD_CHUNK, *t.shape[2:])


def _ssd_states(xc, dtc, a_cum, bc):
    decay = jnp.exp(a_cum[:, :, -1:] - a_cum)
    return jnp.einsum('bclgn,bclgr,bclgrp->bcgrpn', bc, decay * dtc, xc)


def _ssd_final_state(xs, dt, a, bm):
    a_cum = jnp.cumsum(dt * a, axis=1)[:, None]
    return _ssd_states(xs[:, None], dt[:, None], a_cum, bm[:, None])[:, 0]


def _ssd_scan(xs, dt, a, bm, cm, h0):
    xc, dtc, bc, cc = _chunk(xs), _chunk(dt), _chunk(bm), _chunk(cm)
    a_cum = jnp.cumsum(dtc * a, axis=2)
    states = _ssd_states(xc, dtc, a_cum, bc)
    seg = a_cum[:, :, :, None] - a_cum[:, :, None, :]
    lower = jnp.tril(jnp.ones((SSD_CHUNK, SSD_CHUNK), dtype=bool))[:, :, None, None]
    decay = jnp.exp(jnp.where(lower, seg, -jnp.inf))
    scores = jnp.einsum('bclgn,bcsgn->bclsg', cc, bc)
    y = jnp.einsum('bclsgr,bcsgrp->bclgrp', scores[..., None] * decay, xc * dtc[..., None])

    def step(h, inp):
        st, dec = inp
        return h * dec[..., None, None] + st, h

    h_last, h_prev = lax.scan(step, h0, (jnp.moveaxis(states, 1, 0),
                                         jnp.moveaxis(jnp.exp(a_cum[:, :, -1]), 1, 0)))
    h_prev = jnp.moveaxis(h_prev, 0, 1)
    y = y + jnp.einsum('bclgn,bcgrpn->bclgrp', cc, h_prev) * jnp.exp(a_cum)[..., None]
    return y.reshape(xs.shape), h_last


def _ssd_output(y_f, y_b, xs, z, d, norm_w):
    b, n = xs.shape[:2]
    y = y_f + y_b + d.astype(jnp.float32).reshape(SSD_GROUPS, SSD_HPG, 1) * xs
    gated = y.reshape(b, n, SSD_WIDTH) * jax.nn.silu(z.astype(jnp.float32))
    out = _rmsnorm(gated.reshape(b, n, SSD_GROUPS, SSD_WIDTH // SSD_GROUPS),
                   norm_w.reshape(SSD_GROUPS, SSD_WIDTH // SSD_GROUPS))
    return out.reshape(b, n, SSD_WIDTH).astype(z.dtype)


def _heads(t, n_heads, g):
    return _rmsnorm(t.reshape(t.shape[0], t.shape[1], n_heads, ATTN_HEAD_DIM), g)


def _block_attention(q, k, v):
    b, n = q.shape[:2]
    nb = n // ATTN_BLOCK
    qb = jnp.moveaxis(q.reshape(b, nb, ATTN_BLOCK, ATTN_KV_HEADS, ATTN_REP, ATTN_HEAD_DIM), 1, 0)
    scale = ATTN_HEAD_DIM ** -0.5

    def one(qblk):
        s = jnp.einsum('bqgrd,bkgd->bgrqk', qblk, k).astype(jnp.float32) * scale
        p = jax.nn.softmax(s, axis=-1).astype(v.dtype)
        return jnp.einsum('bgrqk,bkgd->bqgrd', p, v)

    o = lax.map(one, qb)
    return jnp.moveaxis(o, 0, 1).reshape(b, n, ATTN_WIDTH)


def _mixer(h_lat, h_ctx, ctx_out, rope, w_in, hy_conv_w, hy_conv_b, hy_ffn_w1, hy_ffn_b1,
           hy_ffn_w2, hy_ffn_b2, hy_ffn_w3, hy_freq, hy_bias, ssd_conv_w, ssd_conv_b,
           ssd_dt_bias, ssd_a_log, ssd_d, ssd_norm_w, attn_q_norm, attn_k_norm, w_out):
    filt = functools.partial(_hyena_filters, w1=hy_ffn_w1, b1=hy_ffn_b1, w2=hy_ffn_w2,
                             b2=hy_ffn_b2, w3=hy_ffn_w3, freq=hy_freq)
    hy_l, ssd_l, at_l = _split_cols(h_lat @ w_in)
    hy_c, ssd_c, at_c = _split_cols(h_ctx @ w_in)

    y_hy_l = _hyena(hy_l, filt(h_lat.shape[1]), hy_conv_w, hy_conv_b, hy_bias)

    a = -jnp.exp(ssd_a_log.astype(jnp.float32)).reshape(2, SSD_GROUPS, SSD_HPG)
    z_l, xs_l, b_l, c_l, dt_l = _ssd_prep(ssd_l, ssd_conv_w, ssd_conv_b, ssd_dt_bias)
    z_c, xs_c, b_c, c_c, dt_c = _ssd_prep(ssd_c, ssd_conv_w, ssd_conv_b, ssd_dt_bias)
    if ctx_out:
        h0 = jnp.zeros((h_ctx.shape[0], SSD_GROUPS, SSD_HPG, SSD_HEAD_DIM, SSD_STATE), jnp.float32)
        yc_f, hc_f = _ssd_scan(xs_c, dt_c[:, :, 0], a[0], b_c, c_c, h0)
        yc_b, hc_b = _ssd_scan(_flip(xs_c), _flip(dt_c[:, :, 1]), a[1], _flip(b_c), _flip(c_c), h0)
        y_ssd_c = _ssd_output(yc_f, _flip(yc_b), xs_c, z_c, ssd_d, ssd_norm_w)
    else:
        hc_f = _ssd_final_state(xs_c, dt_c[:, :, 0], a[0], b_c)
        hc_b = _ssd_final_state(_flip(xs_c), _flip(dt_c[:, :, 1]), a[1], _flip(b_c))
    yl_f, _ = _ssd_scan(xs_l, dt_l[:, :, 0], a[0], b_l, c_l, hc_f)
    yl_b, _ = _ssd_scan(_flip(xs_l), _flip(dt_l[:, :, 1]), a[1], _flip(b_l), _flip(c_l), hc_b)
    y_ssd_l = _ssd_output(yl_f, _flip(yl_b), xs_l, z_l, ssd_d, ssd_norm_w)

    q_l = _apply_rope(_heads(at_l[..., :ATTN_WIDTH], ATTN_HEADS, attn_q_norm), *rope)
    k_l = _apply_rope(_heads(at_l[..., ATTN_WIDTH:ATTN_WIDTH + ATTN_KV_WIDTH], ATTN_KV_HEADS, attn_k_norm), *rope)
    v_l = at_l[..., ATTN_WIDTH + ATTN_KV_WIDTH:].reshape(*at_l.shape[:2], ATTN_KV_HEADS, ATTN_HEAD_DIM)
    k_c = _heads(at_c[..., ATTN_WIDTH:ATTN_WIDTH + ATTN_KV_WIDTH], ATTN_KV_HEADS, attn_k_norm)
    v_c = at_c[..., ATTN_WIDTH + ATTN_KV_WIDTH:].reshape(*at_c.shape[:2], ATTN_KV_HEADS, ATTN_HEAD_DIM)
    y_at_l = _block_attention(q_l, jnp.concatenate([k_c, k_l], axis=1), jnp.concatenate([v_c, v_l], axis=1))

    m_lat = jnp.concatenate([y_hy_l, y_ssd_l, y_at_l], axis=-1) @ w_out
    if not ctx_out:
        return m_lat, None
    y_hy_c = _hyena(hy_c, filt(h_ctx.shape[1]), hy_conv_w, hy_conv_b, hy_bias)
    q_c = _heads(at_c[..., :ATTN_WIDTH], ATTN_HEADS, attn_q_norm)
    y_at_c = _block_attention(q_c, k_c, v_c)
    m_ctx = jnp.concatenate([y_hy_c, y_ssd_c, y_at_c], axis=-1) @ w_out
    return m_lat, m_ctx


def _conv_ffn(h, w_up, conv_w, conv_b, w_down):
    u = _dwconv(h @ w_up, conv_w, conv_b)
    g, val = jnp.split(u, 2, axis=-1)
    return (jax.nn.silu(g) * val) @ w_down


def setup_inputs(seed: int = 0) -> dict:
    key = jax.random.key(seed)
    ks = iter(jax.random.split(key, 40))

    def nrm(shape, std):
        return std * jax.random.normal(next(ks), shape, jnp.float32)

    beta = (8.0 * DEPTH) ** -0.25
    L = DEPTH
    dt0 = jnp.exp(jax.random.uniform(next(ks), (L, 2, SSD_HEADS), jnp.float32,
                                     math.log(1e-3), math.log(1e-1)))
    return {
        'x': nrm((BATCH, SEQ, D_MODEL), 1.0),
        'c': nrm((BATCH, D_MODEL), 1.0),
        'ctx': nrm((BATCH, CTX_LEN, D_MODEL), 1.0),
        'c_ctx': nrm((D_MODEL,), 1.0),
        'w_mod': nrm((L, D_MODEL, 6 * D_MODEL), D_MODEL ** -0.5),
        'b_mod': nrm((L, 6 * D_MODEL), 0.02),
        'w_in': nrm((L, D_MODEL, D_IN), D_MODEL ** -0.5),
        'hy_conv_w': nrm((L, HY_SHORT, HY_COLS), HY_SHORT ** -0.5),
        'hy_conv_b': nrm((L, HY_COLS), 0.02),
        'hy_ffn_w1': nrm((L, HY_EMB, HY_HIDDEN), HY_EMB ** -0.5),
        'hy_ffn_b1': nrm((L, HY_HIDDEN), 0.02),
        'hy_ffn_w2': nrm((L, HY_HIDDEN, HY_HIDDEN), HY_HIDDEN ** -0.5),
        'hy_ffn_b2': nrm((L, HY_HIDDEN), 0.02),
        'hy_ffn_w3': nrm((L, HY_HIDDEN, HY_ORDER * 2 * HY_WIDTH), HY_FILTER_STD),
        'hy_freq': 1.0 + nrm((L, HY_HIDDEN), 0.1),
        'hy_bias': nrm((L, HY_ORDER, HY_WIDTH), 0.5),
        'ssd_conv_w': nrm((L, SSD_CONV, SSD_XBC), SSD_CONV ** -0.5),
        'ssd_conv_b': nrm((L, SSD_XBC), 0.02),
        'ssd_dt_bias': dt0 + jnp.log(-jnp.expm1(-dt0)),
        'ssd_a_log': jnp.log(jax.random.uniform(next(ks), (L, 2, SSD_HEADS), jnp.float32, 1.0, 16.0)),
        'ssd_d': 1.0 + nrm((L, SSD_HEADS), 0.1),
        'ssd_norm_w': 1.0 + nrm((L, SSD_WIDTH), 0.02),
        'attn_q_norm': 1.0 + nrm((L, ATTN_HEAD_DIM), 0.02),
        'attn_k_norm': 1.0 + nrm((L, ATTN_HEAD_DIM), 0.02),
        'w_out': nrm((L, MIX_WIDTH, D_MODEL), beta * MIX_WIDTH ** -0.5),
        'ln1_g': 1.0 + nrm((L, D_MODEL), 0.02),
        'ln1_b': nrm((L, D_MODEL), 0.02),
        'ffn_w_up': nrm((L, D_MODEL, 2 * D_FF), D_MODEL ** -0.5),
        'ffn_conv_w': nrm((L, FFN_CONV, 2 * D_FF), FFN_CONV ** -0.5),
        'ffn_conv_b': nrm((L, 2 * D_FF), 0.02),
        'ffn_w_down': nrm((L, D_FF, D_MODEL), beta * D_FF ** -0.5),
        'ln2_g': 1.0 + nrm((L, D_MODEL), 0.02),
        'ln2_b': nrm((L, D_MODEL), 0.02),
    }


def reference(x, c, ctx, c_ctx, w_mod, b_mod, w_in, hy_conv_w, hy_conv_b, hy_ffn_w1, hy_ffn_b1,
              hy_ffn_w2, hy_ffn_b2, hy_ffn_w3, hy_freq, hy_bias, ssd_conv_w, ssd_conv_b,
              ssd_dt_bias, ssd_a_log, ssd_d, ssd_norm_w, attn_q_norm, attn_k_norm, w_out,
              ln1_g, ln1_b, ffn_w_up, ffn_conv_w, ffn_conv_b, ffn_w_down, ln2_g, ln2_b):
    alpha = (2.0 * DEPTH) ** 0.25
    rope = _grid_rope(x.shape[1])
    s_lat = jax.nn.silu(c)
    s_ctx = jax.nn.silu(c_ctx)
    x_lat, x_ctx = x, ctx
    for i in range(DEPTH):
        ctx_out = i < DEPTH - 1
        sh1, sc1, g1, sh2, sc2, g2 = jnp.split((s_lat @ w_mod[i] + b_mod[i])[:, None, :], 6, axis=-1)
        ch1, cs1, cg1, ch2, cs2, cg2 = jnp.split(s_ctx @ w_mod[i] + b_mod[i], 6, axis=-1)
        m_lat, m_ctx = _mixer(x_lat * (1.0 + sc1) + sh1, x_ctx * (1.0 + cs1) + ch1, ctx_out, rope,
                              w_in[i], hy_conv_w[i], hy_conv_b[i], hy_ffn_w1[i], hy_ffn_b1[i],
                              hy_ffn_w2[i], hy_ffn_b2[i], hy_ffn_w3[i], hy_freq[i], hy_bias[i],
                              ssd_conv_w[i], ssd_conv_b[i], ssd_dt_bias[i], ssd_a_log[i], ssd_d[i],
                              ssd_norm_w[i], attn_q_norm[i], attn_k_norm[i], w_out[i])
        x_lat = _layernorm(alpha * x_lat + g1 * m_lat, ln1_g[i], ln1_b[i])
        f_lat = _conv_ffn(x_lat * (1.0 + sc2) + sh2, ffn_w_up[i], ffn_conv_w[i], ffn_conv_b[i], ffn_w_down[i])
        x_lat = _layernorm(alpha * x_lat + g2 * f_lat, ln2_g[i], ln2_b[i])
        if ctx_out:
            x_ctx = _layernorm(alpha * x_ctx + cg1 * m_ctx, ln1_g[i], ln1_b[i])
            f_ctx = _conv_ffn(x_ctx * (1.0 + cs2) + ch2, ffn_w_up[i], ffn_conv_w[i], ffn_conv_b[i], ffn_w_down[i])
            x_ctx = _layernorm(alpha * x_ctx + cg2 * f_ctx, ln2_g[i], ln2_b[i])
    return x_lat
```

```python
import numpy as np
from contextlib import ExitStack
import concourse.bass as bass
import concourse.mybir as mybir
from concourse.bass_utils import run_bass_kernel_spmd

F32 = mybir.dt.float32
BF16 = mybir.dt.bfloat16
AF = mybir.ActivationFunctionType
ALU = mybir.AluOpType
AX = mybir.AxisListType

ENGS = ("pe", "dve", "act", "pool", "sp")
NDS = 8


class Trk:
    __slots__ = ("w", "rs")

    def __init__(self):
        self.w = None
        self.rs = {}


class V:
    __slots__ = ("ap", "trks", "bank")

    def __init__(self, ap, trks, bank=None):
        self.ap = ap
        self.trks = trks
        self.bank = bank

    def __getitem__(self, idx):
        return V(self.ap[idx], self.trks, self.bank)


class Buf:
    def __init__(self, t, psum=False):
        self.t = t
        self.trk = Trk()
        self.subs = {}
        self.bank = {} if psum else None

    def __getitem__(self, idx):
        return V(self.t[idx], [self.trk], self.bank)

    def sub(self, key, idx):
        if key not in self.subs:
            self.subs[key] = Trk()
        return V(self.t[idx], [self.subs[key]], self.bank)

    def all(self, idx):
        return V(self.t[idx], [self.trk] + list(self.subs.values()), self.bank)


class Prog:
    def __init__(self, nc, self_sync=True):
        self.nc = nc
        self.es = ExitStack()
        self.ops = {e: [] for e in ENGS}
        self.cnt = {e: 0 for e in ENGS}
        self.seen = {e: {} for e in ENGS}
        self.sems = {}
        self.self_sync = self_sync
        for e in ENGS:
            self.sems[e] = self.es.enter_context(nc.semaphore("s_" + e))
        self.dcnt = {"sp": 0, "pool": 0, "act": 0}
        for q in ("sp", "pool", "act"):
            for i in range(NDS):
                self.sems[(q, i)] = self.es.enter_context(nc.semaphore("d_%s%d" % (q, i)))
        self.nbuf = 0
        self.dma_tokens = []

    def sb(self, shape, dt, name=None):
        self.nbuf += 1
        t = self.es.enter_context(self.nc.sbuf_tensor(name or "sb%d" % self.nbuf, list(shape), dt))
        return Buf(t)

    def ps(self, shape, dt, name=None):
        self.nbuf += 1
        full = [128, 2048 // mybir.dt.size(dt)]
        assert shape[0] <= 128 and int(np.prod(shape[1:])) <= full[1]
        t = self.es.enter_context(self.nc.psum_tensor(name or "ps%d" % self.nbuf, full, dt))
        return Buf(t, psum=True)

    def _emit(self, eng, fn, reads, writes, dma=False, pe_acc=False):
        deps = {}

        def add(tok):
            if tok is None:
                return
            k, v = tok
            if deps.get(k, 0) < v:
                deps[k] = v

        for vw in reads:
            for t in vw.trks:
                add(t.w)
        for vw in writes:
            for t in vw.trks:
                add(t.w)
                for k, v in t.rs.items():
                    add((k, v))
        for vw in list(reads) + list(writes):
            if vw.bank is not None:
                for f, tk in vw.bank.items():
                    if f != eng:
                        add(tk)
        waits = []
        seen = self.seen[eng]
        for k, v in deps.items():
            if k == eng:
                if eng == "pe" or not self.self_sync:
                    continue
            if seen.get(k, 0) >= v:
                continue
            seen[k] = v
            waits.append((k, v))
        if dma:
            n = self.dcnt[eng]
            self.dcnt[eng] = n + 1
            key = (eng, n % NDS)
            val = 16 * (n // NDS + 1)
            if n >= NDS and seen.get(key, 0) < val - 16:
                waits.append((key, val - 16))
                seen[key] = val - 16
            tok = (key, val)
            self.dma_tokens.append(tok)
        else:
            self.cnt[eng] += 1
            tok = (eng, self.cnt[eng])
        self.ops[eng].append((waits, fn, tok, dma))
        for vw in list(reads) + list(writes):
            if vw.bank is not None:
                vw.bank[eng] = tok
        for vw in reads:
            for t in vw.trks:
                if t.rs.get(tok[0], 0) < tok[1]:
                    t.rs[tok[0]] = tok[1]
        for vw in writes:
            for t in vw.trks:
                t.w = tok
                t.rs = {}
        return tok

    def dma(self, out, in_, q="sp", **kw):
        reads = [in_] if isinstance(in_, V) else []
        writes = [out] if isinstance(out, V) else []
        o = out.ap if isinstance(out, V) else out
        i = in_.ap if isinstance(in_, V) else in_
        return self._emit(q, lambda e: e.dma_start(out=o, in_=i, **kw), reads, writes, dma=True)

    def mm(self, out, lhsT, rhs, start=True, stop=True, **kw):
        return self._emit("pe", lambda e: e.matmul(out.ap, lhsT.ap, rhs.ap, start=start, stop=stop, **kw),
                          [lhsT, rhs], [out])

    def tr(self, out, in_, ident):
        return self._emit("pe", lambda e: e.transpose(out.ap, in_.ap, ident.ap), [in_, ident], [out])

    def act(self, out, in_, func, bias=None, scale=None, accum=None, extra_reads=()):
        kw = {}
        reads = [in_] + list(extra_reads)
        writes = [out]
        if bias is not None:
            if isinstance(bias, V):
                kw["bias"] = bias.ap
                reads.append(bias)
            else:
                kw["bias"] = bias
        if scale is not None:
            if isinstance(scale, V):
                kw["scale"] = scale.ap
                reads.append(scale)
            else:
                kw["scale"] = scale
        if accum is not None:
            kw["accum_out"] = accum.ap
            writes.append(accum)
        return self._emit("act", lambda e: e.activation(out.ap, in_.ap, func, **kw), reads, writes)

    def tt(self, out, a, b, op, eng="dve"):
        return self._emit(eng, lambda e: e.tensor_tensor(out.ap, a.ap, b.ap, op), [a, b], [out])

    def ts(self, out, a, s1, s2, op0, op1=None, eng="dve", accum=None):
        reads = [a]
        writes = [out]
        x1 = s1
        x2 = s2
        if isinstance(s1, V):
            reads.append(s1)
            x1 = s1.ap
        if isinstance(s2, V):
            reads.append(s2)
            x2 = s2.ap
        kw = {}
        if op1 is not None:
            kw["op1"] = op1
        if accum is not None:
            kw["accum_out"] = accum.ap
            writes.append(accum)
        return self._emit(eng, lambda e: e.tensor_scalar(out.ap, a.ap, x1, x2, op0, **kw), reads, writes)

    def stt(self, out, a, s, b, op0, op1, accum=None):
        reads = [a, b]
        writes = [out]
        x = s
        if isinstance(s, V):
            reads.append(s)
            x = s.ap
        kw = {}
        if accum is not None:
            kw["accum_out"] = accum.ap
            writes.append(accum)
        return self._emit("dve", lambda e: e.scalar_tensor_tensor(out.ap, a.ap, x, b.ap, op0, op1, **kw),
                          reads, writes)

    def copy(self, out, in_, eng="dve"):
        if eng == "act":
            return self._emit("act", lambda e: e.copy(out.ap, in_.ap), [in_], [out])
        return self._emit(eng, lambda e: e.tensor_copy(out.ap, in_.ap), [in_], [out])

    def memset(self, out, val, eng="dve"):
        return self._emit(eng, lambda e: e.memset(out.ap, val), [], [out])

    def gen(self, eng, fn, reads, writes):
        return self._emit(eng, fn, reads, writes)

    def finish(self):
        nc = self.nc
        last = {}
        for k, v in self.dma_tokens:
            if last.get(k, 0) < v:
                last[k] = v
        for e in ENGS:
            if self.cnt[e] > 0:
                last[e] = self.cnt[e]
        final_waits = [(k, v) for k, v in last.items()]
        engmap = {"pe": "tensor", "dve": "vector", "act": "scalar", "pool": "gpsimd", "sp": "sync"}
        with nc.Block() as block:
            for e in ENGS:
                ops = self.ops[e]
                extra = final_waits if e == "sp" else []
                if not ops and not extra:
                    continue

                def body(eng, ops=ops, e=e, extra=extra):
                    for waits, fn, tok, dma in ops:
                        for k, v in waits:
                            eng.wait_ge(self.sems[k], v)
                        ins = fn(eng)
                        if dma:
                            ins.then_inc(self.sems[tok[0]], 16)
                        else:
                            ins.then_inc(self.sems[e], 1)
                    for k, v in extra:
                        eng.wait_ge(self.sems[k], v)

                getattr(block, engmap[e])(body)
        self.es.close()


L = 16384
LC = 256
TT = L + LC
NCORE = 8
NCX = LC // NCORE
NLT = L // NCORE
NTK = NCX + NLT
DM = 1024
D_IN = 2832
HYC = 768
SSDC = 1552
EPS = 1e-6
ALPHA = 2.0 ** 0.5
_PROGS = {}


def _dram(nc, name, shape, dt, out=False):
    return nc.dram_tensor(name, list(shape), dt, kind="ExternalOutput" if out else "ExternalInput").ap()


def _run(key, builder, in_maps):
    if key not in _PROGS:
        _PROGS[key] = builder()
    nc = _PROGS[key]
    res = run_bass_kernel_spmd(nc, in_maps, core_ids=list(range(NCORE)))
    return res.results


def _fm(v):
    v = np.asarray(v, np.float32).reshape(-1, 128)
    return np.ascontiguousarray(v.T)


def build_mod():
    nc = bass.Bass("TRN2", target_bir_lowering=False)
    c8 = _dram(nc, "c8", [128, 16], F32)
    wm = _dram(nc, "wm", [2, 1024, 768], F32)
    bm = _dram(nc, "bm", [2, 1536], F32)
    out = _dram(nc, "out", [2, 1536], F32, out=True)
    P = Prog(nc)
    cs = P.sb([128, 16], F32)
    s = P.sb([128, 16], F32)
    S = P.sb([128, 8, 2], F32)
    W = P.sb([128, 2, 8, 768], F32)
    bt = P.sb([2, 1536], F32)
    ot = P.sb([2, 1536], F32)
    P.dma(cs[:], c8)
    P.dma(bt[:], bm)
    for l in range(2):
        P.dma(W.sub(l, (slice(None), l)), wm[l].rearrange("(k p) n -> p k n", p=128), q="sp" if l == 0 else "act")
    P.act(s[:], cs[:], AF.Silu)
    P.copy(S[:, :, 0], s[:, 0:8])
    P.copy(S[:, :, 1], s[:, 8:16])
    pss = [P.ps([2, 512], F32) for _ in range(2)]
    i = 0
    for l in range(2):
        for (n0, n1) in ((0, 512), (512, 768)):
            ps = pss[i % 2]
            i += 1
            for k in range(8):
                P.mm(ps[0:2, 0:n1 - n0], S[:, k, :], W.sub(l, (slice(None), l, k, slice(n0, n1))),
                     start=(k == 0), stop=(k == 7))
            P.tt(ot[:, l * 768 + n0:l * 768 + n1], ps[0:2, 0:n1 - n0], bt[:, l * 768 + n0:l * 768 + n1], ALU.add)
    P.dma(out, ot[:])
    P.finish()
    return nc


def run_mod(inp):
    c8 = np.concatenate([_fm(inp["c"][0]), _fm(inp["c_ctx"])], axis=1)
    maps = []
    for c in range(NCORE):
        wm = np.ascontiguousarray(inp["w_mod"][:, :, c * 768:(c + 1) * 768])
        b = inp["b_mod"][:, c * 768:(c + 1) * 768].reshape(1, 1536)
        maps.append({"c8": c8, "wm": wm, "bm": np.ascontiguousarray(np.repeat(b, 2, axis=0))})
    res = _run("mod", build_mod, maps)
    full = np.concatenate([r["out"].reshape(2, 2, 768) for r in res], axis=2)
    return full


def build_inproj():
    nc = bass.Bass("TRN2", target_bir_lowering=False)
    xT = _dram(nc, "xT", [1024, NTK], F32)
    md = _dram(nc, "md", [128, 32], F32)
    w = _dram(nc, "w", [1024, D_IN], F32)
    out = _dram(nc, "out", [D_IN, NTK], F32, out=True)
    P = Prog(nc)
    x = P.sb([128, 8, NTK], F32)
    h = P.sb([128, 8, NTK], BF16)
    W = P.sb([128, 8, D_IN], BF16)
    m = P.sb([128, 32], F32)
    m1 = P.sb([128, 32], F32)
    P.dma(m[:], md)
    for k in range(8):
        P.dma(x.sub(k, (slice(None), k)), xT[k * 128:(k + 1) * 128, :], q="sp" if k % 2 == 0 else "act")
    for k in range(8):
        for (c0, c1) in ((0, 1416), (1416, 2832)):
            P.dma(W.sub(k, (slice(None), k, slice(c0, c1))), w[k * 128:(k + 1) * 128, c0:c1], q="pool")
    P.ts(m1[:], m[:], 1.0, None, ALU.add)
    for k in range(8):
        P.act(h.sub(k, (slice(None), k, slice(0, NCX))), x.sub(k, (slice(None), k, slice(0, NCX))), AF.Identity,
              scale=m1[:, k:k + 1], bias=m[:, 8 + k:9 + k])
        P.act(h.sub(k, (slice(None), k, slice(NCX, NTK))), x.sub(k, (slice(None), k, slice(NCX, NTK))), AF.Identity,
              scale=m1[:, 16 + k:17 + k], bias=m[:, 24 + k:25 + k])
    nblk = [(0, 512), (512, 1024), (1024, 1536), (1536, 2048), (2048, NTK)]
    pss = [P.ps([128, 512], F32) for _ in range(4)]
    obs = [P.sb([128, NTK], F32) for _ in range(2)]
    i = 0
    for mi in range(23):
        r0 = mi * 128
        M = min(128, D_IN - r0)
        ob = obs[mi % 2]
        for (n0, n1) in nblk:
            ps = pss[i % 4]
            for k in range(8):
                P.mm(ps[0:M, 0:n1 - n0], W.sub(k, (slice(None), k, slice(r0, r0 + M))),
                     h.sub(k, (slice(None), k, slice(n0, n1))), start=(k == 0), stop=(k == 7))
            if i % 2 == 0:
                P.copy(ob[0:M, n0:n1], ps[0:M, 0:n1 - n0], eng="dve")
            else:
                P.copy(ob[0:M, n0:n1], ps[0:M, 0:n1 - n0], eng="act")
            i += 1
        P.dma(out[r0:r0 + M, :], ob[0:M, :], q="sp")
    P.finish()
    return nc


def tok_shard(a_ctx, a_lat):
    return [np.ascontiguousarray(np.concatenate([a_ctx[:, c * NCX:(c + 1) * NCX], a_lat[:, c * NLT:(c + 1) * NLT]],
                                                axis=1)) for c in range(NCORE)]


def tok_unshard(parts):
    ctx = np.concatenate([p[:, :NCX] for p in parts], axis=1)
    lat = np.concatenate([p[:, NCX:] for p in parts], axis=1)
    return np.concatenate([ctx, lat], axis=1)


def run_inproj(xT_ctx, xT_lat, mod, layer, w_in):
    ml = mod[0, layer].reshape(6, 1024)
    mc = mod[1, layer].reshape(6, 1024)
    md = np.concatenate([_fm(mc[1]), _fm(mc[0]), _fm(ml[1]), _fm(ml[0])], axis=1)
    xs = tok_shard(xT_ctx, xT_lat)
    maps = [{"xT": xs[c], "md": md, "w": w_in} for c in range(NCORE)]
    res = _run("inproj", build_inproj, maps)
    return tok_unshard([r["out"] for r in res])


def _ln_block(P, r, n, lng, lnb, out, ones, ps1, ps2, rc, sq, rstd):
    for k in range(8):
        P.mm(ps1[:, 0:n], ones[:], r[:, k, 0:n], start=(k == 0), stop=(k == 7))
    for k in range(8):
        P.stt(rc.sub(k, (slice(None), k, slice(0, n))), ps1[:, 0:n], -1.0 / DM, r[:, k, 0:n], ALU.mult, ALU.add)
        if k % 2 == 0:
            P.act(sq.sub(k, (slice(None), k, slice(0, n))), rc.sub(k, (slice(None), k, slice(0, n))), AF.Square)
        else:
            P.tt(sq.sub(k, (slice(None), k, slice(0, n))), rc.sub(k, (slice(None), k, slice(0, n))),
                 rc.sub(k, (slice(None), k, slice(0, n))), ALU.mult, eng="pool")
    for k in range(8):
        P.mm(ps2[:, 0:n], ones[:], sq.sub(k, (slice(None), k, slice(0, n))), start=(k == 0), stop=(k == 7))
    P.act(rstd[:, 0:n], ps2[:, 0:n], AF.Sqrt, bias=EPS, scale=1.0 / DM)
    P.gen("dve", lambda e: e.reciprocal(rstd.t[:, 0:n], rstd.t[:, 0:n]), [rstd[:, 0:n]], [rstd[:, 0:n]])
    for k in range(8):
        P.tt(rc.sub(k, (slice(None), k, slice(0, n))), rc.sub(k, (slice(None), k, slice(0, n))), rstd[:, 0:n], ALU.mult,
             eng="dve" if k % 2 == 0 else "pool")
        P.ts(out.sub(k, (slice(None), k, slice(0, n))), rc.sub(k, (slice(None), k, slice(0, n))),
             lng[:, k:k + 1], lnb[:, k:k + 1], ALU.mult, ALU.add, eng="dve" if k % 2 == 1 else "pool")


PB = 256
POST_BLKS = [(0, NCX)] + [(NCX + PB * b, NCX + PB * (b + 1)) for b in range(NLT // PB)]


def build_post1():
    nc = bass.Bass("TRN2", target_bir_lowering=False)
    mixin = _dram(nc, "mixin", [2048, NTK], F32)
    xT = _dram(nc, "xT", [1024, NTK], F32)
    wo = _dram(nc, "wo", [1024, 1024], F32)
    pr = _dram(nc, "pr", [128, 36], F32)
    out = _dram(nc, "out", [1024, NTK], F32, out=True)
    P = Prog(nc)
    W = P.sb([128, 8, 1024], BF16)
    prm = P.sb([128, 36], F32)
    ones = P.sb([128, 128], F32)
    P.dma(prm[:], pr)
    for k in range(8):
        P.dma(W.sub(k, (slice(None), k)), wo[k * 128:(k + 1) * 128, :], q="pool")
    P.memset(ones[:], 1.0)
    ins = [P.sb([128, 16, PB], F32) for _ in range(2)]
    xs = [P.sb([128, 8, PB], F32) for _ in range(2)]
    g = P.sb([128, 4, PB], F32)
    sz = P.sb([128, 4, PB], F32)
    gsq = P.sb([128, 4, PB], F32)
    rstdg = P.sb([128, 2, PB], F32)
    mix = P.sb([128, 8, PB], BF16)
    r = P.sb([128, 8, PB], F32)
    rc = P.sb([128, 8, PB], F32)
    sq = P.sb([128, 8, PB], F32)
    rstd = P.sb([128, PB], F32)
    ob = [P.sb([128, 8, PB], F32) for _ in range(2)]
    psg = [P.ps([128, 512], F32) for _ in range(2)]
    psm = [P.ps([128, 512], F32) for _ in range(3)]
    ps1 = P.ps([128, 512], F32)
    ps2 = P.ps([128, 512], F32)
    mi = 0
    for bi, (n0, n1) in enumerate(POST_BLKS):
        n = n1 - n0
        it = ins[bi % 2]
        xt = xs[bi % 2]
        o = ob[bi % 2]
        for k in range(16):
            P.dma(it.sub(k, (slice(None), k, slice(0, n))), mixin[k * 128:(k + 1) * 128, n0:n1],
                  q="sp" if k % 2 == 0 else "act")
        for k in range(8):
            P.dma(xt.sub(k, (slice(None), k, slice(0, n))), xT[k * 128:(k + 1) * 128, n0:n1],
                  q="sp" if k % 2 == 0 else "act")
        sl = slice(0, n)
        S = slice(None)
        for (kd, ks) in ((0, 0), (1, 1), (6, 14), (7, 15)):
            P.copy(mix.sub(kd, (S, kd, sl)), it.sub(ks, (S, ks, sl)), eng="pool")
        for c in range(4):
            P.tt(g.sub(c, (S, c, sl)), it.sub(2 + c, (S, 2 + c, sl)), it.sub(6 + c, (S, 6 + c, sl)), ALU.add)
            P.act(sz.sub(c, (S, c, sl)), it.sub(10 + c, (S, 10 + c, sl)), AF.Silu)
            P.tt(g.sub(c, (S, c, sl)), g.sub(c, (S, c, sl)), sz.sub(c, (S, c, sl)), ALU.mult)
            P.tt(gsq.sub(c, (S, c, sl)), g.sub(c, (S, c, sl)), g.sub(c, (S, c, sl)), ALU.mult, eng="pool")
        for gi in range(2):
            for j in range(2):
                c = gi * 2 + j
                P.mm(psg[gi][:, sl], ones[:], gsq.sub(c, (S, c, sl)), start=(j == 0), stop=(j == 1))
            P.act(rstdg.sub(gi, (S, gi, sl)), psg[gi][:, sl], AF.Sqrt, bias=EPS, scale=1.0 / 256)
            P.gen("dve", lambda e, gi=gi, sl=sl: e.reciprocal(rstdg.t[:, gi, sl], rstdg.t[:, gi, sl]),
                  [rstdg.sub(gi, (S, gi, sl))], [rstdg.sub(gi, (S, gi, sl))])
            for j in range(2):
                c = gi * 2 + j
                P.stt(mix.sub(2 + c, (S, 2 + c, sl)), g.sub(c, (S, c, sl)), prm[:, c:c + 1],
                      rstdg.sub(gi, (S, gi, sl)), ALU.mult, ALU.mult)
        g1o = 4 if bi == 0 else 12
        for j in range(8):
            ps = psm[mi % 3]
            mi += 1
            for k in range(8):
                P.mm(ps[:, sl], W.sub(k, (S, k, slice(j * 128, (j + 1) * 128))), mix.sub(k, (S, k, sl)),
                     start=(k == 0), stop=(k == 7))
            P.act(r.sub(j, (S, j, sl)), ps[:, sl], AF.Identity, scale=prm[:, g1o + j:g1o + j + 1])
            P.stt(r.sub(j, (S, j, sl)), xt.sub(j, (S, j, sl)), ALPHA, r.sub(j, (S, j, sl)), ALU.mult, ALU.add)
        _ln_block(P, _AllView(r), n, _Cols(prm, 20), _Cols(prm, 28), o, ones, ps1, ps2, rc, sq, rstd)
        for k in range(8):
            P.dma(out[k * 128:(k + 1) * 128, n0:n1], o.sub(k, (S, k, sl)), q="sp" if k % 2 == 0 else "act")
    P.finish()
    return nc


class _AllView:
    def __init__(self, b):
        self.b = b

    def __getitem__(self, idx):
        return self.b.all(idx)


class _Cols:
    def __init__(self, b, off):
        self.b = b
        self.off = off

    def __getitem__(self, idx):
        p, c = idx
        return self.b[p, slice(c.start + self.off, c.stop + self.off)]


def run_post1(mix_rows, xT_ctx, xT_lat, mod, layer, inp):
    ml = mod[0, layer].reshape(6, 1024)
    mc = mod[1, layer].reshape(6, 1024)
    pr = np.concatenate([_fm(inp["ssd_norm_w"][layer]), _fm(mc[2]), _fm(ml[2]), _fm(inp["ln1_g"][layer]),
                         _fm(inp["ln1_b"][layer])], axis=1)
    ms = tok_shard(mix_rows[:, :LC], mix_rows[:, LC:])
    xs = tok_shard(xT_ctx, xT_lat)
    maps = [{"mixin": ms[c], "xT": xs[c], "wo": inp["w_out"][layer], "pr": pr} for c in range(NCORE)]
    res = _run("post1", build_post1, maps)
    return tok_unshard([r["out"] for r in res])


FB = 256
NFB = NLT // FB
FFN_W = NCX + 2 + NLT + 2
FFN_BLKS = [(0, NCX, 0)] + [(NCX + 2 + FB * b, FB, NCX + FB * b) for b in range(NFB)]
DFF = 2816


def build_ffn():
    nc = bass.Bass("TRN2", target_bir_lowering=False)
    x1 = _dram(nc, "x1", [1024, FFN_W], F32)
    mk = _dram(nc, "mk", [128, 2 * (NFB + 1)], F32)
    pr = _dram(nc, "pr", [128, 64], F32)
    cw = _dram(nc, "cw", [128, 132], F32)
    cb = _dram(nc, "cb", [128, 44], F32)
    wu = _dram(nc, "wu", [1024, 2 * DFF], F32)
    wd = _dram(nc, "wd", [DFF, 1024], F32)
    out = _dram(nc, "out", [1024, NTK], F32, out=True)
    P = Prog(nc)
    S = slice(None)
    prm = P.sb([128, 64], F32)
    prm1 = P.sb([128, 64], F32)
    mkt = P.sb([128, 2 * (NFB + 1)], F32)
    cwt = P.sb([128, 132], F32)
    cbt = P.sb([128, 44], F32)
    ones = P.sb([128, 128], F32)
    WU = P.sb([128, 8, 2 * DFF], BF16)
    WD = P.sb([128, 22, 1024], BF16)
    P.dma(prm[:], pr)
    P.dma(mkt[:], mk)
    P.dma(cwt[:], cw)
    P.dma(cbt[:], cb)
    P.memset(ones[:], 1.0)
    P.ts(prm1[:], prm[:], 1.0, None, ALU.add)
    for jj in range(4):
        for k in range(8):
            c0 = jj * 1408
            P.dma(WU.sub((k, jj), (S, k, slice(c0, c0 + 1408))), wu[k * 128:(k + 1) * 128, c0:c0 + 1408], q="pool")
    for j in range(22):
        P.dma(WD.sub(j, (S, j)), wd[j * 128:(j + 1) * 128, :], q="pool")

    def wu_view(k, c0):
        jj = c0 // 1408
        jj2 = (c0 + 127) // 1408
        trks = [WU.subs[(k, jj)]] + ([WU.subs[(k, jj2)]] if jj2 != jj else [])
        return V(WU.t[:, k, c0:c0 + 128], trks)

    WD_ = FB + 2
    xb = [P.sb([128, 8, WD_], F32) for _ in range(2)]
    h2 = P.sb([128, 8, WD_], BF16)
    u = [P.sb([128, WD_], F32) for _ in range(4)]
    cc = [P.sb([128, FB], F32) for _ in range(4)]
    sg = [P.sb([128, FB], F32) for _ in range(2)]
    a = P.sb([128, 22, FB], BF16)
    r = P.sb([128, 8, FB], F32)
    sq = [P.sb([128, 8, FB], F32) for _ in range(2)]
    rstd = P.sb([128, FB], F32)
    psu = [P.ps([128, 512], F32) for _ in range(4)]
    psd = [P.ps([128, 512], F32) for _ in range(2)]
    ps1 = P.ps([128, 512], F32)
    ps2 = P.ps([128, 512], F32)
    ui = 0
    di = 0
    for bi, (c0, n, o0) in enumerate(FFN_BLKS):
        wdt = n + 2
        xt = xb[bi % 2]
        po = 0 if bi == 0 else 24
        for k in range(8):
            P.dma(xt.sub(k, (S, k, slice(0, wdt))), x1[k * 128:(k + 1) * 128, c0:c0 + wdt],
                  q="sp" if k % 2 == 0 else "act")
        for k in range(8):
            P.act(h2.sub(k, (S, k, slice(0, wdt))), xt.sub(k, (S, k, slice(0, wdt))), AF.Identity,
                  scale=prm1[:, po + k:po + k + 1], bias=prm[:, po + 8 + k:po + 9 + k])
        for j in range(22):
            cs = []
            for wi in range(2):
                ch = wi * 22 + j
                col0 = ch * 128
                ps = psu[ui % 4]
                ub = u[ui % 4]
                cb_ = cc[ui % 4]
                ui += 1
                for k in range(8):
                    P.mm(ps[:, 0:wdt], wu_view(k, col0), h2.sub(k, (S, k, slice(0, wdt))), start=(k == 0), stop=(k == 7))
                P.copy(ub[:, 0:wdt], ps[:, 0:wdt], eng="act")
                P.ts(ub[:, 0:1], ub[:, 0:1], mkt[:, 2 * bi:2 * bi + 1], None, ALU.mult)
                P.ts(ub[:, wdt - 1:wdt], ub[:, wdt - 1:wdt], mkt[:, 2 * bi + 1:2 * bi + 2], None, ALU.mult)
                P.ts(cb_[:, 0:n], ub[:, 1:n + 1], cwt[:, ch * 3 + 1:ch * 3 + 2], cbt[:, ch:ch + 1], ALU.mult, ALU.add)
                P.stt(cb_[:, 0:n], ub[:, 0:n], cwt[:, ch * 3:ch * 3 + 1], cb_[:, 0:n], ALU.mult, ALU.add)
                P.stt(cb_[:, 0:n], ub[:, 2:n + 2], cwt[:, ch * 3 + 2:ch * 3 + 3], cb_[:, 0:n], ALU.mult, ALU.add)
                cs.append(cb_)
            sgt = sg[j % 2]
            P.act(sgt[:, 0:n], cs[0][:, 0:n], AF.Silu)
            P.tt(a.sub(j, (S, j, slice(0, n))), sgt[:, 0:n], cs[1][:, 0:n], ALU.mult, eng="pool")
        for i in range(8):
            ps = psd[di % 2]
            di += 1
            for j in range(22):
                P.mm(ps[:, 0:n], WD.sub(j, (S, j, slice(i * 128, (i + 1) * 128))), a.sub(j, (S, j, slice(0, n))),
                     start=(j == 0), stop=(j == 21))
            P.act(r.sub(i, (S, i, slice(0, n))), ps[:, 0:n], AF.Identity, scale=prm[:, po + 16 + i:po + 17 + i])
            P.stt(r.sub(i, (S, i, slice(0, n))), xt.sub(i, (S, i, slice(1, n + 1))), ALPHA, r.sub(i, (S, i, slice(0, n))),
                  ALU.mult, ALU.add)
        o = sq[bi % 2]
        _ln_block(P, _AllView(r), n, _Cols(prm, 48), _Cols(prm, 56), o, ones, ps1, ps2, r, o, rstd)
        for k in range(8):
            P.dma(out[k * 128:(k + 1) * 128, o0:o0 + n], o.sub(k, (S, k, slice(0, n))), q="sp" if k % 2 == 0 else "act")
    P.finish()
    return nc


def run_ffn(x1T, mod, layer, inp):
    ml = mod[0, layer].reshape(6, 1024)
    mc = mod[1, layer].reshape(6, 1024)
    pr = np.concatenate([_fm(mc[4]), _fm(mc[3]), _fm(mc[5]), _fm(ml[4]), _fm(ml[3]), _fm(ml[5]),
                         _fm(inp["ln2_g"][layer]), _fm(inp["ln2_b"][layer])], axis=1)
    cw = np.ascontiguousarray(inp["ffn_conv_w"][layer].T.reshape(44, 128, 3).transpose(1, 0, 2).reshape(128, 132))
    cb = _fm(inp["ffn_conv_b"][layer])
    z = np.zeros((1024, 1), np.float32)
    xc = np.concatenate([z, x1T[:, :LC], z], axis=1)
    xl = np.concatenate([z, x1T[:, LC:], z], axis=1)
    maps = []
    for c in range(NCORE):
        xin = np.ascontiguousarray(np.concatenate([xc[:, c * NCX:c * NCX + NCX + 2], xl[:, c * NLT:c * NLT + NLT + 2]], axis=1))
        mk = np.ones((128, 2 * (NFB + 1)), np.float32)
        if c == 0:
            mk[:, 0] = 0.0
            mk[:, 2] = 0.0
        if c == NCORE - 1:
            mk[:, 1] = 0.0
            mk[:, 2 * NFB + 1] = 0.0
        maps.append({"x1": xin, "mk": mk, "pr": pr, "cw": cw, "cb": cb, "wu": inp["ffn_w_up"][layer],
                     "wd": inp["ffn_w_down"][layer]})
    res = _run("ffn", build_ffn, maps)
    return tok_unshard([r["out"] for r in res])


QH = L // 2
QC = LC // 2
NKT = TT // 128


def build_attn():
    nc = bass.Bass("TRN2", target_bir_lowering=False)
    qT = _dram(nc, "qT", [64, QC + QH], F32)
    kT = _dram(nc, "kT", [64, TT], F32)
    v = _dram(nc, "v", [TT, 64], F32)
    cosk = _dram(nc, "cosk", [64, L], F32)
    sink = _dram(nc, "sink", [64, L], F32)
    cosq = _dram(nc, "cosq", [64, QH], F32)
    sinq = _dram(nc, "sinq", [64, QH], F32)
    cst = _dram(nc, "cst", [64, 66], F32)
    out = _dram(nc, "out", [64, QC + QH], F32, out=True)
    P = Prog(nc)
    S = slice(None)
    ct = P.sb([64, 66], F32)
    ones = P.sb([128, 64], F32)
    KT = P.sb([64, TT], BF16)
    QT = P.sb([64, QC + QH], BF16)
    VA = P.sb([128, NKT, 65], BF16)
    P.dma(ct[:], cst)
    P.memset(ones[:], 1.0)
    vv = v.rearrange("(t p) d -> p t d", p=128)
    for i in range(5):
        P.dma(VA.sub(i, (S, slice(i * 26, (i + 1) * 26), slice(0, 64))), vv[:, i * 26:(i + 1) * 26, :], q="pool")
    P.memset(VA.sub("one", (S, S, slice(64, 65))), 1.0)
    xin = [P.sb([64, 512], F32) for _ in range(2)]
    cin = [P.sb([64, 512], F32) for _ in range(2)]
    sin_ = [P.sb([64, 512], F32) for _ in range(2)]
    sq = P.sb([64, 512], F32)
    rstd = P.sb([64, 512], F32)
    xn = P.sb([64, 512], F32)
    t1 = P.sb([64, 512], F32)
    t2 = P.sb([64, 512], F32)
    pss = P.ps([64, 512], F32)
    psr = P.ps([64, 512], F32)
    cnt = [0]

    def prep(src, c0, n, gcol, tabs, dst, d0):
        i = cnt[0] % 2
        cnt[0] += 1
        x = xin[i]
        P.dma(x[:, 0:n], src[:, c0:c0 + n], q="sp")
        if tabs is not None:
            P.dma(cin[i][:, 0:n], tabs[0][:, tabs[2]:tabs[2] + n], q="act")
            P.dma(sin_[i][:, 0:n], tabs[1][:, tabs[2]:tabs[2] + n], q="act")
        P.act(sq[:, 0:n], x[:, 0:n], AF.Square)
        P.mm(pss[0:64, 0:n], ones[0:64, :], sq[:, 0:n])
        P.act(rstd[:, 0:n], pss[0:64, 0:n], AF.Sqrt, bias=EPS, scale=1.0 / 64)
        P.gen("dve", lambda e: e.reciprocal(rstd.t[:, 0:n], rstd.t[:, 0:n]), [rstd[:, 0:n]], [rstd[:, 0:n]])
        if tabs is None:
            P.stt(dst[:, d0:d0 + n], x[:, 0:n], ct[:, gcol:gcol + 1], rstd[:, 0:n], ALU.mult, ALU.mult)
            return
        P.stt(xn[:, 0:n], x[:, 0:n], ct[:, gcol:gcol + 1], rstd[:, 0:n], ALU.mult, ALU.mult)
        P.mm(psr[0:64, 0:n], ct[:, 0:64], xn[:, 0:n])
        P.tt(t1[:, 0:n], xn[:, 0:n], cin[i][:, 0:n], ALU.mult, eng="pool")
        P.tt(t2[:, 0:n], psr[0:64, 0:n], sin_[i][:, 0:n], ALU.mult)
        P.tt(dst[:, d0:d0 + n], t1[:, 0:n], t2[:, 0:n], ALU.add)

    prep(kT, 0, LC, 65, None, KT, 0)
    for i in range(L // 512):
        prep(kT, LC + 512 * i, 512, 65, (cosk, sink, 512 * i), KT, LC + 512 * i)
    prep(qT, 0, QC, 64, None, QT, 0)
    for i in range(QH // 512):
        prep(qT, QC + 512 * i, 512, 64, (cosq, sinq, 512 * i), QT, QC + 512 * i)

    psS = [P.ps([128, 512], F32) for _ in range(3)]
    psO = [P.ps([128, 512], F32) for _ in range(2)]
    psB = P.ps([64, 512], F32)
    pT = [P.sb([128, 512], BF16) for _ in range(3)]
    osb = [P.sb([65, 512], F32) for _ in range(2)]
    yo = [P.sb([64, 512], F32) for _ in range(2)]
    blocks = [(0, QC, 0, 2)] + [(QC + 512 * i, 512, 0, NKT) for i in range(QH // 512)]
    si = 0
    for bi, (q0, n, k0, k1) in enumerate(blocks):
        po = psO[bi % 2]
        for kt in range(k0, k1):
            ps = psS[si % 3]
            pt = pT[si % 3]
            si += 1
            P.mm(ps[:, 0:n], KT[:, kt * 128:(kt + 1) * 128], QT[:, q0:q0 + n])
            P.act(pt[:, 0:n], ps[:, 0:n], AF.Exp, scale=0.125)
            P.mm(po[0:65, 0:n], VA.all((S, kt, slice(0, 65))), pt[:, 0:n], start=(kt == k0), stop=(kt == k1 - 1))
        ob = osb[bi % 2]
        y = yo[bi % 2]
        P.copy(ob[:, 0:n], po[0:65, 0:n], eng="dve")
        P.gen("dve", lambda e, ob=ob, n=n: e.reciprocal(ob.t[64:65, 0:n], ob.t[64:65, 0:n]), [ob[64:65, 0:n]], [ob[64:65, 0:n]])
        P.mm(psB[0:64, 0:n], ones[64:65, :], ob[64:65, 0:n])
        P.tt(y[:, 0:n], ob[0:64, 0:n], psB[0:64, 0:n], ALU.mult)
        P.dma(out[:, q0:q0 + n], y[:, 0:n], q="sp")
    P.finish()
    return nc


def rope_tables():
    rows = L // 64
    row = np.repeat(np.arange(rows, dtype=np.float32), 64)
    col = np.tile(np.arange(64, dtype=np.float32), rows)
    inv = (np.float32(10000.0) ** (-np.arange(16, dtype=np.float32) / np.float32(16))).astype(np.float32)
    ang = np.concatenate([row[:, None] * inv, col[:, None] * inv], axis=-1).astype(np.float32)
    cos = np.cos(ang).astype(np.float32).T
    sin = np.sin(ang).astype(np.float32).T
    return (np.ascontiguousarray(np.concatenate([cos, cos], axis=0)),
            np.ascontiguousarray(np.concatenate([sin, sin], axis=0)))


def run_attn(proj, layer, inp):
    cosf, sinf = rope_tables()
    RT = np.zeros((64, 64), np.float32)
    for dp in range(32):
        RT[dp + 32, dp] = -1.0
        RT[dp, dp + 32] = 1.0
    cst = np.concatenate([RT, inp["attn_q_norm"][layer][:, None], inp["attn_k_norm"][layer][:, None]], axis=1)
    cst = np.ascontiguousarray(cst.astype(np.float32))
    maps = []
    for c in range(NCORE):
        g, h, half = c // 4, c // 2, c % 2
        qrows = proj[2320 + 64 * h:2320 + 64 * (h + 1)]
        qT = np.ascontiguousarray(np.concatenate([qrows[:, half * QC:(half + 1) * QC],
                                                  qrows[:, LC + half * QH:LC + (half + 1) * QH]], axis=1))
        kT = np.ascontiguousarray(proj[2576 + 64 * g:2576 + 64 * (g + 1)])
        v = np.ascontiguousarray(proj[2704 + 64 * g:2704 + 64 * (g + 1)].T)
        maps.append({"qT": qT, "kT": kT, "v": v, "cosk": cosf, "sink": sinf,
                     "cosq": np.ascontiguousarray(cosf[:, half * QH:(half + 1) * QH]),
                     "sinq": np.ascontiguousarray(sinf[:, half * QH:(half + 1) * QH]), "cst": cst})
    res = _run("attn", build_attn, maps)
    y = np.zeros((256, TT), np.float32)
    for c in range(NCORE):
        h, half = c // 2, c % 2
        o = res[c]["out"]
        y[64 * h:64 * (h + 1), half * QC:(half + 1) * QC] = o[:, :QC]
        y[64 * h:64 * (h + 1), LC + half * QH:LC + (half + 1) * QH] = o[:, QC:]
    return y


NCH = TT // 128
SSD_W = LC + 2 + L + 2
SB = 1024
SSD_STAGE = 9
SSD_BLKS = [(0, LC, 0)] + [(LC + 2 + SB * b, SB, LC + SB * b) for b in range(L // SB)]


def build_ssd():
    nc = bass.Bass("TRN2", target_bir_lowering=False)
    xr = _dram(nc, "xr", [2, 64, SSD_W], F32)
    br = _dram(nc, "br", [2, 128, SSD_W], F32)
    cr = _dram(nc, "cr", [2, 128, SSD_W], F32)
    dtr = _dram(nc, "dtr", [2, 128, NCH], F32)
    scl = _dram(nc, "scl", [2, 128, 4], F32)
    cwx = _dram(nc, "cwx", [2, 64, 4], F32)
    cwb = _dram(nc, "cwb", [2, 128, 4], F32)
    cwc = _dram(nc, "cwc", [2, 128, 4], F32)
    cst = _dram(nc, "cst", [128, 384], F32)
    out = _dram(nc, "out", [2, TT, 64], F32, out=True)
    P = Prog(nc)
    S = slice(None)
    ct = P.sb([128, 384], F32)
    ones = P.sb([128, 128], F32)
    identb = P.sb([128, 128], BF16)
    P.dma(ct[:], cst)
    P.memset(ones[:], 1.0)
    P.copy(identb[:], ct[:, 256:384])
    tri = ct[:, 0:128]
    negm = ct[:, 128:256]
    ident = ct[:, 256:384]
    ps_big = P.ps([128, 512], F32)
    ps_trb = P.ps([128, 128], BF16)
    ps_trx = P.ps([128, 64], F32)
    ps_sc = P.ps([128, 128], F32)
    ps_seg = P.ps([128, 128], F32)
    ps_y = P.ps([128, 64], F32)
    ps_i = P.ps([128, 64], F32)
    ps_s = P.ps([128, 64], F32)
    dtt = P.sb([128, NCH], F32)
    sc4 = P.sb([128, 4], F32)
    e1 = P.sb([128, NCH], F32)
    dt = P.sb([128, NCH], F32)
    at = P.sb([128, 1], F32)
    dta = P.sb([128, NCH], F32)
    acum = P.sb([128, NCH], F32)
    nacum = P.sb([128, NCH], F32)
    ea = P.sb([128, NCH], F32)
    dch = P.sb([128, NCH], F32)
    wv = P.sb([128, NCH], F32)
    cx = P.sb([64, 4], F32)
    cb = P.sb([128, 4], F32)
    cc = P.sb([128, 4], F32)
    W2 = SB + 2
    xraw = P.sb([64, W2], F32)
    braw = P.sb([128, W2], F32)
    craw = P.sb([128, W2], F32)
    tx = P.sb([64, SB], F32)
    tb = P.sb([128, SB], F32)
    tc_ = P.sb([128, SB], F32)
    xT = P.sb([64, SB], F32)
    BT = P.sb([128, SB], BF16)
    CT = P.sb([128, SB], BF16)
    Bc = P.sb([128, 128], BF16)
    xdt = P.sb([128, 64], BF16)
    xw = P.sb([128, 64], BF16)
    xD = P.sb([128, 64], F32)
    dg = P.sb([128, 128], F32)
    dec = P.sb([128, 128], F32)
    MT = P.sb([128, 128], BF16)
    yi = P.sb([128, 64], F32)
    H = P.sb([128, 64], F32)
    Hb = P.sb([128, 64], BF16)
    ybuf = [P.sb([128, SB // 128, 64], F32) for _ in range(2)]
    yb_i = 0
    for j in range(2):
        P.dma(dtt[:], dtr[j])
        P.dma(sc4[:], scl[j])
        P.dma(cx[:], cwx[j])
        P.dma(cb[:], cwb[j])
        P.dma(cc[:], cwc[j])
        P.act(e1[:], dtt[:], AF.Exp, bias=sc4[:, 0:1])
        P.act(dt[:], e1[:], AF.Ln, bias=1.0)
        P.act(at[:], sc4[:, 1:2], AF.Exp)
        P.ts(at[:], at[:], -1.0, None, ALU.mult)
        P.ts(dta[:], dt[:], at[:, 0:1], None, ALU.mult)
        P.mm(ps_big[:, 0:NCH], tri, dta[:])
        P.copy(acum[:], ps_big[:, 0:NCH])
        P.ts(nacum[:], acum[:], -1.0, None, ALU.mult)
        P.act(ea[:], acum[:], AF.Exp)
        P.mm(ps_big[:, 256:256 + NCH], ones[:], dta[:])
        P.act(dch[:], ps_big[:, 256:256 + NCH], AF.Exp)
        P.tt(wv[:], ps_big[:, 256:256 + NCH], acum[:], ALU.subtract)
        P.act(wv[:], wv[:], AF.Exp)
        P.tt(wv[:], wv[:], dt[:], ALU.mult)
        P.memset(H[:], 0.0)
        P.memset(Hb[:], 0.0)
        for (c0, n, t0) in (SSD_BLKS if SSD_STAGE > 0 else []):
            P.dma(xraw[:, 0:n + 2], xr[j, :, c0:c0 + n + 2], q="sp")
            P.dma(braw[:, 0:n + 2], br[j, :, c0:c0 + n + 2], q="act")
            P.dma(craw[:, 0:n + 2], cr[j, :, c0:c0 + n + 2], q="sp")
            for (raw, tmp, w4, dst, np_) in ((xraw, tx, cx, xT, 64), (braw, tb, cb, BT, 128), (craw, tc_, cc, CT, 128)):
                P.ts(tmp[:, 0:n], raw[:, 1:n + 1], w4[:, 1:2], w4[:, 3:4], ALU.mult, ALU.add)
                P.stt(tmp[:, 0:n], raw[:, 0:n], w4[:, 0:1], tmp[:, 0:n], ALU.mult, ALU.add)
                P.stt(tmp[:, 0:n], raw[:, 2:n + 2], w4[:, 2:3], tmp[:, 0:n], ALU.mult, ALU.add)
                P.act(dst[:, 0:n], tmp[:, 0:n], AF.Silu)
            yb = ybuf[yb_i % 2]
            yb_i += 1
            for ci in range(n // 128):
                c = t0 // 128 + ci
                cs = slice(ci * 128, (ci + 1) * 128)
                if SSD_STAGE < 2:
                    P.memset(yb[:, ci, :], 0.0)
                    continue
                P.tr(ps_trb[:, 0:128], BT[:, cs], identb[:])
                P.copy(Bc[:], ps_trb[:, 0:128], eng="act")
                P.tr(ps_trx[:, 0:64], xT[:, cs], ident[0:64, 0:64])
                P.ts(xdt[:], ps_trx[:, 0:64], dt[:, c:c + 1], None, ALU.mult)
                P.ts(xw[:], ps_trx[:, 0:64], wv[:, c:c + 1], None, ALU.mult)
                P.ts(xD[:], ps_trx[:, 0:64], sc4[:, 2:3], None, ALU.mult)
                P.mm(ps_sc[:, 0:128], BT[:, cs], CT[:, cs])
                P.ts(dg[:], ident, acum[:, c:c + 1], None, ALU.mult, eng="pool")
                P.mm(ps_seg[:, 0:128], ones[:], dg[:], start=True, stop=False)
                P.mm(ps_seg[:, 0:128], ident, negm, start=False, stop=True)
                P.act(dec[:], ps_seg[:, 0:128], AF.Exp, bias=nacum[:, c:c + 1])
                P.tt(MT[:], ps_sc[:, 0:128], dec[:], ALU.mult)
                P.mm(ps_y[:, 0:64], MT[:], xdt[:])
                P.mm(ps_i[:, 0:64], CT[:, cs], Hb[:])
                P.act(yi[:], ps_y[:, 0:64], AF.Identity)
                P.tt(yi[:], yi[:], xD[:], ALU.add, eng="pool")
                P.stt(yb[:, ci, :], ps_i[:, 0:64], ea[:, c:c + 1], yi[:], ALU.mult, ALU.add)
                P.mm(ps_s[:, 0:64], Bc[:], xw[:])
                P.stt(H[:], H[:], dch[:, c:c + 1], ps_s[:, 0:64], ALU.mult, ALU.add)
                P.copy(Hb[:], H[:], eng="act")
            P.dma(out[j, t0:t0 + n, :].rearrange("(c p) d -> p c d", p=128), yb[:, 0:n // 128, :], q="act")
    P.finish()
    return nc


def _flipseq(a):
    return np.concatenate([a[..., :LC][..., ::-1], a[..., LC:][..., ::-1]], axis=-1)


def _padseq(a):
    z = np.zeros(a.shape[:-1] + (1,), np.float32)
    return np.concatenate([z, a[..., :LC], z, z, a[..., LC:], z], axis=-1)


def ssd_consts():
    s = np.arange(128)
    tri = (s[:, None] <= s[None, :]).astype(np.float32)
    negm = np.where(s[:, None] > s[None, :], -30000.0, 0.0).astype(np.float32)
    return np.ascontiguousarray(np.concatenate([tri, negm, np.eye(128, dtype=np.float32)], axis=1))


def run_ssd(proj, layer, inp):
    cst = ssd_consts()
    cw = inp["ssd_conv_w"][layer]
    cbias = inp["ssd_conv_b"][layer]
    maps = []
    for hd in range(NCORE):
        g = hd // 4
        rows = {"x": slice(1280 + 64 * hd, 1280 + 64 * (hd + 1)),
                "b": slice(1792 + 128 * g, 1792 + 128 * (g + 1)),
                "c": slice(2048 + 128 * g, 2048 + 128 * (g + 1))}
        chs = {"x": slice(64 * hd, 64 * (hd + 1)), "b": slice(512 + 128 * g, 512 + 128 * (g + 1)),
               "c": slice(768 + 128 * g, 768 + 128 * (g + 1))}
        m = {}
        for nm, key in (("xr", "x"), ("br", "b"), ("cr", "c")):
            a = proj[rows[key]]
            m[nm] = np.ascontiguousarray(np.stack([_padseq(a), _padseq(_flipseq(a))]))
        for nm, key in (("cwx", "x"), ("cwb", "b"), ("cwc", "c")):
            w = cw[:, chs[key]]
            b = cbias[chs[key]]
            f = np.stack([w[0], w[1], w[2], b], axis=1)
            bk = np.stack([w[2], w[1], w[0], b], axis=1)
            m[nm] = np.ascontiguousarray(np.stack([f, bk]).astype(np.float32))
        dts = []
        scl = []
        for dr in range(2):
            raw = proj[2304 + dr * 8 + hd]
            if dr == 1:
                raw = _flipseq(raw)
            dts.append(raw.reshape(NCH, 128).T)
            s4 = np.zeros((128, 4), np.float32)
            s4[:, 0] = inp["ssd_dt_bias"][layer][dr, hd]
            s4[:, 1] = inp["ssd_a_log"][layer][dr, hd]
            s4[:, 2] = inp["ssd_d"][layer][hd] if dr == 0 else 0.0
            scl.append(s4)
        m["dtr"] = np.ascontiguousarray(np.stack(dts).astype(np.float32))
        m["scl"] = np.ascontiguousarray(np.stack(scl))
        m["cst"] = cst
        maps.append(m)
    res = _run("ssd", build_ssd, maps)
    yf = np.concatenate([res[hd]["out"][0].T for hd in range(NCORE)], axis=0)
    yb = np.concatenate([_flipseq(res[hd]["out"][1].T) for hd in range(NCORE)], axis=0)
    return np.ascontiguousarray(yf), np.ascontiguousarray(yb)


HN = 2 * L
HCH = 32
PI = float(np.pi)


def hyena_consts():
    n1 = np.arange(128)
    f1 = np.arange(128)
    n2 = np.arange(256)
    f2 = np.arange(256)
    a1 = 2 * np.pi * np.outer(n1, f1) / 128
    D1 = np.concatenate([np.cos(a1), -np.sin(a1)], axis=1)
    at = 2 * np.pi * np.outer(n2, f1) / HN
    TwC, TwS = np.cos(at), np.sin(at)
    a3 = 2 * np.pi * np.outer(n2, f2) / 256
    C3, S3 = np.cos(a3), np.sin(a3)
    E1 = np.concatenate([C3.T, S3.T], axis=1)
    E2 = np.concatenate([-S3.T, C3.T], axis=1)
    F1c = np.cos(a1.T)[:, :64] / HN
    F1s = -np.sin(a1.T)[:, :64] / HN
    c = {}
    c["d1"] = np.stack([D1[0:64], D1[64:128]])
    c["tw"] = np.stack([np.stack([np.concatenate([TwC[h * 128:(h + 1) * 128]] * 2, axis=1),
                                  np.concatenate([TwS[h * 128:(h + 1) * 128]] * 2, axis=1)]) for h in range(2)])
    c["tw"] = c["tw"].transpose(2, 0, 1, 3)
    c["c3"] = np.stack([np.stack([C3[h * 128:(h + 1) * 128], S3[h * 128:(h + 1) * 128], -S3[h * 128:(h + 1) * 128]])
                        for h in range(2)]).transpose(2, 0, 1, 3)
    c["e"] = np.stack([np.stack([E1[g * 128:(g + 1) * 128], E2[g * 128:(g + 1) * 128]]) for g in range(2)]
                      ).transpose(2, 0, 1, 3)
    c["tw2"] = np.stack([np.concatenate([TwC.T, TwC.T], axis=1), np.concatenate([TwS.T, TwS.T], axis=1)]
                        ).transpose(1, 0, 2)
    c["f1"] = np.stack([F1c, F1s]).transpose(1, 0, 2)
    return {k: np.ascontiguousarray(v.astype(np.float32)) for k, v in c.items()}


def _hy_feats(n, pos):
    t = np.linspace(0.0, 1.0, n, dtype=np.float32)
    w = ((2.0 * np.pi / n) * np.arange(n, dtype=np.float32)).astype(np.float32)
    f = np.linspace(1e-4, 15, 16, dtype=np.float32)[None, :]
    tt_ = t[pos][:, None]
    ww = w[pos][:, None]
    return np.concatenate([tt_, np.cos(f * ww), -np.sin(f * ww)], axis=-1).astype(np.float32)


def _hy_deltas():
    mn = np.log(1e-2) / 1.5
    mx = np.log(1e-2) / 0.3
    return np.abs(np.linspace(mn, mx, 256, dtype=np.float32))


def hyena_tables():
    n = np.arange(HN)
    t = np.where(n < L, n, HN - n)
    t = np.where(n == L, 0, t)
    feats = _hy_feats(L, t)
    featsP = feats.reshape(128, 256, 33).transpose(1, 0, 2).reshape(HN, 33).T
    tl = np.linspace(0.0, 1.0, L, dtype=np.float32)
    win = np.exp(-tl[t][:, None] * _hy_deltas()[None, :]).astype(np.float32)
    win[L] = 0.0
    win = win.reshape(2, 64, 256, 256)
    return np.ascontiguousarray(featsP.astype(np.float32)), win


def build_hyena(with_ctx):
    nc = bass.Bass("TRN2", target_bir_lowering=False)
    raw = _dram(nc, "raw", [3, HCH, 64, 258], F32)
    cwl = _dram(nc, "cwl", [64, 3 * HCH * 4], F32)
    skp = _dram(nc, "skp", [64, 2 * HCH], F32)
    w1 = _dram(nc, "w1", [33, 64], F32)
    w2 = _dram(nc, "w2", [64, 64], F32)
    w3s = _dram(nc, "w3s", [64, 4 * HCH], F32)
    fb = _dram(nc, "fb", [64, 3], F32)
    featsP = _dram(nc, "featsP", [33, HN], F32)
    win = _dram(nc, "win", [2, 64, 256, HCH], F32)
    d1 = _dram(nc, "d1", [2, 64, 256], F32)
    tw = _dram(nc, "tw", [128, 2, 2, 256], F32)
    c3 = _dram(nc, "c3", [128, 2, 3, 256], F32)
    ee = _dram(nc, "e", [128, 2, 2, 512], F32)
    tw2 = _dram(nc, "tw2", [128, 2, 512], F32)
    f1 = _dram(nc, "f1", [128, 2, 64], F32)
    out = _dram(nc, "out", [HCH, L], F32, out=True)
    if with_ctx:
        rawc = _dram(nc, "rawc", [3, HCH, 258], F32)
        cwc = _dram(nc, "cwc", [HCH, 12], F32)
        skc = _dram(nc, "skc", [HCH, 2], F32)
        featsC = _dram(nc, "featsC", [33, 256], F32)
        winC = _dram(nc, "winC", [HCH, 256], F32)
        outc = _dram(nc, "outc", [HCH, 256], F32, out=True)
    P = Prog(nc)
    S = slice(None)
    cw = P.sb([64, 3 * HCH * 4], F32)
    sk = P.sb([64, 2 * HCH], F32)
    W1 = P.sb([33, 64], F32)
    W2 = P.sb([64, 64], BF16)
    W3 = P.sb([64, 4 * HCH], BF16)
    FB = P.sb([64, 3], F32)
    FBB = P.sb([64, 2], F32)
    D1 = P.sb([64, 2, 256], BF16)
    TW = P.sb([128, 2, 2, 256], F32)
    C3 = P.sb([128, 2, 3, 256], BF16)
    EE = P.sb([128, 2, 2, 512], BF16)
    TW2 = P.sb([128, 2, 512], F32)
    F1 = P.sb([128, 2, 64], BF16)
    P.dma(cw[:], cwl)
    P.dma(sk[:], skp)
    P.dma(W1[:], w1)
    P.dma(FB[:], fb)
    P.dma(TW[:], tw)
    P.dma(TW2[:], tw2)
    P.dma(W2[:], w2, q="pool")
    P.dma(W3[:], w3s, q="pool")
    P.dma(D1[:], d1.rearrange("a p f -> p a f"), q="pool")
    P.dma(C3[:], c3, q="pool")
    P.dma(EE[:], ee, q="pool")
    P.dma(F1[:], f1, q="pool")
    P.ts(FBB[:], FB[:, 1:3], FB[:, 0:1], None, ALU.mult)
    banks = [P.ps([128, 512], F32) for _ in range(8)]

    def mlp(ft, n, h1, G):
        a = mlp_a
        P.mm(banks[0][0:64, 0:n], W1[:], ft)
        P.ts(a[:, 0:n], banks[0][0:64, 0:n], FB[:, 0:1], FBB[:, 0:1], ALU.mult, ALU.add)
        wrap(a, n)
        P.act(h1[:, 0:n], a[:, 0:n], AF.Sin)
        P.mm(banks[1][0:64, 0:n], W2[:], h1[:, 0:n])
        P.ts(a[:, 0:n], banks[1][0:64, 0:n], FB[:, 0:1], FBB[:, 1:2], ALU.mult, ALU.add)
        wrap(a, n)
        P.act(G[:, 0:n], a[:, 0:n], AF.Sin)

    def wrap(a, n):
        P.ts(m1[:, 0:n], a[:, 0:n], -PI, 2 * PI, ALU.is_lt, ALU.mult, eng="pool")
        P.ts(m2[:, 0:n], a[:, 0:n], PI, -2 * PI, ALU.is_gt, ALU.mult)
        P.tt(a[:, 0:n], a[:, 0:n], m1[:, 0:n], ALU.add)
        P.tt(a[:, 0:n], a[:, 0:n], m2[:, 0:n], ALU.add)

    mlp_a = P.sb([64, 512], F32)
    m1 = P.sb([64, 512], F32)
    m2 = P.sb([64, 512], F32)
    h1b = P.sb([64, 512], BF16)
    Gb = [P.sb([64, 512], BF16) for _ in range(2)]
    ftb = [P.sb([33, 512], F32) for _ in range(2)]
    wt = [P.sb([64, 2, 32, HCH], F32) for _ in range(2)]
    kf = P.sb([64, HCH, 256], BF16)
    kb = P.sb([64, HCH, 256], BF16)
    KH = P.sb([128, 2, 2, HCH * 128], BF16)
    z = P.sb([64, HCH, 256], F32)
    Bt = [P.sb([128, 2, 2, 512], BF16) for _ in range(2)]
    t12 = P.sb([128, 512], F32)
    t34 = P.sb([128, 512], F32)
    rawt = [P.sb([64, 258], F32) for _ in range(3)]
    vin = [P.sb([64, 4, 256], F32) for _ in range(2)]
    vb = [P.sb([64, 256], BF16) for _ in range(2)]
    Zt = P.sb([128, 2, 2, 512], BF16)
    pa = P.sb([128, 512], F32)
    pb = P.sb([128, 512], F32)
    Dt = [P.sb([128, 2, 512], BF16) for _ in range(2)]
    gate = [P.sb([64, 256], F32) for _ in range(2)]
    tq = [P.sb([64, 256], F32) for _ in range(2)]
    ot = [P.sb([64, 256], F32) for _ in range(2)]
    cnt = {"raw": 0, "g": 0, "vb": 0, "o": 0}

    def conv_row(w, ch, dst):
        r = rawt[cnt["raw"] % 3]
        cnt["raw"] += 1
        P.dma(r[:], raw[w, ch], q="sp" if cnt["raw"] % 2 == 0 else "act")
        o = (w * HCH + ch) * 4
        P.ts(dst, r[:, 1:257], cw[:, o + 1:o + 2], cw[:, o + 3:o + 4], ALU.mult, ALU.add)
        P.stt(dst, r[:, 0:256], cw[:, o:o + 1], dst, ALU.mult, ALU.add)
        P.stt(dst, r[:, 2:258], cw[:, o + 2:o + 3], dst, ALU.mult, ALU.add)

    def twiddle_fwd(psA, h, btile, ch4):
        P.tt(t12[:, 0:256], psA[:, 0:256], TW[:, h, 0, :], ALU.mult)
        P.tt(t34[:, 0:256], psA[:, 0:256], TW[:, h, 1, :], ALU.mult)
        cs = slice(ch4 * 128, (ch4 + 1) * 128)
        P.tt(btile.sub((h, 0, ch4), (S, h, 0, cs)), t12[:, 0:128], t34[:, 128:256], ALU.add)
        P.tt(btile.sub((h, 1, ch4), (S, h, 1, cs)), t12[:, 128:256], t34[:, 0:128], ALU.subtract, eng="pool")

    def step3(btile, evac):
        ball = _AllView(btile)
        for g in range(2):
            gs = slice(g * 128, (g + 1) * 128)
            for ri in range(2):
                ps = banks[2 + g * 2 + ri]
                terms = []
                for h in range(2):
                    if ri == 0:
                        terms += [(0, h, 0), (1, h, 1)]
                    else:
                        terms += [(0, h, 1), (2, h, 0)]
                for ti, (m, h, bri) in enumerate(terms):
                    P.mm(ps[:, :], C3[:, h, m, gs], ball[:, h, bri, :], start=(ti == 0), stop=(ti == 3))
                evac(g, ri, ps)

    for o in range(2):
        for blk in range(64):
            if blk % 8 == 0:
                w_ = wt[(blk // 8) % 2]
                for half in range(2):
                    P.dma(w_.sub(half, (S, half)), win[half, :, blk * 4:blk * 4 + 32, :], q="act")
            ft = ftb[blk % 2]
            G = Gb[blk % 2]
            P.dma(ft[:], featsP[:, blk * 512:(blk + 1) * 512], q="sp")
            mlp(ft[:], 512, h1b, G)
            for half in range(2):
                pk = banks[2 + half]
                wc = (o * 2 + half) * HCH
                for q in range(4):
                    P.mm(pk[0:64, q * HCH:(q + 1) * HCH], G[:, q * 128 + half * 64:q * 128 + half * 64 + 64],
                         W3[:, wc:wc + HCH])
                kt = kf if half == 0 else kb
                q0 = (blk % 8) * 4
                src = V(pk.t[0:64, 0:4 * HCH].rearrange("p (q c) -> p c q", q=4), [pk.trk], pk.bank)
                wv_ = V(w_.t[:, half, q0:q0 + 4, :].rearrange("p q c -> p c q"), [w_.subs[half]])
                P.tt(kt.sub(blk, (S, S, slice(blk * 4, blk * 4 + 4))), src, wv_, ALU.mult)
        kfa = _AllView(kf)
        kba = _AllView(kb)
        for cg in range(HCH // 4):
            bt = Bt[cg % 2]
            for ch4 in range(4):
                ch = cg * 4 + ch4
                for h in range(2):
                    psA = banks[h]
                    hs = slice(h * 128, (h + 1) * 128)
                    P.mm(psA[:, 0:256], kfa[:, ch, hs], D1[:, 0, :], start=True, stop=False)
                    P.mm(psA[:, 0:256], kba[:, ch, hs], D1[:, 1, :], start=False, stop=True)
                    twiddle_fwd(psA, h, bt, ch4)

            def evac_k(g, ri, ps, cg=cg):
                P.copy(KH.sub((g, ri, cg), (S, g, ri, slice(cg * 512, (cg + 1) * 512))), ps[:, :], eng="act")
            step3(bt, evac_k)
        for cg in range(HCH // 4):
            bt = Bt[cg % 2]
            vi = vin[cg % 2]
            for ch4 in range(4):
                ch = cg * 4 + ch4
                if o == 0:
                    conv_row(0, ch, vi.sub(ch4, (S, ch4, S)))
                    src = vi.sub(ch4, (S, ch4, S))
                else:
                    src = z.sub(ch, (S, ch, S))
                vbt = vb[cnt["vb"] % 2]
                cnt["vb"] += 1
                P.copy(vbt[:], src, eng="act")
                for h in range(2):
                    psA = banks[h]
                    P.mm(psA[:, 0:256], vbt[:, h * 128:(h + 1) * 128], D1[:, 0, :])
                    twiddle_fwd(psA, h, bt, ch4)

            def evac_z(g, ri, ps, cg=cg):
                if ri == 1:
                    xr = banks[2 + g * 2]
                    xi = banks[2 + g * 2 + 1]
                    cs = slice(cg * 512, (cg + 1) * 512)
                    kr = KH.sub((g, 0, cg), (S, g, 0, cs))
                    ki = KH.sub((g, 1, cg), (S, g, 1, cs))
                    P.tt(pa[:], xr[:, :], kr, ALU.mult)
                    P.tt(pb[:], xi[:, :], ki, ALU.mult)
                    P.tt(Zt.sub((g, 0), (S, g, 0, S)), pa[:], pb[:], ALU.subtract, eng="pool")
                    P.tt(pa[:], xr[:, :], ki, ALU.mult)
                    P.tt(pb[:], xi[:, :], kr, ALU.mult)
                    P.tt(Zt.sub((g, 1), (S, g, 1, S)), pa[:], pb[:], ALU.add, eng="pool")
            step3(bt, evac_z)
            for ch4 in range(4):
                ch = cg * 4 + ch4
                cs = slice(ch4 * 128, (ch4 + 1) * 128)
                psC = banks[6]
                ti = 0
                for g in range(2):
                    for ri in range(2):
                        P.mm(psC[:, :], Zt.sub((g, ri), (S, g, ri, cs)), EE[:, g, ri, :], start=(ti == 0), stop=(ti == 3))
                        ti += 1
                dt_ = Dt[(ch4 // 2) % 2]
                c2 = ch4 % 2
                P.tt(t12[:], psC[:, :], TW2[:, 0, :], ALU.mult)
                P.tt(t34[:], psC[:, :], TW2[:, 1, :], ALU.mult)
                P.tt(dt_.sub((0, c2), (S, 0, slice(c2 * 256, (c2 + 1) * 256))), t12[:, 0:256], t34[:, 256:512], ALU.subtract)
                P.tt(dt_.sub((1, c2), (S, 1, slice(c2 * 256, (c2 + 1) * 256))), t34[:, 0:256], t12[:, 256:512], ALU.add,
                     eng="pool")
                if c2 == 1:
                    psY = banks[7]
                    da = _AllView(dt_)
                    P.mm(psY[0:64, :], F1[:, 0, :], da[:, 0, :], start=True, stop=False)
                    P.mm(psY[0:64, :], F1[:, 1, :], da[:, 1, :], start=False, stop=True)
                    for cc2 in range(2):
                        chx = ch - 1 + cc2
                        c4x = ch4 - 1 + cc2
                        gt = gate[cnt["g"] % 2]
                        tqq = tq[cnt["g"] % 2]
                        cnt["g"] += 1
                        conv_row(1 + o, chx, gt[:])
                        vsrc = vi.sub(c4x, (S, c4x, S)) if o == 0 else z.sub(chx, (S, chx, S))
                        so = o * HCH + chx
                        P.stt(tqq[:], vsrc, sk[:, so:so + 1], psY[0:64, cc2 * 256:(cc2 + 1) * 256], ALU.mult, ALU.add)
                        if o == 0:
                            P.tt(z.sub(chx, (S, chx, S)), tqq[:], gt[:], ALU.mult, eng="pool")
                        else:
                            oo = ot[cnt["o"] % 2]
                            cnt["o"] += 1
                            P.tt(oo[:], tqq[:], gt[:], ALU.mult, eng="pool")
                            P.dma(out[chx].rearrange("(a b) -> a b", b=256), oo[:], q="sp")
    if with_ctx:
        ftc = P.sb([33, 256], F32)
        Gc = P.sb([64, 256], BF16)
        wc_ = P.sb([HCH, 256], F32)
        cwc_t = P.sb([HCH, 12], F32)
        skc_t = P.sb([HCH, 2], F32)
        hfb = P.sb([HCH, 4, 256], F32)
        rc = P.sb([HCH, 3, 258], F32)
        u3 = P.sb([HCH, 3, 256], F32)
        accs = [P.sb([HCH, 256], F32) for _ in range(4)]
        zc = P.sb([HCH, 256], F32)
        P.dma(ftc[:], featsC)
        P.dma(wc_[:], winC)
        P.dma(cwc_t[:], cwc)
        P.dma(skc_t[:], skc)
        P.dma(rc[:], rawc.rearrange("w c n -> c w n"))
        mlp(ftc[:], 256, h1b, Gc)
        for od in range(4):
            P.mm(banks[2][0:HCH, 0:256], W3[:, od * HCH:(od + 1) * HCH], Gc[:])
            P.tt(hfb.sub(od, (S, od, S)), banks[2][0:HCH, 0:256], wc_[:], ALU.mult)
        for w in range(3):
            dst = u3.sub(w, (S, w, S))
            P.ts(dst, rc[:, w, 1:257], cwc_t[:, w * 4 + 1:w * 4 + 2], cwc_t[:, w * 4 + 3:w * 4 + 4], ALU.mult, ALU.add)
            P.stt(dst, rc[:, w, 0:256], cwc_t[:, w * 4:w * 4 + 1], dst, ALU.mult, ALU.add)
            P.stt(dst, rc[:, w, 2:258], cwc_t[:, w * 4 + 2:w * 4 + 3], dst, ALU.mult, ALU.add)
        for o in range(2):
            vsrc = u3.sub(0, (S, 0, S)) if o == 0 else zc[:]
            vt = u3.t[:, 0, :] if o == 0 else zc.t
            vtr = [u3.subs[0]] if o == 0 else [zc.trk]
            P.ts(accs[0][:], vsrc, skc_t[:, o:o + 1], None, ALU.mult)
            for a_ in accs[1:]:
                P.memset(a_[:], 0.0)
            ai = 0
            hf_ = hfb.sub(o * 2, (S, o * 2, S))
            hb_ = hfb.sub(o * 2 + 1, (S, o * 2 + 1, S))
            for dd in range(256):
                a_ = accs[ai % 4]
                ai += 1
                P.stt(a_[:, dd:256], V(vt[:, 0:256 - dd], vtr), hf_[:, dd:dd + 1], a_[:, dd:256], ALU.mult, ALU.add)
                if dd >= 1:
                    a_ = accs[ai % 4]
                    ai += 1
                    P.stt(a_[:, 0:256 - dd], V(vt[:, dd:256], vtr), hb_[:, dd:dd + 1], a_[:, 0:256 - dd], ALU.mult, ALU.add)
            P.tt(accs[0][:], accs[0][:], accs[1][:], ALU.add)
            P.tt(accs[2][:], accs[2][:], accs[3][:], ALU.add, eng="pool")
            P.tt(accs[0][:], accs[0][:], accs[2][:], ALU.add)
            P.tt(zc[:], accs[0][:], u3.sub(1 + o, (S, 1 + o, S)), ALU.mult)
        P.dma(outc, zc[:])
    P.finish()
    return nc


def run_hyena(proj, layer, inp, with_ctx):
    cs = hyena_consts()
    featsP, win = hyena_tables()
    cwf = inp["hy_conv_w"][layer]
    cbf = inp["hy_conv_b"][layer]
    w3 = inp["hy_ffn_w3"][layer].reshape(64, 2, 2, 256)
    fb = np.stack([inp["hy_freq"][layer], inp["hy_ffn_b1"][layer], inp["hy_ffn_b2"][layer]], axis=1).astype(np.float32)
    z1 = np.zeros((1,), np.float32)
    if with_ctx:
        featsC = np.ascontiguousarray(_hy_feats(LC, np.arange(LC)).T)
        tlc = np.linspace(0.0, 1.0, LC, dtype=np.float32)
        winC_all = np.exp(-tlc[None, :] * _hy_deltas()[:, None]).astype(np.float32)
    maps = []
    for c in range(NCORE):
        chs = np.arange(HCH * c, HCH * (c + 1))
        rows = np.stack([proj[w * 256 + chs] for w in range(3)])
        lat = rows[:, :, LC:]
        pad = np.concatenate([np.zeros((3, HCH, 1), np.float32), lat, np.zeros((3, HCH, 257), np.float32)], axis=2)
        idx = (np.arange(64) * 256)[:, None] + np.arange(258)[None, :]
        raw = np.ascontiguousarray(pad[:, :, idx])
        cw4 = np.stack([np.stack([cwf[0, w * 256 + chs], cwf[1, w * 256 + chs], cwf[2, w * 256 + chs],
                                  cbf[w * 256 + chs]], axis=1) for w in range(3)])
        m = {"raw": raw,
             "cwl": np.ascontiguousarray(np.broadcast_to(cw4.reshape(1, -1), (64, 3 * HCH * 4))).astype(np.float32),
             "skp": np.ascontiguousarray(np.broadcast_to(inp["hy_bias"][layer][:, chs].reshape(1, -1), (64, 2 * HCH))).astype(np.float32),
             "w1": inp["hy_ffn_w1"][layer], "w2": inp["hy_ffn_w2"][layer],
             "w3s": np.ascontiguousarray(w3[:, :, :, chs].reshape(64, 4 * HCH)), "fb": fb,
             "featsP": featsP, "win": np.ascontiguousarray(win[:, :, :, chs]),
             "d1": cs["d1"], "tw": cs["tw"], "c3": cs["c3"], "e": cs["e"], "tw2": cs["tw2"], "f1": cs["f1"]}
        if with_ctx:
            cr = rows[:, :, :LC]
            m["rawc"] = np.ascontiguousarray(np.concatenate([np.zeros((3, HCH, 1), np.float32), cr,
                                                             np.zeros((3, HCH, 1), np.float32)], axis=2))
            m["cwc"] = np.ascontiguousarray(cw4.transpose(1, 0, 2).reshape(HCH, 12).astype(np.float32))
            m["skc"] = np.ascontiguousarray(inp["hy_bias"][layer][:, chs].T.astype(np.float32))
            m["featsC"] = featsC
            m["winC"] = np.ascontiguousarray(winC_all[chs])
        maps.append(m)
    key = "hyena_ctx" if with_ctx else "hyena"
    res = _run(key, lambda: build_hyena(with_ctx), maps)
    y = np.zeros((256, TT), np.float32)
    for c in range(NCORE):
        y[HCH * c:HCH * (c + 1), LC:] = res[c]["out"]
        if with_ctx:
            y[HCH * c:HCH * (c + 1), :LC] = res[c]["outc"]
    return y


def kernel(**inp):
    inp = {k: np.asarray(v) for k, v in inp.items()}
    mod = run_mod(inp)
    xT_lat = np.ascontiguousarray(inp["x"][0].T)
    xT_ctx = np.ascontiguousarray(inp["ctx"][0].T)
    for layer in range(2):
        proj = run_inproj(xT_ctx, xT_lat, mod, layer, inp["w_in"][layer])
        hy = run_hyena(proj, layer, inp, with_ctx=(layer == 0))
        yf, yb = run_ssd(proj, layer, inp)
        at = run_attn(proj, layer, inp)
        mix_rows = np.ascontiguousarray(np.concatenate([hy, yf, yb, proj[768:1280], at], axis=0))
        x1T = run_post1(mix_rows, xT_ctx, xT_lat, mod, layer, inp)
        x2T = run_ffn(x1T, mod, layer, inp)
        xT_ctx = np.ascontiguousarray(x2T[:, :LC])
        xT_lat = np.ascontiguousarray(x2T[:, LC:])
    return np.ascontiguousarray(xT_lat.T)[None].astype(np.float32)
```

```python
import numpy as np
from contextlib import ExitStack
import concourse.bass as bass
import concourse.mybir as mybir
from concourse.bass_utils import run_bass_kernel_spmd

F32 = mybir.dt.float32
BF16 = mybir.dt.bfloat16
AF = mybir.ActivationFunctionType
ALU = mybir.AluOpType
AX = mybir.AxisListType

ENGS = ("pe", "dve", "act", "pool", "sp")
NDS = 8


class Trk:
    __slots__ = ("w", "rs")

    def __init__(self):
        self.w = None
        self.rs = {}


class V:
    __slots__ = ("ap", "trks", "bank")

    def __init__(self, ap, trks, bank=None):
        self.ap = ap
        self.trks = trks
        self.bank = bank

    def __getitem__(self, idx):
        return V(self.ap[idx], self.trks, self.bank)


class Buf:
    def __init__(self, t, psum=False):
        self.t = t
        self.trk = Trk()
        self.subs = {}
        self.bank = {} if psum else None

    def __getitem__(self, idx):
        return V(self.t[idx], [self.trk], self.bank)

    def sub(self, key, idx):
        if key not in self.subs:
            self.subs[key] = Trk()
        return V(self.t[idx], [self.subs[key]], self.bank)

    def all(self, idx):
        return V(self.t[idx], [self.trk] + list(self.subs.values()), self.bank)


class Prog:
    def __init__(self, nc, self_sync=True):
        self.nc = nc
        self.es = ExitStack()
        self.ops = {e: [] for e in ENGS}
        self.cnt = {e: 0 for e in ENGS}
        self.seen = {e: {} for e in ENGS}
        self.sems = {}
        self.self_sync = self_sync
        for e in ENGS:
            self.sems[e] = self.es.enter_context(nc.semaphore("s_" + e))
        self.dcnt = {"sp": 0, "pool": 0, "act": 0}
        for q in ("sp", "pool", "act"):
            for i in range(NDS):
                self.sems[(q, i)] = self.es.enter_context(nc.semaphore("d_%s%d" % (q, i)))
        self.nbuf = 0
        self.dma_tokens = []

    def sb(self, shape, dt, name=None):
        self.nbuf += 1
        t = self.es.enter_context(self.nc.sbuf_tensor(name or "sb%d" % self.nbuf, list(shape), dt))
        return Buf(t)

    def ps(self, shape, dt, name=None):
        self.nbuf += 1
        full = [128, 2048 // mybir.dt.size(dt)]
        assert shape[0] <= 128 and int(np.prod(shape[1:])) <= full[1]
        t = self.es.enter_context(self.nc.psum_tensor(name or "ps%d" % self.nbuf, full, dt))
        return Buf(t, psum=True)

    def _emit(self, eng, fn, reads, writes, dma=False, pe_acc=False):
        deps = {}

        def add(tok):
            if tok is None:
                return
            k, v = tok
            if deps.get(k, 0) < v:
                deps[k] = v

        for vw in reads:
            for t in vw.trks:
                add(t.w)
        for vw in writes:
            for t in vw.trks:
                add(t.w)
                for k, v in t.rs.items():
                    add((k, v))
        for vw in list(reads) + list(writes):
            if vw.bank is not None:
                for f, tk in vw.bank.items():
                    if f != eng:
                        add(tk)
        waits = []
        seen = self.seen[eng]
        for k, v in deps.items():
            if k == eng:
                if eng == "pe" or not self.self_sync:
                    continue
            if seen.get(k, 0) >= v:
                continue
            seen[k] = v
            waits.append((k, v))
        if dma:
            n = self.dcnt[eng]
            self.dcnt[eng] = n + 1
            key = (eng, n % NDS)
            val = 16 * (n // NDS + 1)
            if n >= NDS and seen.get(key, 0) < val - 16:
                waits.append((key, val - 16))
                seen[key] = val - 16
            tok = (key, val)
            self.dma_tokens.append(tok)
        else:
            self.cnt[eng] += 1
            tok = (eng, self.cnt[eng])
        self.ops[eng].append((waits, fn, tok, dma))
        for vw in list(reads) + list(writes):
            if vw.bank is not None:
                vw.bank[eng] = tok
        for vw in reads:
            for t in vw.trks:
                if t.rs.get(tok[0], 0) < tok[1]:
                    t.rs[tok[0]] = tok[1]
        for vw in writes:
            for t in vw.trks:
                t.w = tok
                t.rs = {}
        return tok

    def dma(self, out, in_, q="sp", **kw):
        reads = [in_] if isinstance(in_, V) else []
        writes = [out] if isinstance(out, V) else []
        o = out.ap if isinstance(out, V) else out
        i = in_.ap if isinstance(in_, V) else in_
        return self._emit(q, lambda e: e.dma_start(out=o, in_=i, **kw), reads, writes, dma=True)

    def mm(self, out, lhsT, rhs, start=True, stop=True, **kw):
        return self._emit("pe", lambda e: e.matmul(out.ap, lhsT.ap, rhs.ap, start=start, stop=stop, **kw),
                          [lhsT, rhs], [out])

    def tr(self, out, in_, ident):
        return self._emit("pe", lambda e: e.transpose(out.ap, in_.ap, ident.ap), [in_, ident], [out])

    def act(self, out, in_, func, bias=None, scale=None, accum=None, extra_reads=()):
        kw = {}
        reads = [in_] + list(extra_reads)
        writes = [out]
        if bias is not None:
            if isinstance(bias, V):
                kw["bias"] = bias.ap
                reads.append(bias)
            else:
                kw["bias"] = bias
        if scale is not None:
            if isinstance(scale, V):
                kw["scale"] = scale.ap
                reads.append(scale)
            else:
                kw["scale"] = scale
        if accum is not None:
            kw["accum_out"] = accum.ap
            writes.append(accum)
        return self._emit("act", lambda e: e.activation(out.ap, in_.ap, func, **kw), reads, writes)

    def tt(self, out, a, b, op, eng="dve"):
        return self._emit(eng, lambda e: e.tensor_tensor(out.ap, a.ap, b.ap, op), [a, b], [out])

    def ts(self, out, a, s1, s2, op0, op1=None, eng="dve", accum=None):
        reads = [a]
        writes = [out]
        x1 = s1
        x2 = s2
        if isinstance(s1, V):
            reads.append(s1)
            x1 = s1.ap
        if isinstance(s2, V):
            reads.append(s2)
            x2 = s2.ap
        kw = {}
        if op1 is not None:
            kw["op1"] = op1
        if accum is not None:
            kw["accum_out"] = accum.ap
            writes.append(accum)
        return self._emit(eng, lambda e: e.tensor_scalar(out.ap, a.ap, x1, x2, op0, **kw), reads, writes)

    def stt(self, out, a, s, b, op0, op1, accum=None):
        reads = [a, b]
        writes = [out]
        x = s
        if isinstance(s, V):
            reads.append(s)
            x = s.ap
        kw = {}
        if accum is not None:
            kw["accum_out"] = accum.ap
            writes.append(accum)
        return self._emit("dve", lambda e: e.scalar_tensor_tensor(out.ap, a.ap, x, b.ap, op0, op1, **kw),
                          reads, writes)

    def copy(self, out, in_, eng="dve"):
        if eng == "act":
            return self._emit("act", lambda e: e.copy(out.ap, in_.ap), [in_], [out])
        return self._emit(eng, lambda e: e.tensor_copy(out.ap, in_.ap), [in_], [out])

    def memset(self, out, val, eng="dve"):
        return self._emit(eng, lambda e: e.memset(out.ap, val), [], [out])

    def gen(self, eng, fn, reads, writes):
        return self._emit(eng, fn, reads, writes)

    def finish(self):
        nc = self.nc
        last = {}
        for k, v in self.dma_tokens:
            if last.get(k, 0) < v:
                last[k] = v
        for e in ENGS:
            if self.cnt[e] > 0:
                last[e] = self.cnt[e]
        final_waits = [(k, v) for k, v in last.items()]
        engmap = {"pe": "tensor", "dve": "vector", "act": "scalar", "pool": "gpsimd", "sp": "sync"}
        with nc.Block() as block:
            for e in ENGS:
                ops = self.ops[e]
                extra = final_waits if e == "sp" else []
                if not ops and not extra:
                    continue

                def body(eng, ops=ops, e=e, extra=extra):
                    for waits, fn, tok, dma in ops:
                        for k, v in waits:
                            eng.wait_ge(self.sems[k], v)
                        ins = fn(eng)
                        if dma:
                            ins.then_inc(self.sems[tok[0]], 16)
                        else:
                            ins.then_inc(self.sems[e], 1)
                    for k, v in extra:
                        eng.wait_ge(self.sems[k], v)

                getattr(block, engmap[e])(body)
        self.es.close()


L = 16384
LC = 256
TT = L + LC
NCORE = 8
NCX = LC // NCORE
NLT = L // NCORE
NTK = NCX + NLT
DM = 1024
D_IN = 2832
HYC = 768
SSDC = 1552
EPS = 1e-6
ALPHA = 2.0 ** 0.5
_PROGS = {}


def _dram(nc, name, shape, dt, out=False):
    return nc.dram_tensor(name, list(shape), dt, kind="ExternalOutput" if out else "ExternalInput").ap()


def _run(key, builder, in_maps):
    if key not in _PROGS:
        _PROGS[key] = builder()
    nc = _PROGS[key]
    res = run_bass_kernel_spmd(nc, in_maps, core_ids=list(range(NCORE)))
    return res.results


def _fm(v):
    v = np.asarray(v, np.float32).reshape(-1, 128)
    return np.ascontiguousarray(v.T)


def build_mod():
    nc = bass.Bass("TRN2", target_bir_lowering=False)
    c8 = _dram(nc, "c8", [128, 16], F32)
    wm = _dram(nc, "wm", [2, 1024, 768], F32)
    bm = _dram(nc, "bm", [2, 1536], F32)
    out = _dram(nc, "out", [2, 1536], F32, out=True)
    P = Prog(nc)
    cs = P.sb([128, 16], F32)
    s = P.sb([128, 16], F32)
    S = P.sb([128, 8, 2], F32)
    W = P.sb([128, 2, 8, 768], F32)
    bt = P.sb([2, 1536], F32)
    ot = P.sb([2, 1536], F32)
    P.dma(cs[:], c8)
    P.dma(bt[:], bm)
    for l in range(2):
        P.dma(W.sub(l, (slice(None), l)), wm[l].rearrange("(k p) n -> p k n", p=128), q="sp" if l == 0 else "act")
    P.act(s[:], cs[:], AF.Silu)
    P.copy(S[:, :, 0], s[:, 0:8])
    P.copy(S[:, :, 1], s[:, 8:16])
    pss = [P.ps([2, 512], F32) for _ in range(2)]
    i = 0
    for l in range(2):
        for (n0, n1) in ((0, 512), (512, 768)):
            ps = pss[i % 2]
            i += 1
            for k in range(8):
                P.mm(ps[0:2, 0:n1 - n0], S[:, k, :], W.sub(l, (slice(None), l, k, slice(n0, n1))),
                     start=(k == 0), stop=(k == 7))
            P.tt(ot[:, l * 768 + n0:l * 768 + n1], ps[0:2, 0:n1 - n0], bt[:, l * 768 + n0:l * 768 + n1], ALU.add)
    P.dma(out, ot[:])
    P.finish()
    return nc


def run_mod(inp):
    c8 = np.concatenate([_fm(inp["c"][0]), _fm(inp["c_ctx"])], axis=1)
    maps = []
    for c in range(NCORE):
        wm = np.ascontiguousarray(inp["w_mod"][:, :, c * 768:(c + 1) * 768])
        b = inp["b_mod"][:, c * 768:(c + 1) * 768].reshape(1, 1536)
        maps.append({"c8": c8, "wm": wm, "bm": np.ascontiguousarray(np.repeat(b, 2, axis=0))})
    res = _run("mod", build_mod, maps)
    full = np.concatenate([r["out"].reshape(2, 2, 768) for r in res], axis=2)
    return full


def build_inproj():
    nc = bass.Bass("TRN2", target_bir_lowering=False)
    xT = _dram(nc, "xT", [1024, NTK], F32)
    md = _dram(nc, "md", [128, 32], F32)
    w = _dram(nc, "w", [1024, D_IN], F32)
    out = _dram(nc, "out", [D_IN, NTK], F32, out=True)
    P = Prog(nc)
    x = P.sb([128, 8, NTK], F32)
    h = P.sb([128, 8, NTK], BF16)
    W = P.sb([128, 8, D_IN], BF16)
    m = P.sb([128, 32], F32)
    m1 = P.sb([128, 32], F32)
    P.dma(m[:], md)
    for k in range(8):
        P.dma(x.sub(k, (slice(None), k)), xT[k * 128:(k + 1) * 128, :], q="sp" if k % 2 == 0 else "act")
    for k in range(8):
        for (c0, c1) in ((0, 1416), (1416, 2832)):
            P.dma(W.sub(k, (slice(None), k, slice(c0, c1))), w[k * 128:(k + 1) * 128, c0:c1], q="pool")
    P.ts(m1[:], m[:], 1.0, None, ALU.add)
    for k in range(8):
        P.act(h.sub(k, (slice(None), k, slice(0, NCX))), x.sub(k, (slice(None), k, slice(0, NCX))), AF.Identity,
              scale=m1[:, k:k + 1], bias=m[:, 8 + k:9 + k])
        P.act(h.sub(k, (slice(None), k, slice(NCX, NTK))), x.sub(k, (slice(None), k, slice(NCX, NTK))), AF.Identity,
              scale=m1[:, 16 + k:17 + k], bias=m[:, 24 + k:25 + k])
    nblk = [(0, 512), (512, 1024), (1024, 1536), (1536, 2048), (2048, NTK)]
    pss = [P.ps([128, 512], F32) for _ in range(4)]
    obs = [P.sb([128, NTK], F32) for _ in range(2)]
    i = 0
    for mi in range(23):
        r0 = mi * 128
        M = min(128, D_IN - r0)
        ob = obs[mi % 2]
        for (n0, n1) in nblk:
            ps = pss[i % 4]
            for k in range(8):
                P.mm(ps[0:M, 0:n1 - n0], W.sub(k, (slice(None), k, slice(r0, r0 + M))),
                     h.sub(k, (slice(None), k, slice(n0, n1))), start=(k == 0), stop=(k == 7))
            if i % 2 == 0:
                P.copy(ob[0:M, n0:n1], ps[0:M, 0:n1 - n0], eng="dve")
            else:
                P.copy(ob[0:M, n0:n1], ps[0:M, 0:n1 - n0], eng="act")
            i += 1
        P.dma(out[r0:r0 + M, :], ob[0:M, :], q="sp")
    P.finish()
    return nc


def tok_shard(a_ctx, a_lat):
    return [np.ascontiguousarray(np.concatenate([a_ctx[:, c * NCX:(c + 1) * NCX], a_lat[:, c * NLT:(c + 1) * NLT]],
                                                axis=1)) for c in range(NCORE)]


def tok_unshard(parts):
    ctx = np.concatenate([p[:, :NCX] for p in parts], axis=1)
    lat = np.concatenate([p[:, NCX:] for p in parts], axis=1)
    return np.concatenate([ctx, lat], axis=1)


def run_inproj(xT_ctx, xT_lat, mod, layer, w_in):
    ml = mod[0, layer].reshape(6, 1024)
    mc = mod[1, layer].reshape(6, 1024)
    md = np.concatenate([_fm(mc[1]), _fm(mc[0]), _fm(ml[1]), _fm(ml[0])], axis=1)
    xs = tok_shard(xT_ctx, xT_lat)
    maps = [{"xT": xs[c], "md": md, "w": w_in} for c in range(NCORE)]
    res = _run("inproj", build_inproj, maps)
    return tok_unshard([r["out"] for r in res])


def _ln_block(P, r, n, lng, lnb, out, ones, ps1, ps2, rc, sq, rstd):
    for k in range(8):
        P.mm(ps1[:, 0:n], ones[:], r[:, k, 0:n], start=(k == 0), stop=(k == 7))
    for k in range(8):
        P.stt(rc.sub(k, (slice(None), k, slice(0, n))), ps1[:, 0:n], -1.0 / DM, r[:, k, 0:n], ALU.mult, ALU.add)
        if k % 2 == 0:
            P.act(sq.sub(k, (slice(None), k, slice(0, n))), rc.sub(k, (slice(None), k, slice(0, n))), AF.Square)
        else:
            P.tt(sq.sub(k, (slice(None), k, slice(0, n))), rc.sub(k, (slice(None), k, slice(0, n))),
                 rc.sub(k, (slice(None), k, slice(0, n))), ALU.mult, eng="pool")
    for k in range(8):
        P.mm(ps2[:, 0:n], ones[:], sq.sub(k, (slice(None), k, slice(0, n))), start=(k == 0), stop=(k == 7))
    P.act(rstd[:, 0:n], ps2[:, 0:n], AF.Sqrt, bias=EPS, scale=1.0 / DM)
    P.gen("dve", lambda e: e.reciprocal(rstd.t[:, 0:n], rstd.t[:, 0:n]), [rstd[:, 0:n]], [rstd[:, 0:n]])
    for k in range(8):
        P.tt(rc.sub(k, (slice(None), k, slice(0, n))), rc.sub(k, (slice(None), k, slice(0, n))), rstd[:, 0:n], ALU.mult,
             eng="dve" if k % 2 == 0 else "pool")
        P.ts(out.sub(k, (slice(None), k, slice(0, n))), rc.sub(k, (slice(None), k, slice(0, n))),
             lng[:, k:k + 1], lnb[:, k:k + 1], ALU.mult, ALU.add, eng="dve" if k % 2 == 1 else "pool")


PB = 256
POST_BLKS = [(0, NCX)] + [(NCX + PB * b, NCX + PB * (b + 1)) for b in range(NLT // PB)]


def build_post1():
    nc = bass.Bass("TRN2", target_bir_lowering=False)
    mixin = _dram(nc, "mixin", [2048, NTK], F32)
    xT = _dram(nc, "xT", [1024, NTK], F32)
    wo = _dram(nc, "wo", [1024, 1024], F32)
    pr = _dram(nc, "pr", [128, 36], F32)
    out = _dram(nc, "out", [1024, NTK], F32, out=True)
    P = Prog(nc)
    W = P.sb([128, 8, 1024], BF16)
    prm = P.sb([128, 36], F32)
    ones = P.sb([128, 128], F32)
    P.dma(prm[:], pr)
    for k in range(8):
        P.dma(W.sub(k, (slice(None), k)), wo[k * 128:(k + 1) * 128, :], q="pool")
    P.memset(ones[:], 1.0)
    ins = [P.sb([128, 16, PB], F32) for _ in range(2)]
    xs = [P.sb([128, 8, PB], F32) for _ in range(2)]
    g = P.sb([128, 4, PB], F32)
    sz = P.sb([128, 4, PB], F32)
    gsq = P.sb([128, 4, PB], F32)
    rstdg = P.sb([128, 2, PB], F32)
    mix = P.sb([128, 8, PB], BF16)
    r = P.sb([128, 8, PB], F32)
    rc = P.sb([128, 8, PB], F32)
    sq = P.sb([128, 8, PB], F32)
    rstd = P.sb([128, PB], F32)
    ob = [P.sb([128, 8, PB], F32) for _ in range(2)]
    psg = [P.ps([128, 512], F32) for _ in range(2)]
    psm = [P.ps([128, 512], F32) for _ in range(3)]
    ps1 = P.ps([128, 512], F32)
    ps2 = P.ps([128, 512], F32)
    mi = 0
    for bi, (n0, n1) in enumerate(POST_BLKS):
        n = n1 - n0
        it = ins[bi % 2]
        xt = xs[bi % 2]
        o = ob[bi % 2]
        for k in range(16):
            P.dma(it.sub(k, (slice(None), k, slice(0, n))), mixin[k * 128:(k + 1) * 128, n0:n1],
                  q="sp" if k % 2 == 0 else "act")
        for k in range(8):
            P.dma(xt.sub(k, (slice(None), k, slice(0, n))), xT[k * 128:(k + 1) * 128, n0:n1],
                  q="sp" if k % 2 == 0 else "act")
        sl = slice(0, n)
        S = slice(None)
        for (kd, ks) in ((0, 0), (1, 1), (6, 14), (7, 15)):
            P.copy(mix.sub(kd, (S, kd, sl)), it.sub(ks, (S, ks, sl)), eng="pool")
        for c in range(4):
            P.tt(g.sub(c, (S, c, sl)), it.sub(2 + c, (S, 2 + c, sl)), it.sub(6 + c, (S, 6 + c, sl)), ALU.add)
            P.act(sz.sub(c, (S, c, sl)), it.sub(10 + c, (S, 10 + c, sl)), AF.Silu)
            P.tt(g.sub(c, (S, c, sl)), g.sub(c, (S, c, sl)), sz.sub(c, (S, c, sl)), ALU.mult)
            P.tt(gsq.sub(c, (S, c, sl)), g.sub(c, (S, c, sl)), g.sub(c, (S, c, sl)), ALU.mult, eng="pool")
        for gi in range(2):
            for j in range(2):
                c = gi * 2 + j
                P.mm(psg[gi][:, sl], ones[:], gsq.sub(c, (S, c, sl)), start=(j == 0), stop=(j == 1))
            P.act(rstdg.sub(gi, (S, gi, sl)), psg[gi][:, sl], AF.Sqrt, bias=EPS, scale=1.0 / 256)
            P.gen("dve", lambda e, gi=gi, sl=sl: e.reciprocal(rstdg.t[:, gi, sl], rstdg.t[:, gi, sl]),
                  [rstdg.sub(gi, (S, gi, sl))], [rstdg.sub(gi, (S, gi, sl))])
            for j in range(2):
                c = gi * 2 + j
                P.stt(mix.sub(2 + c, (S, 2 + c, sl)), g.sub(c, (S, c, sl)), prm[:, c:c + 1],
                      rstdg.sub(gi, (S, gi, sl)), ALU.mult, ALU.mult)
        g1o = 4 if bi == 0 else 12
        for j in range(8):
            ps = psm[mi % 3]
            mi += 1
            for k in range(8):
                P.mm(ps[:, sl], W.sub(k, (S, k, slice(j * 128, (j + 1) * 128))), mix.sub(k, (S, k, sl)),
                     start=(k == 0), stop=(k == 7))
            P.act(r.sub(j, (S, j, sl)), ps[:, sl], AF.Identity, scale=prm[:, g1o + j:g1o + j + 1])
            P.stt(r.sub(j, (S, j, sl)), xt.sub(j, (S, j, sl)), ALPHA, r.sub(j, (S, j, sl)), ALU.mult, ALU.add)
        _ln_block(P, _AllView(r), n, _Cols(prm, 20), _Cols(prm, 28), o, ones, ps1, ps2, rc, sq, rstd)
        for k in range(8):
            P.dma(out[k * 128:(k + 1) * 128, n0:n1], o.sub(k, (S, k, sl)), q="sp" if k % 2 == 0 else "act")
    P.finish()
    return nc


class _AllView:
    def __init__(self, b):
        self.b = b

    def __getitem__(self, idx):
        return self.b.all(idx)


class _Cols:
    def __init__(self, b, off):
        self.b = b
        self.off = off

    def __getitem__(self, idx):
        p, c = idx
        return self.b[p, slice(c.start + self.off, c.stop + self.off)]


def run_post1(mix_rows, xT_ctx, xT_lat, mod, layer, inp):
    ml = mod[0, layer].reshape(6, 1024)
    mc = mod[1, layer].reshape(6, 1024)
    pr = np.concatenate([_fm(inp["ssd_norm_w"][layer]), _fm(mc[2]), _fm(ml[2]), _fm(inp["ln1_g"][layer]),
                         _fm(inp["ln1_b"][layer])], axis=1)
    ms = tok_shard(mix_rows[:, :LC], mix_rows[:, LC:])
    xs = tok_shard(xT_ctx, xT_lat)
    maps = [{"mixin": ms[c], "xT": xs[c], "wo": inp["w_out"][layer], "pr": pr} for c in range(NCORE)]
    res = _run("post1", build_post1, maps)
    return tok_unshard([r["out"] for r in res])


FB = 256
NFB = NLT // FB
FFN_W = NCX + 2 + NLT + 2
FFN_BLKS = [(0, NCX, 0)] + [(NCX + 2 + FB * b, FB, NCX + FB * b) for b in range(NFB)]
DFF = 2816


def build_ffn():
    nc = bass.Bass("TRN2", target_bir_lowering=False)
    x1 = _dram(nc, "x1", [1024, FFN_W], F32)
    mk = _dram(nc, "mk", [128, 2 * (NFB + 1)], F32)
    pr = _dram(nc, "pr", [128, 64], F32)
    cw = _dram(nc, "cw", [128, 132], F32)
    cb = _dram(nc, "cb", [128, 44], F32)
    wu = _dram(nc, "wu", [1024, 2 * DFF], F32)
    wd = _dram(nc, "wd", [DFF, 1024], F32)
    out = _dram(nc, "out", [1024, NTK], F32, out=True)
    P = Prog(nc)
    S = slice(None)
    prm = P.sb([128, 64], F32)
    prm1 = P.sb([128, 64], F32)
    mkt = P.sb([128, 2 * (NFB + 1)], F32)
    cwt = P.sb([128, 132], F32)
    cbt = P.sb([128, 44], F32)
    ones = P.sb([128, 128], F32)
    WU = P.sb([128, 8, 2 * DFF], BF16)
    WD = P.sb([128, 22, 1024], BF16)
    P.dma(prm[:], pr)
    P.dma(mkt[:], mk)
    P.dma(cwt[:], cw)
    P.dma(cbt[:], cb)
    P.memset(ones[:], 1.0)
    P.ts(prm1[:], prm[:], 1.0, None, ALU.add)
    for jj in range(4):
        for k in range(8):
            c0 = jj * 1408
            P.dma(WU.sub((k, jj), (S, k, slice(c0, c0 + 1408))), wu[k * 128:(k + 1) * 128, c0:c0 + 1408], q="pool")
    for j in range(22):
        P.dma(WD.sub(j, (S, j)), wd[j * 128:(j + 1) * 128, :], q="pool")

    def wu_view(k, c0):
        jj = c0 // 1408
        jj2 = (c0 + 127) // 1408
        trks = [WU.subs[(k, jj)]] + ([WU.subs[(k, jj2)]] if jj2 != jj else [])
        return V(WU.t[:, k, c0:c0 + 128], trks)

    WD_ = FB + 2
    xb = [P.sb([128, 8, WD_], F32) for _ in range(2)]
    h2 = P.sb([128, 8, WD_], BF16)
    u = [P.sb([128, WD_], F32) for _ in range(4)]
    cc = [P.sb([128, FB], F32) for _ in range(4)]
    sg = [P.sb([128, FB], F32) for _ in range(2)]
    a = P.sb([128, 22, FB], BF16)
    r = P.sb([128, 8, FB], F32)
    sq = [P.sb([128, 8, FB], F32) for _ in range(2)]
    rstd = P.sb([128, FB], F32)
    psu = [P.ps([128, 512], F32) for _ in range(4)]
    psd = [P.ps([128, 512], F32) for _ in range(2)]
    ps1 = P.ps([128, 512], F32)
    ps2 = P.ps([128, 512], F32)
    ui = 0
    di = 0
    for bi, (c0, n, o0) in enumerate(FFN_BLKS):
        wdt = n + 2
        xt = xb[bi % 2]
        po = 0 if bi == 0 else 24
        for k in range(8):
            P.dma(xt.sub(k, (S, k, slice(0, wdt))), x1[k * 128:(k + 1) * 128, c0:c0 + wdt],
                  q="sp" if k % 2 == 0 else "act")
        for k in range(8):
            P.act(h2.sub(k, (S, k, slice(0, wdt))), xt.sub(k, (S, k, slice(0, wdt))), AF.Identity,
                  scale=prm1[:, po + k:po + k + 1], bias=prm[:, po + 8 + k:po + 9 + k])
        for j in range(22):
            cs = []
            for wi in range(2):
                ch = wi * 22 + j
                col0 = ch * 128
                ps = psu[ui % 4]
                ub = u[ui % 4]
                cb_ = cc[ui % 4]
                ui += 1
                for k in range(8):
                    P.mm(ps[:, 0:wdt], wu_view(k, col0), h2.sub(k, (S, k, slice(0, wdt))), start=(k == 0), stop=(k == 7))
                P.copy(ub[:, 0:wdt], ps[:, 0:wdt], eng="act")
                P.ts(ub[:, 0:1], ub[:, 0:1], mkt[:, 2 * bi:2 * bi + 1], None, ALU.mult)
                P.ts(ub[:, wdt - 1:wdt], ub[:, wdt - 1:wdt], mkt[:, 2 * bi + 1:2 * bi + 2], None, ALU.mult)
                P.ts(cb_[:, 0:n], ub[:, 1:n + 1], cwt[:, ch * 3 + 1:ch * 3 + 2], cbt[:, ch:ch + 1], ALU.mult, ALU.add)
                P.stt(cb_[:, 0:n], ub[:, 0:n], cwt[:, ch * 3:ch * 3 + 1], cb_[:, 0:n], ALU.mult, ALU.add)
                P.stt(cb_[:, 0:n], ub[:, 2:n + 2], cwt[:, ch * 3 + 2:ch * 3 + 3], cb_[:, 0:n], ALU.mult, ALU.add)
                cs.append(cb_)
            sgt = sg[j % 2]
            P.act(sgt[:, 0:n], cs[0][:, 0:n], AF.Silu)
            P.tt(a.sub(j, (S, j, slice(0, n))), sgt[:, 0:n], cs[1][:, 0:n], ALU.mult, eng="pool")
        for i in range(8):
            ps = psd[di % 2]
            di += 1
            for j in range(22):
                P.mm(ps[:, 0:n], WD.sub(j, (S, j, slice(i * 128, (i + 1) * 128))), a.sub(j, (S, j, slice(0, n))),
                     start=(j == 0), stop=(j == 21))
            P.act(r.sub(i, (S, i, slice(0, n))), ps[:, 0:n], AF.Identity, scale=prm[:, po + 16 + i:po + 17 + i])
            P.stt(r.sub(i, (S, i, slice(0, n))), xt.sub(i, (S, i, slice(1, n + 1))), ALPHA, r.sub(i, (S, i, slice(0, n))),
                  ALU.mult, ALU.add)
        o = sq[bi % 2]
        _ln_block(P, _AllView(r), n, _Cols(prm, 48), _Cols(prm, 56), o, ones, ps1, ps2, r, o, rstd)
        for k in range(8):
            P.dma(out[k * 128:(k + 1) * 128, o0:o0 + n], o.sub(k, (S, k, slice(0, n))), q="sp" if k % 2 == 0 else "act")
    P.finish()
    return nc


def run_ffn(x1T, mod, layer, inp):
    ml = mod[0, layer].reshape(6, 1024)
    mc = mod[1, layer].reshape(6, 1024)
    pr = np.concatenate([_fm(mc[4]), _fm(mc[3]), _fm(mc[5]), _fm(ml[4]), _fm(ml[3]), _fm(ml[5]),
                         _fm(inp["ln2_g"][layer]), _fm(inp["ln2_b"][layer])], axis=1)
    cw = np.ascontiguousarray(inp["ffn_conv_w"][layer].T.reshape(44, 128, 3).transpose(1, 0, 2).reshape(128, 132))
    cb = _fm(inp["ffn_conv_b"][layer])
    z = np.zeros((1024, 1), np.float32)
    xc = np.concatenate([z, x1T[:, :LC], z], axis=1)
    xl = np.concatenate([z, x1T[:, LC:], z], axis=1)
    maps = []
    for c in range(NCORE):
        xin = np.ascontiguousarray(np.concatenate([xc[:, c * NCX:c * NCX + NCX + 2], xl[:, c * NLT:c * NLT + NLT + 2]], axis=1))
        mk = np.ones((128, 2 * (NFB + 1)), np.float32)
        if c == 0:
            mk[:, 0] = 0.0
            mk[:, 2] = 0.0
        if c == NCORE - 1:
            mk[:, 1] = 0.0
            mk[:, 2 * NFB + 1] = 0.0
        maps.append({"x1": xin, "mk": mk, "pr": pr, "cw": cw, "cb": cb, "wu": inp["ffn_w_up"][layer],
                     "wd": inp["ffn_w_down"][layer]})
    res = _run("ffn", build_ffn, maps)
    return tok_unshard([r["out"] for r in res])


QH = L // 2
QC = LC // 2
NKT = TT // 128


def build_attn():
    nc = bass.Bass("TRN2", target_bir_lowering=False)
    qT = _dram(nc, "qT", [64, QC + QH], F32)
    kT = _dram(nc, "kT", [64, TT], F32)
    v = _dram(nc, "v", [TT, 64], F32)
    cosk = _dram(nc, "cosk", [64, L], F32)
    sink = _dram(nc, "sink", [64, L], F32)
    cosq = _dram(nc, "cosq", [64, QH], F32)
    sinq = _dram(nc, "sinq", [64, QH], F32)
    cst = _dram(nc, "cst", [64, 66], F32)
    out = _dram(nc, "out", [64, QC + QH], F32, out=True)
    P = Prog(nc)
    S = slice(None)
    ct = P.sb([64, 66], F32)
    ones = P.sb([128, 64], F32)
    KT = P.sb([64, TT], BF16)
    QT = P.sb([64, QC + QH], BF16)
    VA = P.sb([128, NKT, 65], BF16)
    P.dma(ct[:], cst)
    P.memset(ones[:], 1.0)
    vv = v.rearrange("(t p) d -> p t d", p=128)
    for i in range(5):
        P.dma(VA.sub(i, (S, slice(i * 26, (i + 1) * 26), slice(0, 64))), vv[:, i * 26:(i + 1) * 26, :], q="pool")
    P.memset(VA.sub("one", (S, S, slice(64, 65))), 1.0)
    xin = [P.sb([64, 512], F32) for _ in range(2)]
    cin = [P.sb([64, 512], F32) for _ in range(2)]
    sin_ = [P.sb([64, 512], F32) for _ in range(2)]
    sq = P.sb([64, 512], F32)
    rstd = P.sb([64, 512], F32)
    xn = P.sb([64, 512], F32)
    t1 = P.sb([64, 512], F32)
    t2 = P.sb([64, 512], F32)
    pss = P.ps([64, 512], F32)
    psr = P.ps([64, 512], F32)
    cnt = [0]

    def prep(src, c0, n, gcol, tabs, dst, d0):
        i = cnt[0] % 2
        cnt[0] += 1
        x = xin[i]
        P.dma(x[:, 0:n], src[:, c0:c0 + n], q="sp")
        if tabs is not None:
            P.dma(cin[i][:, 0:n], tabs[0][:, tabs[2]:tabs[2] + n], q="act")
            P.dma(sin_[i][:, 0:n], tabs[1][:, tabs[2]:tabs[2] + n], q="act")
        P.act(sq[:, 0:n], x[:, 0:n], AF.Square)
        P.mm(pss[0:64, 0:n], ones[0:64, :], sq[:, 0:n])
        P.act(rstd[:, 0:n], pss[0:64, 0:n], AF.Sqrt, bias=EPS, scale=1.0 / 64)
        P.gen("dve", lambda e: e.reciprocal(rstd.t[:, 0:n], rstd.t[:, 0:n]), [rstd[:, 0:n]], [rstd[:, 0:n]])
        if tabs is None:
            P.stt(dst[:, d0:d0 + n], x[:, 0:n], ct[:, gcol:gcol + 1], rstd[:, 0:n], ALU.mult, ALU.mult)
            return
        P.stt(xn[:, 0:n], x[:, 0:n], ct[:, gcol:gcol + 1], rstd[:, 0:n], ALU.mult, ALU.mult)
        P.mm(psr[0:64, 0:n], ct[:, 0:64], xn[:, 0:n])
        P.tt(t1[:, 0:n], xn[:, 0:n], cin[i][:, 0:n], ALU.mult, eng="pool")
        P.tt(t2[:, 0:n], psr[0:64, 0:n], sin_[i][:, 0:n], ALU.mult)
        P.tt(dst[:, d0:d0 + n], t1[:, 0:n], t2[:, 0:n], ALU.add)

    prep(kT, 0, LC, 65, None, KT, 0)
    for i in range(L // 512):
        prep(kT, LC + 512 * i, 512, 65, (cosk, sink, 512 * i), KT, LC + 512 * i)
    prep(qT, 0, QC, 64, None, QT, 0)
    for i in range(QH // 512):
        prep(qT, QC + 512 * i, 512, 64, (cosq, sinq, 512 * i), QT, QC + 512 * i)

    psS = [P.ps([128, 512], F32) for _ in range(3)] + [pss]
    psO = [P.ps([128, 512], F32) for _ in range(2)]
    psB = psr
    NS = len(psS)
    LOOK = 2
    pT = [P.sb([128, 512], BF16) for _ in range(NS)]
    osb = [P.sb([65, 512], F32) for _ in range(2)]
    yo = [P.sb([64, 512], F32) for _ in range(2)]
    blocks = [(0, QC, 0, 2)] + [(QC + 512 * i, 512, 0, NKT) for i in range(QH // 512)]
    work = []
    for bi, (q0, n, k0, k1) in enumerate(blocks):
        for kt in range(k0, k1):
            work.append((bi, q0, n, kt, kt == k0, kt == k1 - 1))

    def emit_s(w, si):
        bi, q0, n, kt, first, last = w
        P.mm(psS[si % NS][:, 0:n], KT[:, kt * 128:(kt + 1) * 128], QT[:, q0:q0 + n])

    for si in range(min(LOOK, len(work))):
        emit_s(work[si], si)
    for si, w in enumerate(work):
        bi, q0, n, kt, first, last = w
        if si + LOOK < len(work):
            emit_s(work[si + LOOK], si + LOOK)
        ps = psS[si % NS]
        pt = pT[si % NS]
        po = psO[bi % 2]
        P.act(pt[:, 0:n], ps[:, 0:n], AF.Exp, scale=0.125)
        P.mm(po[0:65, 0:n], VA.all((S, kt, slice(0, 65))), pt[:, 0:n], start=first, stop=last)
        if last:
            ob = osb[bi % 2]
            y = yo[bi % 2]
            P.copy(ob[:, 0:n], po[0:65, 0:n], eng="dve")
            P.gen("dve", lambda e, ob=ob, n=n: e.reciprocal(ob.t[64:65, 0:n], ob.t[64:65, 0:n]), [ob[64:65, 0:n]], [ob[64:65, 0:n]])
            P.mm(psB[0:64, 0:n], ones[64:65, :], ob[64:65, 0:n])
            P.tt(y[:, 0:n], ob[0:64, 0:n], psB[0:64, 0:n], ALU.mult)
            P.dma(out[:, q0:q0 + n], y[:, 0:n], q="sp")
    P.finish()
    return nc


def rope_tables():
    rows = L // 64
    row = np.repeat(np.arange(rows, dtype=np.float32), 64)
    col = np.tile(np.arange(64, dtype=np.float32), rows)
    inv = (np.float32(10000.0) ** (-np.arange(16, dtype=np.float32) / np.float32(16))).astype(np.float32)
    ang = np.concatenate([row[:, None] * inv, col[:, None] * inv], axis=-1).astype(np.float32)
    cos = np.cos(ang).astype(np.float32).T
    sin = np.sin(ang).astype(np.float32).T
    return (np.ascontiguousarray(np.concatenate([cos, cos], axis=0)),
            np.ascontiguousarray(np.concatenate([sin, sin], axis=0)))


def run_attn(proj, layer, inp):
    cosf, sinf = rope_tables()
    RT = np.zeros((64, 64), np.float32)
    for dp in range(32):
        RT[dp + 32, dp] = -1.0
        RT[dp, dp + 32] = 1.0
    cst = np.concatenate([RT, inp["attn_q_norm"][layer][:, None], inp["attn_k_norm"][layer][:, None]], axis=1)
    cst = np.ascontiguousarray(cst.astype(np.float32))
    maps = []
    for c in range(NCORE):
        g, h, half = c // 4, c // 2, c % 2
        qrows = proj[2320 + 64 * h:2320 + 64 * (h + 1)]
        qT = np.ascontiguousarray(np.concatenate([qrows[:, half * QC:(half + 1) * QC],
                                                  qrows[:, LC + half * QH:LC + (half + 1) * QH]], axis=1))
        kT = np.ascontiguousarray(proj[2576 + 64 * g:2576 + 64 * (g + 1)])
        v = np.ascontiguousarray(proj[2704 + 64 * g:2704 + 64 * (g + 1)].T)
        maps.append({"qT": qT, "kT": kT, "v": v, "cosk": cosf, "sink": sinf,
                     "cosq": np.ascontiguousarray(cosf[:, half * QH:(half + 1) * QH]),
                     "sinq": np.ascontiguousarray(sinf[:, half * QH:(half + 1) * QH]), "cst": cst})
    res = _run("attn", build_attn, maps)
    y = np.zeros((256, TT), np.float32)
    for c in range(NCORE):
        h, half = c // 2, c % 2
        o = res[c]["out"]
        y[64 * h:64 * (h + 1), half * QC:(half + 1) * QC] = o[:, :QC]
        y[64 * h:64 * (h + 1), LC + half * QH:LC + (half + 1) * QH] = o[:, QC:]
    return y


NCH = TT // 128
SSD_W = LC + 2 + L + 2
SB = 1024
SSD_STAGE = 9
SSD_BLKS = [(0, LC, 0)] + [(LC + 2 + SB * b, SB, LC + SB * b) for b in range(L // SB)]


def build_ssd():
    nc = bass.Bass("TRN2", target_bir_lowering=False)
    xr = _dram(nc, "xr", [2, 64, SSD_W], F32)
    br = _dram(nc, "br", [2, 128, SSD_W], F32)
    cr = _dram(nc, "cr", [2, 128, SSD_W], F32)
    dtr = _dram(nc, "dtr", [2, 128, NCH], F32)
    scl = _dram(nc, "scl", [2, 128, 4], F32)
    cwx = _dram(nc, "cwx", [2, 64, 4], F32)
    cwb = _dram(nc, "cwb", [2, 128, 4], F32)
    cwc = _dram(nc, "cwc", [2, 128, 4], F32)
    cst = _dram(nc, "cst", [128, 384], F32)
    out = _dram(nc, "out", [2, TT, 64], F32, out=True)
    P = Prog(nc)
    S = slice(None)
    ct = P.sb([128, 384], F32)
    ones = P.sb([128, 128], F32)
    identb = P.sb([128, 128], BF16)
    P.dma(ct[:], cst)
    P.memset(ones[:], 1.0)
    P.copy(identb[:], ct[:, 256:384])
    tri = ct[:, 0:128]
    negm = ct[:, 128:256]
    ident = ct[:, 256:384]
    ps_big = P.ps([128, 512], F32)
    ps_trb = P.ps([128, 128], BF16)
    ps_trx = P.ps([128, 64], F32)
    ps_scs = [P.ps([128, 128], F32), ps_big]
    ps_segs = [P.ps([128, 128], F32), P.ps([128, 128], F32)]
    ps_ys = P.ps([128, 128], F32)
    ps_i = P.ps([128, 64], F32)
    negmb = P.sb([128, 128], BF16)
    P.copy(negmb[:], negm)
    W2 = SB + 2

    class Job:
        pass

    def mk_job(j):
        J = Job()
        J.j = j
        for nm in ("dtt", "e1", "dt", "dta", "acum", "nacum", "ea", "dch", "wv"):
            setattr(J, nm, P.sb([128, NCH], F32))
        J.sc4 = P.sb([128, 4], F32)
        J.at = P.sb([128, 1], F32)
        J.cx = P.sb([64, 4], F32)
        J.cb = P.sb([128, 4], F32)
        J.cc = P.sb([128, 4], F32)
        J.xraw = P.sb([64, W2], F32)
        J.braw = P.sb([128, W2], F32)
        J.craw = P.sb([128, W2], F32)
        J.tx = P.sb([64, SB], F32)
        J.tb = P.sb([128, SB], F32)
        J.tc_ = P.sb([128, SB], F32)
        J.xT = P.sb([64, SB], F32)
        J.BT = P.sb([128, SB], BF16)
        J.CT = P.sb([128, SB], BF16)
        J.Bc = P.sb([128, 128], BF16)
        J.xdt = P.sb([128, 64], BF16)
        J.xw = P.sb([128, 64], BF16)
        J.xD = P.sb([128, 64], F32)
        J.dg = P.sb([128, 128], F32)
        J.dec = P.sb([128, 128], F32)
        J.MT = P.sb([128, 128], BF16)
        J.yi = P.sb([128, 64], F32)
        J.H = P.sb([128, 64], F32)
        J.Hb = P.sb([128, 64], BF16)
        J.ybuf = [P.sb([128, SB // 128, 64], F32) for _ in range(2)]
        J.yb_i = 0
        P.dma(J.dtt[:], dtr[j])
        P.dma(J.sc4[:], scl[j])
        P.dma(J.cx[:], cwx[j])
        P.dma(J.cb[:], cwb[j])
        P.dma(J.cc[:], cwc[j])
        P.act(J.e1[:], J.dtt[:], AF.Exp, bias=J.sc4[:, 0:1])
        P.act(J.dt[:], J.e1[:], AF.Ln, bias=1.0)
        P.act(J.at[:], J.sc4[:, 1:2], AF.Exp)
        P.ts(J.at[:], J.at[:], -1.0, None, ALU.mult)
        P.ts(J.dta[:], J.dt[:], J.at[:, 0:1], None, ALU.mult)
        P.mm(ps_big[:, 0:NCH], tri, J.dta[:])
        P.copy(J.acum[:], ps_big[:, 0:NCH])
        P.ts(J.nacum[:], J.acum[:], -1.0, None, ALU.mult)
        P.act(J.ea[:], J.acum[:], AF.Exp)
        P.mm(ps_big[:, 256:256 + NCH], ones[:], J.dta[:])
        P.act(J.dch[:], ps_big[:, 256:256 + NCH], AF.Exp)
        P.tt(J.wv[:], ps_big[:, 256:256 + NCH], J.acum[:], ALU.subtract)
        P.act(J.wv[:], J.wv[:], AF.Exp)
        P.tt(J.wv[:], J.wv[:], J.dt[:], ALU.mult)
        P.memset(J.H[:], 0.0)
        P.memset(J.Hb[:], 0.0)
        return J

    def conv_block(J, c0, n):
        j = J.j
        P.dma(J.xraw[:, 0:n + 2], xr[j, :, c0:c0 + n + 2], q="sp")
        P.dma(J.braw[:, 0:n + 2], br[j, :, c0:c0 + n + 2], q="act")
        P.dma(J.craw[:, 0:n + 2], cr[j, :, c0:c0 + n + 2], q="sp")
        for (raw, tmp, w4, dst) in ((J.xraw, J.tx, J.cx, J.xT), (J.braw, J.tb, J.cb, J.BT), (J.craw, J.tc_, J.cc, J.CT)):
            P.ts(tmp[:, 0:n], raw[:, 1:n + 1], w4[:, 1:2], w4[:, 3:4], ALU.mult, ALU.add)
            P.stt(tmp[:, 0:n], raw[:, 0:n], w4[:, 0:1], tmp[:, 0:n], ALU.mult, ALU.add)
            P.stt(tmp[:, 0:n], raw[:, 2:n + 2], w4[:, 2:3], tmp[:, 0:n], ALU.mult, ALU.add)
            P.act(dst[:, 0:n], tmp[:, 0:n], AF.Silu)

    def chunk_a(J, yb, ci, c):
        cs = slice(ci * 128, (ci + 1) * 128)
        ps_sc = ps_scs[J.j]
        ps_seg = ps_segs[J.j]
        P.tr(ps_trb[:, 0:128], J.BT[:, cs], identb[:])
        P.copy(J.Bc[:], ps_trb[:, 0:128], eng="act")
        P.tr(ps_trx[:, 0:64], J.xT[:, cs], ident[0:64, 0:64])
        P.ts(J.xdt[:], ps_trx[:, 0:64], J.dt[:, c:c + 1], None, ALU.mult)
        P.ts(J.xw[:], ps_trx[:, 0:64], J.wv[:, c:c + 1], None, ALU.mult)
        P.ts(J.xD[:], ps_trx[:, 0:64], J.sc4[:, 2:3], None, ALU.mult)
        P.mm(ps_sc[:, 0:128], J.BT[:, cs], J.CT[:, cs])
        P.ts(J.dg[:], ident, J.acum[:, c:c + 1], 0.0, ALU.mult, ALU.add, eng="pool")
        P.mm(ps_seg[:, 0:128], ones[:], J.dg[:], start=True, stop=False)
        P.mm(ps_seg[:, 0:128], identb[:], negmb[:], start=False, stop=True)
        P.act(J.dec[:], ps_seg[:, 0:128], AF.Exp, bias=J.nacum[:, c:c + 1])
        P.tt(J.MT[:], ps_sc[:, 0:128], J.dec[:], ALU.mult)

    def chunk_b(J, yb, ci, c):
        cs = slice(ci * 128, (ci + 1) * 128)
        ps_y = ps_ys.sub("y", (S, slice(0, 64)))
        ps_s = ps_ys.sub("s", (S, slice(64, 128)))
        P.mm(ps_y, J.MT[:], J.xdt[:])
        P.mm(ps_i[:, 0:64], J.CT[:, cs], J.Hb[:])
        P.mm(ps_s, J.Bc[:], J.xw[:])
        P.act(J.yi[:], ps_y, AF.Identity)
        P.tt(J.yi[:], J.yi[:], J.xD[:], ALU.add, eng="pool")
        P.stt(yb[:, ci, :], ps_i[:, 0:64], J.ea[:, c:c + 1], J.yi[:], ALU.mult, ALU.add)
        P.stt(J.H[:], J.H[:], J.dch[:, c:c + 1], ps_s, ALU.mult, ALU.add)
        P.copy(J.Hb[:], J.H[:], eng="act")

    jobs = [mk_job(0), mk_job(1)]
    for (c0, n, t0) in SSD_BLKS:
        ybs = []
        for J in jobs:
            conv_block(J, c0, n)
            ybs.append(J.ybuf[J.yb_i % 2])
            J.yb_i += 1
        for ci in range(n // 128):
            for J, yb in zip(jobs, ybs):
                chunk_a(J, yb, ci, t0 // 128 + ci)
            for J, yb in zip(jobs, ybs):
                chunk_b(J, yb, ci, t0 // 128 + ci)
        for J, yb in zip(jobs, ybs):
            P.dma(out[J.j, t0:t0 + n, :].rearrange("(c p) d -> p c d", p=128), yb[:, 0:n // 128, :], q="act")
    P.finish()
    return nc


def _flipseq(a):
    return np.concatenate([a[..., :LC][..., ::-1], a[..., LC:][..., ::-1]], axis=-1)


def _padseq(a):
    z = np.zeros(a.shape[:-1] + (1,), np.float32)
    return np.concatenate([z, a[..., :LC], z, z, a[..., LC:], z], axis=-1)


def ssd_consts():
    s = np.arange(128)
    tri = (s[:, None] <= s[None, :]).astype(np.float32)
    negm = np.where(s[:, None] > s[None, :], -30000.0, 0.0).astype(np.float32)
    return np.ascontiguousarray(np.concatenate([tri, negm, np.eye(128, dtype=np.float32)], axis=1))


def run_ssd(proj, layer, inp):
    cst = ssd_consts()
    cw = inp["ssd_conv_w"][layer]
    cbias = inp["ssd_conv_b"][layer]
    maps = []
    for hd in range(NCORE):
        g = hd // 4
        rows = {"x": slice(1280 + 64 * hd, 1280 + 64 * (hd + 1)),
                "b": slice(1792 + 128 * g, 1792 + 128 * (g + 1)),
                "c": slice(2048 + 128 * g, 2048 + 128 * (g + 1))}
        chs = {"x": slice(64 * hd, 64 * (hd + 1)), "b": slice(512 + 128 * g, 512 + 128 * (g + 1)),
               "c": slice(768 + 128 * g, 768 + 128 * (g + 1))}
        m = {}
        for nm, key in (("xr", "x"), ("br", "b"), ("cr", "c")):
            a = proj[rows[key]]
            m[nm] = np.ascontiguousarray(np.stack([_padseq(a), _padseq(_flipseq(a))]))
        for nm, key in (("cwx", "x"), ("cwb", "b"), ("cwc", "c")):
            w = cw[:, chs[key]]
            b = cbias[chs[key]]
            f = np.stack([w[0], w[1], w[2], b], axis=1)
            bk = np.stack([w[2], w[1], w[0], b], axis=1)
            m[nm] = np.ascontiguousarray(np.stack([f, bk]).astype(np.float32))
        dts = []
        scl = []
        for dr in range(2):
            raw = proj[2304 + dr * 8 + hd]
            if dr == 1:
                raw = _flipseq(raw)
            dts.append(raw.reshape(NCH, 128).T)
            s4 = np.zeros((128, 4), np.float32)
            s4[:, 0] = inp["ssd_dt_bias"][layer][dr, hd]
            s4[:, 1] = inp["ssd_a_log"][layer][dr, hd]
            s4[:, 2] = inp["ssd_d"][layer][hd] if dr == 0 else 0.0
            scl.append(s4)
        m["dtr"] = np.ascontiguousarray(np.stack(dts).astype(np.float32))
        m["scl"] = np.ascontiguousarray(np.stack(scl))
        m["cst"] = cst
        maps.append(m)
    res = _run("ssd", build_ssd, maps)
    yf = np.concatenate([res[hd]["out"][0].T for hd in range(NCORE)], axis=0)
    yb = np.concatenate([_flipseq(res[hd]["out"][1].T) for hd in range(NCORE)], axis=0)
    return np.ascontiguousarray(yf), np.ascontiguousarray(yb)


HN = 2 * L
HCH = 32
HY_SKIP = {}
PI = float(np.pi)


def hyena_consts():
    n1 = np.arange(128)
    f1 = np.arange(128)
    n2 = np.arange(256)
    f2 = np.arange(256)
    a1 = 2 * np.pi * np.outer(n1, f1) / 128
    D1 = np.concatenate([np.cos(a1), -np.sin(a1)], axis=1)
    at = 2 * np.pi * np.outer(n2, f1) / HN
    TwC, TwS = np.cos(at), np.sin(at)
    a3 = 2 * np.pi * np.outer(n2, f2) / 256
    C3, S3 = np.cos(a3), np.sin(a3)
    E1 = np.concatenate([C3.T, S3.T], axis=1)
    E2 = np.concatenate([-S3.T, C3.T], axis=1)
    F1c = np.cos(a1.T)[:, :64] / HN
    F1s = -np.sin(a1.T)[:, :64] / HN
    c = {}
    c["d1"] = np.stack([D1[0:64], D1[64:128]])
    c["tw"] = np.stack([np.stack([np.concatenate([TwC[h * 128:(h + 1) * 128]] * 2, axis=1),
                                  np.concatenate([TwS[h * 128:(h + 1) * 128]] * 2, axis=1)]) for h in range(2)])
    c["tw"] = c["tw"].transpose(2, 0, 1, 3)
    c["c3"] = np.stack([np.stack([C3[h * 128:(h + 1) * 128], S3[h * 128:(h + 1) * 128], -S3[h * 128:(h + 1) * 128]])
                        for h in range(2)]).transpose(2, 0, 1, 3)
    c["e"] = np.stack([np.stack([E1[g * 128:(g + 1) * 128], E2[g * 128:(g + 1) * 128]]) for g in range(2)]
                      ).transpose(2, 0, 1, 3)
    c["tw2"] = np.stack([np.concatenate([TwC.T, TwC.T], axis=1), np.concatenate([TwS.T, TwS.T], axis=1)]
                        ).transpose(1, 0, 2)
    c["f1"] = np.stack([F1c, F1s]).transpose(1, 0, 2)
    return {k: np.ascontiguousarray(v.astype(np.float32)) for k, v in c.items()}


def _hy_feats(n, pos):
    t = np.linspace(0.0, 1.0, n, dtype=np.float32)
    w = ((2.0 * np.pi / n) * np.arange(n, dtype=np.float32)).astype(np.float32)
    f = np.linspace(1e-4, 15, 16, dtype=np.float32)[None, :]
    tt_ = t[pos][:, None]
    ww = w[pos][:, None]
    return np.concatenate([tt_, np.cos(f * ww), -np.sin(f * ww)], axis=-1).astype(np.float32)


def _hy_deltas():
    mn = np.log(1e-2) / 1.5
    mx = np.log(1e-2) / 0.3
    return np.abs(np.linspace(mn, mx, 256, dtype=np.float32))


def hyena_tables():
    n = np.arange(HN)
    t = np.where(n < L, n, HN - n)
    t = np.where(n == L, 0, t)
    feats = _hy_feats(L, t)
    featsP = feats.reshape(128, 256, 33).transpose(1, 0, 2).reshape(HN, 33).T
    tl = np.linspace(0.0, 1.0, L, dtype=np.float32)
    win = np.exp(-tl[t][:, None] * _hy_deltas()[None, :]).astype(np.float32)
    win[L] = 0.0
    win = win.reshape(2, 64, 256, 256)
    return np.ascontiguousarray(featsP.astype(np.float32)), win


def build_hyena(with_ctx):
    nc = bass.Bass("TRN2", target_bir_lowering=False)
    raw = _dram(nc, "raw", [3, HCH, 64, 258], F32)
    cwl = _dram(nc, "cwl", [64, 3 * HCH * 4], F32)
    skp = _dram(nc, "skp", [64, 2 * HCH], F32)
    w1 = _dram(nc, "w1", [33, 64], F32)
    w2 = _dram(nc, "w2", [64, 64], F32)
    w3s = _dram(nc, "w3s", [64, 4 * HCH], F32)
    fb = _dram(nc, "fb", [64, 3], F32)
    featsP = _dram(nc, "featsP", [33, HN], F32)
    win = _dram(nc, "win", [2, 64, 256, HCH], F32)
    d1 = _dram(nc, "d1", [2, 64, 256], F32)
    tw = _dram(nc, "tw", [128, 2, 2, 256], F32)
    c3 = _dram(nc, "c3", [128, 2, 3, 256], F32)
    ee = _dram(nc, "e", [128, 2, 2, 512], F32)
    tw2 = _dram(nc, "tw2", [128, 2, 512], F32)
    f1 = _dram(nc, "f1", [128, 2, 64], F32)
    out = _dram(nc, "out", [HCH, L], F32, out=True)
    if with_ctx:
        rawc = _dram(nc, "rawc", [3, HCH, 258], F32)
        cwc = _dram(nc, "cwc", [HCH, 12], F32)
        skc = _dram(nc, "skc", [HCH, 2], F32)
        featsC = _dram(nc, "featsC", [33, 256], F32)
        winC = _dram(nc, "winC", [HCH, 256], F32)
        outc = _dram(nc, "outc", [HCH, 256], F32, out=True)
    P = Prog(nc)
    S = slice(None)
    cw = P.sb([64, 3 * HCH * 4], F32)
    sk = P.sb([64, 2 * HCH], F32)
    W1 = P.sb([33, 64], F32)
    W2 = P.sb([64, 64], BF16)
    W3 = P.sb([64, 4 * HCH], BF16)
    FB = P.sb([64, 3], F32)
    FBB = P.sb([64, 2], F32)
    D1 = P.sb([64, 2, 256], BF16)
    TW = P.sb([128, 2, 2, 256], F32)
    C3 = P.sb([128, 2, 3, 256], BF16)
    EE = P.sb([128, 2, 2, 512], BF16)
    TW2 = P.sb([128, 2, 512], F32)
    F1 = P.sb([128, 2, 64], BF16)
    P.dma(cw[:], cwl)
    P.dma(sk[:], skp)
    P.dma(W1[:], w1)
    P.dma(FB[:], fb)
    P.dma(TW[:], tw)
    P.dma(TW2[:], tw2)
    P.dma(W2[:], w2, q="pool")
    P.dma(W3[:], w3s, q="pool")
    P.dma(D1[:], d1.rearrange("a p f -> p a f"), q="pool")
    P.dma(C3[:], c3, q="pool")
    P.dma(EE[:], ee, q="pool")
    P.dma(F1[:], f1, q="pool")
    P.ts(FBB[:], FB[:, 1:3], FB[:, 0:1], None, ALU.mult)
    banks = [P.ps([128, 512], F32) for _ in range(8)]

    NB3 = 3
    fts = [P.sb([33, 512], F32) for _ in range(NB3)]
    aas = [P.sb([64, 512], F32) for _ in range(NB3)]
    m1s = [P.sb([64, 512], F32) for _ in range(NB3)]
    m2s = [P.sb([64, 512], F32) for _ in range(NB3)]
    h1s = [P.sb([64, 512], BF16) for _ in range(NB3)]
    Gs = [P.sb([64, 512], BF16) for _ in range(NB3)]
    wt = [P.sb([64, 2, 32, HCH], F32)] * 2

    def wrap(i, n):
        a, m1, m2 = aas[i], m1s[i], m2s[i]
        P.ts(m2[:, 0:n], a[:, 0:n], PI, -1.0, ALU.is_gt, ALU.mult)
        P.stt(m1[:, 0:n], a[:, 0:n], -PI, m2[:, 0:n], ALU.is_lt, ALU.add)
        P.stt(a[:, 0:n], m1[:, 0:n], 2 * PI, a[:, 0:n], ALU.mult, ALU.add)

    def mlp_s1(i, ft, n, bank):
        P.mm(bank[0:64, 0:n], W1[:], ft)
        P.act(aas[i][:, 0:n], bank[0:64, 0:n], AF.Identity, scale=FB[:, 0:1], bias=FBB[:, 0:1])

    def mlp_s2(i, n, bank):
        wrap(i, n)
        P.act(h1s[i][:, 0:n], aas[i][:, 0:n], AF.Sin)
        P.mm(bank[0:64, 0:n], W2[:], h1s[i][:, 0:n])
        P.act(aas[i][:, 0:n], bank[0:64, 0:n], AF.Identity, scale=FB[:, 0:1], bias=FBB[:, 1:2])

    def mlp_s3(i, n, G):
        wrap(i, n)
        P.act(G[:, 0:n], aas[i][:, 0:n], AF.Sin)

    def mlp(ft, n, h1_unused, G):
        mlp_s1(0, ft, n, banks[0])
        mlp_s2(0, n, banks[1])
        mlp_s3(0, n, G)

    kf = P.sb([64, HCH, 256], BF16)
    kb = P.sb([64, HCH, 256], BF16)
    KH = P.sb([128, 2, 2, HCH * 128], BF16)
    z = P.sb([64, HCH, 256], F32)
    Bt = [P.sb([128, 2, 2, 512], BF16) for _ in range(2)]
    t12 = P.sb([128, 512], F32)
    t34 = P.sb([128, 512], F32)
    rawt = [P.sb([64, 258], F32) for _ in range(3)]
    vin = [P.sb([64, 4, 256], F32) for _ in range(2)]
    vb = [P.sb([64, 256], BF16) for _ in range(2)]
    Zt = P.sb([128, 2, 2, 512], BF16)
    pa = t12
    pb = t34
    Dt = [P.sb([128, 2, 512], BF16) for _ in range(2)]
    gate = [P.sb([64, 256], F32) for _ in range(2)]
    tq = [P.sb([64, 256], F32) for _ in range(2)]
    ot = [P.sb([64, 256], F32) for _ in range(2)]
    cnt = {"raw": 0, "g": 0, "vb": 0, "o": 0}

    def conv_row(w, ch, dst):
        r = rawt[cnt["raw"] % 3]
        cnt["raw"] += 1
        P.dma(r[:], raw[w, ch], q="sp" if cnt["raw"] % 2 == 0 else "act")
        o = (w * HCH + ch) * 4
        P.ts(dst, r[:, 1:257], cw[:, o + 1:o + 2], cw[:, o + 3:o + 4], ALU.mult, ALU.add)
        P.stt(dst, r[:, 0:256], cw[:, o:o + 1], dst, ALU.mult, ALU.add)
        P.stt(dst, r[:, 2:258], cw[:, o + 2:o + 3], dst, ALU.mult, ALU.add)

    def twiddle_fwd(psA, h, btile, ch4):
        P.tt(t12[:, 0:256], psA[:, 0:256], TW[:, h, 0, :], ALU.mult)
        P.tt(t34[:, 0:256], psA[:, 0:256], TW[:, h, 1, :], ALU.mult)
        cs = slice(ch4 * 128, (ch4 + 1) * 128)
        P.tt(btile.sub((h, 0, ch4), (S, h, 0, cs)), t12[:, 0:128], t34[:, 128:256], ALU.add)
        P.tt(btile.sub((h, 1, ch4), (S, h, 1, cs)), t12[:, 128:256], t34[:, 0:128], ALU.subtract, eng="pool")

    def step3(btile, evac):
        ball = _AllView(btile)
        for g in range(2):
            gs = slice(g * 128, (g + 1) * 128)
            for ri in range(2):
                ps = banks[2 + g * 2 + ri]
                terms = []
                for h in range(2):
                    if ri == 0:
                        terms += [(0, h, 0), (1, h, 1)]
                    else:
                        terms += [(0, h, 1), (2, h, 0)]
                for ti, (m, h, bri) in enumerate(terms):
                    P.mm(ps[:, :], C3[:, h, m, gs], ball[:, h, bri, :], start=(ti == 0), stop=(ti == 3))
                evac(g, ri, ps)

    for o in range(2):
        def fg_s1(blk):
            i = blk % NB3
            P.dma(fts[i][:], featsP[:, blk * 512:(blk + 1) * 512], q="sp")
            mlp_s1(i, fts[i][:], 512, banks[blk % 2])

        def fg_s2(blk):
            mlp_s2(blk % NB3, 512, banks[4 + blk % 2])

        def fg_s3(blk):
            i = blk % NB3
            G = Gs[i]
            mlp_s3(i, 512, G)
            w_ = wt[(blk // 8) % 2]
            if blk % 8 == 0:
                for half in range(2):
                    P.dma(w_.sub(half, (S, half)), win[half, :, blk * 4:blk * 4 + 32, :], q="act")
            for half in range(2):
                pk = banks[2 + half + 4 * (blk % 2)]
                wc = (o * 2 + half) * HCH
                for q in range(4):
                    P.mm(pk[0:64, q * HCH:(q + 1) * HCH], G[:, q * 128 + half * 64:q * 128 + half * 64 + 64],
                         W3[:, wc:wc + HCH])
                kt = kf if half == 0 else kb
                q0 = (blk % 8) * 4
                src = V(pk.t[0:64, 0:4 * HCH].rearrange("p (q c) -> p c q", q=4), [pk.trk], pk.bank)
                wv_ = V(w_.t[:, half, q0:q0 + 4, :].rearrange("p q c -> p c q"), [w_.subs[half]])
                P.tt(kt.sub(blk, (S, S, slice(blk * 4, blk * 4 + 4))), src, wv_, ALU.mult)

        nblk = 64 if not HY_SKIP.get('filt') else 0
        for t_ in range(nblk + 2):
            if t_ < nblk:
                fg_s1(t_)
            if 0 <= t_ - 1 < nblk:
                fg_s2(t_ - 1)
            if 0 <= t_ - 2 < nblk:
                fg_s3(t_ - 2)
        kfa = _AllView(kf)
        kba = _AllView(kb)
        for cg in range(HCH // 4 if not HY_SKIP.get('fdft') else 0):
            bt = Bt[cg % 2]
            for ch4 in range(4):
                ch = cg * 4 + ch4
                for h in range(2):
                    psA = banks[h]
                    hs = slice(h * 128, (h + 1) * 128)
                    P.mm(psA[:, 0:256], kfa[:, ch, hs], D1[:, 0, :], start=True, stop=False)
                    P.mm(psA[:, 0:256], kba[:, ch, hs], D1[:, 1, :], start=False, stop=True)
                    twiddle_fwd(psA, h, bt, ch4)

            def evac_k(g, ri, ps, cg=cg):
                P.copy(KH.sub((g, ri, cg), (S, g, ri, slice(cg * 512, (cg + 1) * 512))), ps[:, :], eng="act")
            step3(bt, evac_k)
        for cg in range(HCH // 4):
            bt = Bt[cg % 2]
            vi = vin[cg % 2]
            for ch4 in range(4):
                ch = cg * 4 + ch4
                if o == 0:
                    conv_row(0, ch, vi.sub(ch4, (S, ch4, S)))
                    src = vi.sub(ch4, (S, ch4, S))
                else:
                    src = z.sub(ch, (S, ch, S))
                vbt = vb[cnt["vb"] % 2]
                cnt["vb"] += 1
                P.copy(vbt[:], src, eng="act")
                for h in range(2):
                    psA = banks[h]
                    P.mm(psA[:, 0:256], vbt[:, h * 128:(h + 1) * 128], D1[:, 0, :])
                    twiddle_fwd(psA, h, bt, ch4)

            def evac_z(g, ri, ps, cg=cg):
                if ri == 1:
                    xr = banks[2 + g * 2]
                    xi = banks[2 + g * 2 + 1]
                    cs = slice(cg * 512, (cg + 1) * 512)
                    kr = KH.sub((g, 0, cg), (S, g, 0, cs))
                    ki = KH.sub((g, 1, cg), (S, g, 1, cs))
                    P.tt(pa[:], xr[:, :], kr, ALU.mult)
                    P.tt(pb[:], xi[:, :], ki, ALU.mult)
                    P.tt(Zt.sub((g, 0), (S, g, 0, S)), pa[:], pb[:], ALU.subtract, eng="pool")
                    P.tt(pa[:], xr[:, :], ki, ALU.mult)
                    P.tt(pb[:], xi[:, :], kr, ALU.mult)
                    P.tt(Zt.sub((g, 1), (S, g, 1, S)), pa[:], pb[:], ALU.add, eng="pool")
            step3(bt, evac_z)
            for ch4 in range(4):
                ch = cg * 4 + ch4
                cs = slice(ch4 * 128, (ch4 + 1) * 128)
                psC = banks[6]
                ti = 0
                for g in range(2):
                    for ri in range(2):
                        P.mm(psC[:, :], Zt.sub((g, ri), (S, g, ri, cs)), EE[:, g, ri, :], start=(ti == 0), stop=(ti == 3))
                        ti += 1
                dt_ = Dt[(ch4 // 2) % 2]
                c2 = ch4 % 2
                P.tt(t12[:], psC[:, :], TW2[:, 0, :], ALU.mult)
                P.tt(t34[:], psC[:, :], TW2[:, 1, :], ALU.mult)
                P.tt(dt_.sub((0, c2), (S, 0, slice(c2 * 256, (c2 + 1) * 256))), t12[:, 0:256], t34[:, 256:512], ALU.subtract)
                P.tt(dt_.sub((1, c2), (S, 1, slice(c2 * 256, (c2 + 1) * 256))), t34[:, 0:256], t12[:, 256:512], ALU.add,
                     eng="pool")
                if c2 == 1:
                    psY = banks[7]
                    da = _AllView(dt_)
                    P.mm(psY[0:64, :], F1[:, 0, :], da[:, 0, :], start=True, stop=False)
                    P.mm(psY[0:64, :], F1[:, 1, :], da[:, 1, :], start=False, stop=True)
                    for cc2 in range(2):
                        chx = ch - 1 + cc2
                        c4x = ch4 - 1 + cc2
                        gt = gate[cnt["g"] % 2]
                        tqq = tq[cnt["g"] % 2]
                        cnt["g"] += 1
                        conv_row(1 + o, chx, gt[:])
                        vsrc = vi.sub(c4x, (S, c4x, S)) if o == 0 else z.sub(chx, (S, chx, S))
                        so = o * HCH + chx
                        P.stt(tqq[:], vsrc, sk[:, so:so + 1], psY[0:64, cc2 * 256:(cc2 + 1) * 256], ALU.mult, ALU.add)
                        if o == 0:
                            P.tt(z.sub(chx, (S, chx, S)), tqq[:], gt[:], ALU.mult, eng="pool")
                        else:
                            oo = ot[cnt["o"] % 2]
                            cnt["o"] += 1
                            P.tt(oo[:], tqq[:], gt[:], ALU.mult, eng="pool")
                            P.dma(out[chx].rearrange("(a b) -> a b", b=256), oo[:], q="sp")
    if with_ctx:
        ftc = P.sb([33, 256], F32)
        wc_ = P.sb([HCH, 256], F32)
        cwc_t = P.sb([HCH, 12], F32)
        skc_t = P.sb([HCH, 2], F32)
        hfb = P.sb([HCH, 4, 256], F32)
        rc = P.sb([HCH, 3, 258], F32)
        class _Sl:
            def __init__(self, b):
                self.b = b
                self.t = b.t[0:HCH, 0:256]
                self.trk = b.trk

            def __getitem__(self, idx):
                return V(self.t[idx], [self.b.trk])
        u3 = [_Sl(m2s[0]), _Sl(m2s[1]), _Sl(m2s[2])]
        accs = [_Sl(aas[0]), _Sl(aas[1]), _Sl(aas[2]), _Sl(m1s[0])]
        zc = _Sl(m1s[1])
        Gc = P.sb([64, 256], BF16)
        P.dma(ftc[:], featsC)
        P.dma(wc_[:], winC)
        P.dma(cwc_t[:], cwc)
        P.dma(skc_t[:], skc)
        P.dma(rc[:], rawc.rearrange("w c n -> c w n"))
        mlp_s1(0, ftc[:], 256, banks[0])
        mlp_s2(0, 256, banks[1])
        mlp_s3(0, 256, Gc)
        for od in range(4):
            P.mm(banks[2][0:HCH, 0:256], W3[:, od * HCH:(od + 1) * HCH], Gc[:])
            P.tt(hfb.sub(od, (S, od, S)), banks[2][0:HCH, 0:256], wc_[:], ALU.mult)
        for w in range(3):
            dst = u3[w][:, :]
            P.ts(dst, rc[:, w, 1:257], cwc_t[:, w * 4 + 1:w * 4 + 2], cwc_t[:, w * 4 + 3:w * 4 + 4], ALU.mult, ALU.add)
            P.stt(dst, rc[:, w, 0:256], cwc_t[:, w * 4:w * 4 + 1], dst, ALU.mult, ALU.add)
            P.stt(dst, rc[:, w, 2:258], cwc_t[:, w * 4 + 2:w * 4 + 3], dst, ALU.mult, ALU.add)
        for o in range(2):
            vv_ = u3[0] if o == 0 else zc
            P.ts(accs[0][:, :], vv_[:, :], skc_t[:, o:o + 1], None, ALU.mult)
            for a_ in accs[1:]:
                P.memset(a_[:, :], 0.0)
            ai = 0
            hf_ = hfb.sub(o * 2, (S, o * 2, S))
            hb_ = hfb.sub(o * 2 + 1, (S, o * 2 + 1, S))
            for dd in range(256):
                a_ = accs[ai % 4]
                ai += 1
                P.stt(a_[:, dd:256], vv_[:, 0:256 - dd], hf_[:, dd:dd + 1], a_[:, dd:256], ALU.mult, ALU.add)
                if dd >= 1:
                    a_ = accs[ai % 4]
                    ai += 1
                    P.stt(a_[:, 0:256 - dd], vv_[:, dd:256], hb_[:, dd:dd + 1], a_[:, 0:256 - dd], ALU.mult, ALU.add)
            P.tt(accs[0][:, :], accs[0][:, :], accs[1][:, :], ALU.add)
            P.tt(accs[2][:, :], accs[2][:, :], accs[3][:, :], ALU.add, eng="pool")
            P.tt(accs[0][:, :], accs[0][:, :], accs[2][:, :], ALU.add)
            P.tt(zc[:, :], accs[0][:, :], u3[1 + o][:, :], ALU.mult)
        P.dma(outc, zc[:, :])
    P.finish()
    return nc


def run_hyena(proj, layer, inp, with_ctx):
    cs = hyena_consts()
    featsP, win = hyena_tables()
    cwf = inp["hy_conv_w"][layer]
    cbf = inp["hy_conv_b"][layer]
    w3 = inp["hy_ffn_w3"][layer].reshape(64, 2, 2, 256)
    fb = np.stack([inp["hy_freq"][layer], inp["hy_ffn_b1"][layer], inp["hy_ffn_b2"][layer]], axis=1).astype(np.float32)
    z1 = np.zeros((1,), np.float32)
    if with_ctx:
        featsC = np.ascontiguousarray(_hy_feats(LC, np.arange(LC)).T)
        tlc = np.linspace(0.0, 1.0, LC, dtype=np.float32)
        winC_all = np.exp(-tlc[None, :] * _hy_deltas()[:, None]).astype(np.float32)
    maps = []
    for c in range(NCORE):
        chs = np.arange(HCH * c, HCH * (c + 1))
        rows = np.stack([proj[w * 256 + chs] for w in range(3)])
        lat = rows[:, :, LC:]
        pad = np.concatenate([np.zeros((3, HCH, 1), np.float32), lat, np.zeros((3, HCH, 257), np.float32)], axis=2)
        idx = (np.arange(64) * 256)[:, None] + np.arange(258)[None, :]
        raw = np.ascontiguousarray(pad[:, :, idx])
        cw4 = np.stack([np.stack([cwf[0, w * 256 + chs], cwf[1, w * 256 + chs], cwf[2, w * 256 + chs],
                                  cbf[w * 256 + chs]], axis=1) for w in range(3)])
        m = {"raw": raw,
             "cwl": np.ascontiguousarray(np.broadcast_to(cw4.reshape(1, -1), (64, 3 * HCH * 4))).astype(np.float32),
             "skp": np.ascontiguousarray(np.broadcast_to(inp["hy_bias"][layer][:, chs].reshape(1, -1), (64, 2 * HCH))).astype(np.float32),
             "w1": inp["hy_ffn_w1"][layer], "w2": inp["hy_ffn_w2"][layer],
             "w3s": np.ascontiguousarray(w3[:, :, :, chs].reshape(64, 4 * HCH)), "fb": fb,
             "featsP": featsP, "win": np.ascontiguousarray(win[:, :, :, chs]),
             "d1": cs["d1"], "tw": cs["tw"], "c3": cs["c3"], "e": cs["e"], "tw2": cs["tw2"], "f1": cs["f1"]}
        if with_ctx:
            cr = rows[:, :, :LC]
            m["rawc"] = np.ascontiguousarray(np.concatenate([np.zeros((3, HCH, 1), np.float32), cr,
                                                             np.zeros((3, HCH, 1), np.float32)], axis=2))
            m["cwc"] = np.ascontiguousarray(cw4.transpose(1, 0, 2).reshape(HCH, 12).astype(np.float32))
            m["skc"] = np.ascontiguousarray(inp["hy_bias"][layer][:, chs].T.astype(np.float32))
            m["featsC"] = featsC
            m["winC"] = np.ascontiguousarray(winC_all[chs])
        maps.append(m)
    key = "hyena_ctx" if with_ctx else "hyena"
    res = _run(key, lambda: build_hyena(with_ctx), maps)
    y = np.zeros((256, TT), np.float32)
    for c in range(NCORE):
        y[HCH * c:HCH * (c + 1), LC:] = res[c]["out"]
        if with_ctx:
            y[HCH * c:HCH * (c + 1), :LC] = res[c]["outc"]
    return y


def kernel(**inp):
    inp = {k: np.asarray(v) for k, v in inp.items()}
    mod = run_mod(inp)
    xT_lat = np.ascontiguousarray(inp["x"][0].T)
    xT_ctx = np.ascontiguousarray(inp["ctx"][0].T)
    for layer in range(2):
        proj = run_inproj(xT_ctx, xT_lat, mod, layer, inp["w_in"][layer])
        hy = run_hyena(proj, layer, inp, with_ctx=(layer == 0))
        yf, yb = run_ssd(proj, layer, inp)
        at = run_attn(proj, layer, inp)
        mix_rows = np.ascontiguousarray(np.concatenate([hy, yf, yb, proj[768:1280], at], axis=0))
        x1T = run_post1(mix_rows, xT_ctx, xT_lat, mod, layer, inp)
        x2T = run_ffn(x1T, mod, layer, inp)
        xT_ctx = np.ascontiguousarray(x2T[:, :LC])
        xT_lat = np.ascontiguousarray(x2T[:, LC:])
    return np.ascontiguousarray(xT_lat.T)[None].astype(np.float32)
```

```python
import numpy as np
from contextlib import ExitStack
import concourse.bass as bass
import concourse.mybir as mybir
from concourse.bass_utils import run_bass_kernel_spmd

F32 = mybir.dt.float32
BF16 = mybir.dt.bfloat16
AF = mybir.ActivationFunctionType
ALU = mybir.AluOpType
AX = mybir.AxisListType

ENGS = ("pe", "dve", "act", "pool", "sp")
NDS = 8


class Trk:
    __slots__ = ("w", "rs")

    def __init__(self):
        self.w = None
        self.rs = {}


class V:
    __slots__ = ("ap", "trks", "bank")

    def __init__(self, ap, trks, bank=None):
        self.ap = ap
        self.trks = trks
        self.bank = bank

    def __getitem__(self, idx):
        return V(self.ap[idx], self.trks, self.bank)


class Buf:
    def __init__(self, t, psum=False):
        self.t = t
        self.trk = Trk()
        self.subs = {}
        self.bank = {} if psum else None

    def __getitem__(self, idx):
        return V(self.t[idx], [self.trk], self.bank)

    def sub(self, key, idx):
        if key not in self.subs:
            self.subs[key] = Trk()
        return V(self.t[idx], [self.subs[key]], self.bank)

    def all(self, idx):
        return V(self.t[idx], [self.trk] + list(self.subs.values()), self.bank)


class Prog:
    def __init__(self, nc, self_sync=True):
        self.nc = nc
        self.es = ExitStack()
        self.ops = {e: [] for e in ENGS}
        self.cnt = {e: 0 for e in ENGS}
        self.seen = {e: {} for e in ENGS}
        self.sems = {}
        self.self_sync = self_sync
        for e in ENGS:
            self.sems[e] = self.es.enter_context(nc.semaphore("s_" + e))
        self.dcnt = {"sp": 0, "pool": 0, "act": 0}
        for q in ("sp", "pool", "act"):
            for i in range(NDS):
                self.sems[(q, i)] = self.es.enter_context(nc.semaphore("d_%s%d" % (q, i)))
        self.nbuf = 0
        self.dma_tokens = []

    def sb(self, shape, dt, name=None):
        self.nbuf += 1
        t = self.es.enter_context(self.nc.sbuf_tensor(name or "sb%d" % self.nbuf, list(shape), dt))
        return Buf(t)

    def ps(self, shape, dt, name=None):
        self.nbuf += 1
        full = [128, 2048 // mybir.dt.size(dt)]
        assert shape[0] <= 128 and int(np.prod(shape[1:])) <= full[1]
        t = self.es.enter_context(self.nc.psum_tensor(name or "ps%d" % self.nbuf, full, dt))
        return Buf(t, psum=True)

    def _emit(self, eng, fn, reads, writes, dma=False, pe_acc=False):
        deps = {}

        def add(tok):
            if tok is None:
                return
            k, v = tok
            if deps.get(k, 0) < v:
                deps[k] = v

        for vw in reads:
            for t in vw.trks:
                add(t.w)
        for vw in writes:
            for t in vw.trks:
                add(t.w)
                for k, v in t.rs.items():
                    add((k, v))
        for vw in list(reads) + list(writes):
            if vw.bank is not None:
                for f, tk in vw.bank.items():
                    if f != eng:
                        add(tk)
        waits = []
        seen = self.seen[eng]
        for k, v in deps.items():
            if k == eng:
                if eng == "pe" or not self.self_sync:
                    continue
            if seen.get(k, 0) >= v:
                continue
            seen[k] = v
            waits.append((k, v))
        if dma:
            n = self.dcnt[eng]
            self.dcnt[eng] = n + 1
            key = (eng, n % NDS)
            val = 16 * (n // NDS + 1)
            if n >= NDS and seen.get(key, 0) < val - 16:
                waits.append((key, val - 16))
                seen[key] = val - 16
            tok = (key, val)
            self.dma_tokens.append(tok)
        else:
            self.cnt[eng] += 1
            tok = (eng, self.cnt[eng])
        self.ops[eng].append((waits, fn, tok, dma))
        for vw in list(reads) + list(writes):
            if vw.bank is not None:
                vw.bank[eng] = tok
        for vw in reads:
            for t in vw.trks:
                if t.rs.get(tok[0], 0) < tok[1]:
                    t.rs[tok[0]] = tok[1]
        for vw in writes:
            for t in vw.trks:
                t.w = tok
                t.rs = {}
        return tok

    def dma(self, out, in_, q="sp", **kw):
        reads = [in_] if isinstance(in_, V) else []
        writes = [out] if isinstance(out, V) else []
        o = out.ap if isinstance(out, V) else out
        i = in_.ap if isinstance(in_, V) else in_
        return self._emit(q, lambda e: e.dma_start(out=o, in_=i, **kw), reads, writes, dma=True)

    def mm(self, out, lhsT, rhs, start=True, stop=True, **kw):
        return self._emit("pe", lambda e: e.matmul(out.ap, lhsT.ap, rhs.ap, start=start, stop=stop, **kw),
                          [lhsT, rhs], [out])

    def tr(self, out, in_, ident):
        return self._emit("pe", lambda e: e.transpose(out.ap, in_.ap, ident.ap), [in_, ident], [out])

    def act(self, out, in_, func, bias=None, scale=None, accum=None, extra_reads=()):
        kw = {}
        reads = [in_] + list(extra_reads)
        writes = [out]
        if bias is not None:
            if isinstance(bias, V):
                kw["bias"] = bias.ap
                reads.append(bias)
            else:
                kw["bias"] = bias
        if scale is not None:
            if isinstance(scale, V):
                kw["scale"] = scale.ap
                reads.append(scale)
            else:
                kw["scale"] = scale
        if accum is not None:
            kw["accum_out"] = accum.ap
            writes.append(accum)
        return self._emit("act", lambda e: e.activation(out.ap, in_.ap, func, **kw), reads, writes)

    def tt(self, out, a, b, op, eng="dve"):
        return self._emit(eng, lambda e: e.tensor_tensor(out.ap, a.ap, b.ap, op), [a, b], [out])

    def ts(self, out, a, s1, s2, op0, op1=None, eng="dve", accum=None):
        reads = [a]
        writes = [out]
        x1 = s1
        x2 = s2
        if isinstance(s1, V):
            reads.append(s1)
            x1 = s1.ap
        if isinstance(s2, V):
            reads.append(s2)
            x2 = s2.ap
        kw = {}
        if op1 is not None:
            kw["op1"] = op1
        if accum is not None:
            kw["accum_out"] = accum.ap
            writes.append(accum)
        return self._emit(eng, lambda e: e.tensor_scalar(out.ap, a.ap, x1, x2, op0, **kw), reads, writes)

    def stt(self, out, a, s, b, op0, op1, accum=None):
        reads = [a, b]
        writes = [out]
        x = s
        if isinstance(s, V):
            reads.append(s)
            x = s.ap
        kw = {}
        if accum is not None:
            kw["accum_out"] = accum.ap
            writes.append(accum)
        return self._emit("dve", lambda e: e.scalar_tensor_tensor(out.ap, a.ap, x, b.ap, op0, op1, **kw),
                          reads, writes)

    def copy(self, out, in_, eng="dve"):
        if eng == "act":
            return self._emit("act", lambda e: e.copy(out.ap, in_.ap), [in_], [out])
        return self._emit(eng, lambda e: e.tensor_copy(out.ap, in_.ap), [in_], [out])

    def memset(self, out, val, eng="dve"):
        return self._emit(eng, lambda e: e.memset(out.ap, val), [], [out])

    def gen(self, eng, fn, reads, writes):
        return self._emit(eng, fn, reads, writes)

    def finish(self):
        nc = self.nc
        last = {}
        for k, v in self.dma_tokens:
            if last.get(k, 0) < v:
                last[k] = v
        for e in ENGS:
            if self.cnt[e] > 0:
                last[e] = self.cnt[e]
        final_waits = [(k, v) for k, v in last.items()]
        engmap = {"pe": "tensor", "dve": "vector", "act": "scalar", "pool": "gpsimd", "sp": "sync"}
        with nc.Block() as block:
            for e in ENGS:
                ops = self.ops[e]
                extra = final_waits if e == "sp" else []
                if not ops and not extra:
                    continue

                def body(eng, ops=ops, e=e, extra=extra):
                    for waits, fn, tok, dma in ops:
                        for k, v in waits:
                            eng.wait_ge(self.sems[k], v)
                        ins = fn(eng)
                        if dma:
                            ins.then_inc(self.sems[tok[0]], 16)
                        else:
                            ins.then_inc(self.sems[e], 1)
                    for k, v in extra:
                        eng.wait_ge(self.sems[k], v)

                getattr(block, engmap[e])(body)
        self.es.close()


L = 16384
LC = 256
TT = L + LC
NCORE = 8
NCX = LC // NCORE
NLT = L // NCORE
NTK = NCX + NLT
DM = 1024
D_IN = 2832
HYC = 768
SSDC = 1552
EPS = 1e-6
ALPHA = 2.0 ** 0.5
_PROGS = {}


def _dram(nc, name, shape, dt, out=False):
    return nc.dram_tensor(name, list(shape), dt, kind="ExternalOutput" if out else "ExternalInput").ap()


def _run(key, builder, in_maps):
    if key not in _PROGS:
        _PROGS[key] = builder()
    nc = _PROGS[key]
    res = run_bass_kernel_spmd(nc, in_maps, core_ids=list(range(NCORE)))
    return res.results


def _fm(v):
    v = np.asarray(v, np.float32).reshape(-1, 128)
    return np.ascontiguousarray(v.T)


def build_mod():
    nc = bass.Bass("TRN2", target_bir_lowering=False)
    c8 = _dram(nc, "c8", [128, 16], F32)
    wm = _dram(nc, "wm", [2, 1024, 768], F32)
    bm = _dram(nc, "bm", [2, 1536], F32)
    out = _dram(nc, "out", [2, 1536], F32, out=True)
    P = Prog(nc)
    cs = P.sb([128, 16], F32)
    s = P.sb([128, 16], F32)
    S = P.sb([128, 8, 2], F32)
    W = P.sb([128, 2, 8, 768], F32)
    bt = P.sb([2, 1536], F32)
    ot = P.sb([2, 1536], F32)
    P.dma(cs[:], c8)
    P.dma(bt[:], bm)
    for l in range(2):
        P.dma(W.sub(l, (slice(None), l)), wm[l].rearrange("(k p) n -> p k n", p=128), q="sp" if l == 0 else "act")
    P.act(s[:], cs[:], AF.Silu)
    P.copy(S[:, :, 0], s[:, 0:8])
    P.copy(S[:, :, 1], s[:, 8:16])
    pss = [P.ps([2, 512], F32) for _ in range(2)]
    i = 0
    for l in range(2):
        for (n0, n1) in ((0, 512), (512, 768)):
            ps = pss[i % 2]
            i += 1
            for k in range(8):
                P.mm(ps[0:2, 0:n1 - n0], S[:, k, :], W.sub(l, (slice(None), l, k, slice(n0, n1))),
                     start=(k == 0), stop=(k == 7))
            P.tt(ot[:, l * 768 + n0:l * 768 + n1], ps[0:2, 0:n1 - n0], bt[:, l * 768 + n0:l * 768 + n1], ALU.add)
    P.dma(out, ot[:])
    P.finish()
    return nc


def run_mod(inp):
    c8 = np.concatenate([_fm(inp["c"][0]), _fm(inp["c_ctx"])], axis=1)
    maps = []
    for c in range(NCORE):
        wm = np.ascontiguousarray(inp["w_mod"][:, :, c * 768:(c + 1) * 768])
        b = inp["b_mod"][:, c * 768:(c + 1) * 768].reshape(1, 1536)
        maps.append({"c8": c8, "wm": wm, "bm": np.ascontiguousarray(np.repeat(b, 2, axis=0))})
    res = _run("mod", build_mod, maps)
    full = np.concatenate([r["out"].reshape(2, 2, 768) for r in res], axis=2)
    return full


def build_inproj():
    nc = bass.Bass("TRN2", target_bir_lowering=False)
    xT = _dram(nc, "xT", [1024, NTK], F32)
    md = _dram(nc, "md", [128, 32], F32)
    w = _dram(nc, "w", [1024, D_IN], F32)
    out = _dram(nc, "out", [D_IN, NTK], F32, out=True)
    P = Prog(nc)
    x = P.sb([128, 8, NTK], F32)
    h = P.sb([128, 8, NTK], BF16)
    W = P.sb([128, 8, D_IN], BF16)
    m = P.sb([128, 32], F32)
    m1 = P.sb([128, 32], F32)
    P.dma(m[:], md)
    for k in range(8):
        P.dma(x.sub(k, (slice(None), k)), xT[k * 128:(k + 1) * 128, :], q="sp" if k % 2 == 0 else "act")
    for k in range(8):
        for (c0, c1) in ((0, 1416), (1416, 2832)):
            P.dma(W.sub(k, (slice(None), k, slice(c0, c1))), w[k * 128:(k + 1) * 128, c0:c1], q="pool")
    P.ts(m1[:], m[:], 1.0, None, ALU.add)
    for k in range(8):
        P.act(h.sub(k, (slice(None), k, slice(0, NCX))), x.sub(k, (slice(None), k, slice(0, NCX))), AF.Identity,
              scale=m1[:, k:k + 1], bias=m[:, 8 + k:9 + k])
        P.act(h.sub(k, (slice(None), k, slice(NCX, NTK))), x.sub(k, (slice(None), k, slice(NCX, NTK))), AF.Identity,
              scale=m1[:, 16 + k:17 + k], bias=m[:, 24 + k:25 + k])
    nblk = [(0, 512), (512, 1024), (1024, 1536), (1536, 2048), (2048, NTK)]
    pss = [P.ps([128, 512], F32) for _ in range(4)]
    obs = [P.sb([128, NTK], F32) for _ in range(2)]
    i = 0
    for mi in range(23):
        r0 = mi * 128
        M = min(128, D_IN - r0)
        ob = obs[mi % 2]
        for (n0, n1) in nblk:
            ps = pss[i % 4]
            for k in range(8):
                P.mm(ps[0:M, 0:n1 - n0], W.sub(k, (slice(None), k, slice(r0, r0 + M))),
                     h.sub(k, (slice(None), k, slice(n0, n1))), start=(k == 0), stop=(k == 7))
            if i % 2 == 0:
                P.copy(ob[0:M, n0:n1], ps[0:M, 0:n1 - n0], eng="dve")
            else:
                P.copy(ob[0:M, n0:n1], ps[0:M, 0:n1 - n0], eng="act")
            i += 1
        P.dma(out[r0:r0 + M, :], ob[0:M, :], q="sp")
    P.finish()
    return nc


def tok_shard(a_ctx, a_lat):
    return [np.ascontiguousarray(np.concatenate([a_ctx[:, c * NCX:(c + 1) * NCX], a_lat[:, c * NLT:(c + 1) * NLT]],
                                                axis=1)) for c in range(NCORE)]


def tok_unshard(parts):
    ctx = np.concatenate([p[:, :NCX] for p in parts], axis=1)
    lat = np.concatenate([p[:, NCX:] for p in parts], axis=1)
    return np.concatenate([ctx, lat], axis=1)


def run_inproj(xT_ctx, xT_lat, mod, layer, w_in):
    ml = mod[0, layer].reshape(6, 1024)
    mc = mod[1, layer].reshape(6, 1024)
    md = np.concatenate([_fm(mc[1]), _fm(mc[0]), _fm(ml[1]), _fm(ml[0])], axis=1)
    xs = tok_shard(xT_ctx, xT_lat)
    maps = [{"xT": xs[c], "md": md, "w": w_in} for c in range(NCORE)]
    res = _run("inproj", build_inproj, maps)
    return tok_unshard([r["out"] for r in res])


def _ln_block(P, r, n, lng, lnb, out, ones, ps1, ps2, rc, sq, rstd):
    for k in range(8):
        P.mm(ps1[:, 0:n], ones[:], r[:, k, 0:n], start=(k == 0), stop=(k == 7))
    for k in range(8):
        P.stt(rc.sub(k, (slice(None), k, slice(0, n))), ps1[:, 0:n], -1.0 / DM, r[:, k, 0:n], ALU.mult, ALU.add)
        if k % 2 == 0:
            P.act(sq.sub(k, (slice(None), k, slice(0, n))), rc.sub(k, (slice(None), k, slice(0, n))), AF.Square)
        else:
            P.tt(sq.sub(k, (slice(None), k, slice(0, n))), rc.sub(k, (slice(None), k, slice(0, n))),
                 rc.sub(k, (slice(None), k, slice(0, n))), ALU.mult, eng="pool")
    for k in range(8):
        P.mm(ps2[:, 0:n], ones[:], sq.sub(k, (slice(None), k, slice(0, n))), start=(k == 0), stop=(k == 7))
    P.act(rstd[:, 0:n], ps2[:, 0:n], AF.Sqrt, bias=EPS, scale=1.0 / DM)
    P.gen("dve", lambda e: e.reciprocal(rstd.t[:, 0:n], rstd.t[:, 0:n]), [rstd[:, 0:n]], [rstd[:, 0:n]])
    for k in range(8):
        P.tt(rc.sub(k, (slice(None), k, slice(0, n))), rc.sub(k, (slice(None), k, slice(0, n))), rstd[:, 0:n], ALU.mult,
             eng="dve" if k % 2 == 0 else "pool")
        P.ts(out.sub(k, (slice(None), k, slice(0, n))), rc.sub(k, (slice(None), k, slice(0, n))),
             lng[:, k:k + 1], lnb[:, k:k + 1], ALU.mult, ALU.add, eng="dve" if k % 2 == 1 else "pool")


PB = 256
POST_BLKS = [(0, NCX)] + [(NCX + PB * b, NCX + PB * (b + 1)) for b in range(NLT // PB)]


def build_post1():
    nc = bass.Bass("TRN2", target_bir_lowering=False)
    mixin = _dram(nc, "mixin", [2048, NTK], F32)
    xT = _dram(nc, "xT", [1024, NTK], F32)
    wo = _dram(nc, "wo", [1024, 1024], F32)
    pr = _dram(nc, "pr", [128, 36], F32)
    out = _dram(nc, "out", [1024, NTK], F32, out=True)
    P = Prog(nc)
    W = P.sb([128, 8, 1024], BF16)
    prm = P.sb([128, 36], F32)
    ones = P.sb([128, 128], F32)
    P.dma(prm[:], pr)
    for k in range(8):
        P.dma(W.sub(k, (slice(None), k)), wo[k * 128:(k + 1) * 128, :], q="pool")
    P.memset(ones[:], 1.0)
    ins = [P.sb([128, 16, PB], F32) for _ in range(2)]
    xs = [P.sb([128, 8, PB], F32) for _ in range(2)]
    g = P.sb([128, 4, PB], F32)
    sz = P.sb([128, 4, PB], F32)
    gsq = P.sb([128, 4, PB], F32)
    rstdg = P.sb([128, 2, PB], F32)
    mix = P.sb([128, 8, PB], BF16)
    r = P.sb([128, 8, PB], F32)
    rc = P.sb([128, 8, PB], F32)
    sq = P.sb([128, 8, PB], F32)
    rstd = P.sb([128, PB], F32)
    ob = [P.sb([128, 8, PB], F32) for _ in range(2)]
    psg = [P.ps([128, 512], F32) for _ in range(2)]
    psm = [P.ps([128, 512], F32) for _ in range(3)]
    ps1 = P.ps([128, 512], F32)
    ps2 = P.ps([128, 512], F32)
    mi = 0
    for bi, (n0, n1) in enumerate(POST_BLKS):
        n = n1 - n0
        it = ins[bi % 2]
        xt = xs[bi % 2]
        o = ob[bi % 2]
        for k in range(16):
            P.dma(it.sub(k, (slice(None), k, slice(0, n))), mixin[k * 128:(k + 1) * 128, n0:n1],
                  q="sp" if k % 2 == 0 else "act")
        for k in range(8):
            P.dma(xt.sub(k, (slice(None), k, slice(0, n))), xT[k * 128:(k + 1) * 128, n0:n1],
                  q="sp" if k % 2 == 0 else "act")
        sl = slice(0, n)
        S = slice(None)
        for (kd, ks) in ((0, 0), (1, 1), (6, 14), (7, 15)):
            P.copy(mix.sub(kd, (S, kd, sl)), it.sub(ks, (S, ks, sl)), eng="pool")
        for c in range(4):
            P.tt(g.sub(c, (S, c, sl)), it.sub(2 + c, (S, 2 + c, sl)), it.sub(6 + c, (S, 6 + c, sl)), ALU.add)
            P.act(sz.sub(c, (S, c, sl)), it.sub(10 + c, (S, 10 + c, sl)), AF.Silu)
            P.tt(g.sub(c, (S, c, sl)), g.sub(c, (S, c, sl)), sz.sub(c, (S, c, sl)), ALU.mult)
            P.tt(gsq.sub(c, (S, c, sl)), g.sub(c, (S, c, sl)), g.sub(c, (S, c, sl)), ALU.mult, eng="pool")
        for gi in range(2):
            for j in range(2):
                c = gi * 2 + j
                P.mm(psg[gi][:, sl], ones[:], gsq.sub(c, (S, c, sl)), start=(j == 0), stop=(j == 1))
            P.act(rstdg.sub(gi, (S, gi, sl)), psg[gi][:, sl], AF.Sqrt, bias=EPS, scale=1.0 / 256)
            P.gen("dve", lambda e, gi=gi, sl=sl: e.reciprocal(rstdg.t[:, gi, sl], rstdg.t[:, gi, sl]),
                  [rstdg.sub(gi, (S, gi, sl))], [rstdg.sub(gi, (S, gi, sl))])
            for j in range(2):
                c = gi * 2 + j
                P.stt(mix.sub(2 + c, (S, 2 + c, sl)), g.sub(c, (S, c, sl)), prm[:, c:c + 1],
                      rstdg.sub(gi, (S, gi, sl)), ALU.mult, ALU.mult)
        g1o = 4 if bi == 0 else 12
        for j in range(8):
            ps = psm[mi % 3]
            mi += 1
            for k in range(8):
                P.mm(ps[:, sl], W.sub(k, (S, k, slice(j * 128, (j + 1) * 128))), mix.sub(k, (S, k, sl)),
                     start=(k == 0), stop=(k == 7))
            P.act(r.sub(j, (S, j, sl)), ps[:, sl], AF.Identity, scale=prm[:, g1o + j:g1o + j + 1])
            P.stt(r.sub(j, (S, j, sl)), xt.sub(j, (S, j, sl)), ALPHA, r.sub(j, (S, j, sl)), ALU.mult, ALU.add)
        _ln_block(P, _AllView(r), n, _Cols(prm, 20), _Cols(prm, 28), o, ones, ps1, ps2, rc, sq, rstd)
        for k in range(8):
            P.dma(out[k * 128:(k + 1) * 128, n0:n1], o.sub(k, (S, k, sl)), q="sp" if k % 2 == 0 else "act")
    P.finish()
    return nc


class _AllView:
    def __init__(self, b):
        self.b = b

    def __getitem__(self, idx):
        return self.b.all(idx)


class _Cols:
    def __init__(self, b, off):
        self.b = b
        self.off = off

    def __getitem__(self, idx):
        p, c = idx
        return self.b[p, slice(c.start + self.off, c.stop + self.off)]


def run_post1(mix_rows, xT_ctx, xT_lat, mod, layer, inp):
    ml = mod[0, layer].reshape(6, 1024)
    mc = mod[1, layer].reshape(6, 1024)
    pr = np.concatenate([_fm(inp["ssd_norm_w"][layer]), _fm(mc[2]), _fm(ml[2]), _fm(inp["ln1_g"][layer]),
                         _fm(inp["ln1_b"][layer])], axis=1)
    ms = tok_shard(mix_rows[:, :LC], mix_rows[:, LC:])
    xs = tok_shard(xT_ctx, xT_lat)
    maps = [{"mixin": ms[c], "xT": xs[c], "wo": inp["w_out"][layer], "pr": pr} for c in range(NCORE)]
    res = _run("post1", build_post1, maps)
    return tok_unshard([r["out"] for r in res])


FB = 256
NFB = NLT // FB
FFN_W = NCX + 2 + NLT + 2
FFN_BLKS = [(0, NCX, 0)] + [(NCX + 2 + FB * b, FB, NCX + FB * b) for b in range(NFB)]
DFF = 2816


def build_ffn():
    nc = bass.Bass("TRN2", target_bir_lowering=False)
    x1 = _dram(nc, "x1", [1024, FFN_W], F32)
    mk = _dram(nc, "mk", [128, 2 * (NFB + 1)], F32)
    pr = _dram(nc, "pr", [128, 64], F32)
    cw = _dram(nc, "cw", [128, 132], F32)
    cb = _dram(nc, "cb", [128, 44], F32)
    wu = _dram(nc, "wu", [1024, 2 * DFF], F32)
    wd = _dram(nc, "wd", [DFF, 1024], F32)
    out = _dram(nc, "out", [1024, NTK], F32, out=True)
    P = Prog(nc)
    S = slice(None)
    prm = P.sb([128, 64], F32)
    prm1 = P.sb([128, 64], F32)
    mkt = P.sb([128, 2 * (NFB + 1)], F32)
    cwt = P.sb([128, 132], F32)
    cbt = P.sb([128, 44], F32)
    ones = P.sb([128, 128], F32)
    WU = P.sb([128, 8, 2 * DFF], BF16)
    WD = P.sb([128, 22, 1024], BF16)
    P.dma(prm[:], pr)
    P.dma(mkt[:], mk)
    P.dma(cwt[:], cw)
    P.dma(cbt[:], cb)
    P.memset(ones[:], 1.0)
    P.ts(prm1[:], prm[:], 1.0, None, ALU.add)
    for jj in range(4):
        for k in range(8):
            c0 = jj * 1408
            P.dma(WU.sub((k, jj), (S, k, slice(c0, c0 + 1408))), wu[k * 128:(k + 1) * 128, c0:c0 + 1408], q="pool")
    for j in range(22):
        P.dma(WD.sub(j, (S, j)), wd[j * 128:(j + 1) * 128, :], q="pool")

    def wu_view(k, c0):
        jj = c0 // 1408
        jj2 = (c0 + 127) // 1408
        trks = [WU.subs[(k, jj)]] + ([WU.subs[(k, jj2)]] if jj2 != jj else [])
        return V(WU.t[:, k, c0:c0 + 128], trks)

    WD_ = FB + 2
    xb = [P.sb([128, 8, WD_], F32) for _ in range(2)]
    h2 = P.sb([128, 8, WD_], BF16)
    u = [P.sb([128, WD_], F32) for _ in range(4)]
    cc = [P.sb([128, FB], F32) for _ in range(4)]
    sg = [P.sb([128, FB], F32) for _ in range(2)]
    a = P.sb([128, 22, FB], BF16)
    r = P.sb([128, 8, FB], F32)
    sq = [P.sb([128, 8, FB], F32) for _ in range(2)]
    rstd = P.sb([128, FB], F32)
    psu = [P.ps([128, 512], F32) for _ in range(4)]
    psd = [P.ps([128, 512], F32) for _ in range(2)]
    ps1 = P.ps([128, 512], F32)
    ps2 = P.ps([128, 512], F32)
    ui = 0
    di = 0
    for bi, (c0, n, o0) in enumerate(FFN_BLKS):
        wdt = n + 2
        xt = xb[bi % 2]
        po = 0 if bi == 0 else 24
        for k in range(8):
            P.dma(xt.sub(k, (S, k, slice(0, wdt))), x1[k * 128:(k + 1) * 128, c0:c0 + wdt],
                  q="sp" if k % 2 == 0 else "act")
        for k in range(8):
            P.act(h2.sub(k, (S, k, slice(0, wdt))), xt.sub(k, (S, k, slice(0, wdt))), AF.Identity,
                  scale=prm1[:, po + k:po + k + 1], bias=prm[:, po + 8 + k:po + 9 + k])
        for j in range(22):
            cs = []
            for wi in range(2):
                ch = wi * 22 + j
                col0 = ch * 128
                ps = psu[ui % 4]
                ub = u[ui % 4]
                cb_ = cc[ui % 4]
                ui += 1
                for k in range(8):
                    P.mm(ps[:, 0:wdt], wu_view(k, col0), h2.sub(k, (S, k, slice(0, wdt))), start=(k == 0), stop=(k == 7))
                P.copy(ub[:, 0:wdt], ps[:, 0:wdt], eng="act")
                P.ts(ub[:, 0:1], ub[:, 0:1], mkt[:, 2 * bi:2 * bi + 1], None, ALU.mult)
                P.ts(ub[:, wdt - 1:wdt], ub[:, wdt - 1:wdt], mkt[:, 2 * bi + 1:2 * bi + 2], None, ALU.mult)
                P.ts(cb_[:, 0:n], ub[:, 1:n + 1], cwt[:, ch * 3 + 1:ch * 3 + 2], cbt[:, ch:ch + 1], ALU.mult, ALU.add)
                P.stt(cb_[:, 0:n], ub[:, 0:n], cwt[:, ch * 3:ch * 3 + 1], cb_[:, 0:n], ALU.mult, ALU.add)
                P.stt(cb_[:, 0:n], ub[:, 2:n + 2], cwt[:, ch * 3 + 2:ch * 3 + 3], cb_[:, 0:n], ALU.mult, ALU.add)
                cs.append(cb_)
            sgt = sg[j % 2]
            P.act(sgt[:, 0:n], cs[0][:, 0:n], AF.Silu)
            P.tt(a.sub(j, (S, j, slice(0, n))), sgt[:, 0:n], cs[1][:, 0:n], ALU.mult, eng="pool")
        for i in range(8):
            ps = psd[di % 2]
            di += 1
            for j in range(22):
                P.mm(ps[:, 0:n], WD.sub(j, (S, j, slice(i * 128, (i + 1) * 128))), a.sub(j, (S, j, slice(0, n))),
                     start=(j == 0), stop=(j == 21))
            P.act(r.sub(i, (S, i, slice(0, n))), ps[:, 0:n], AF.Identity, scale=prm[:, po + 16 + i:po + 17 + i])
            P.stt(r.sub(i, (S, i, slice(0, n))), xt.sub(i, (S, i, slice(1, n + 1))), ALPHA, r.sub(i, (S, i, slice(0, n))),
                  ALU.mult, ALU.add)
        o = sq[bi % 2]
        _ln_block(P, _AllView(r), n, _Cols(prm, 48), _Cols(prm, 56), o, ones, ps1, ps2, r, o, rstd)
        for k in range(8):
            P.dma(out[k * 128:(k + 1) * 128, o0:o0 + n], o.sub(k, (S, k, slice(0, n))), q="sp" if k % 2 == 0 else "act")
    P.finish()
    return nc


def run_ffn(x1T, mod, layer, inp):
    ml = mod[0, layer].reshape(6, 1024)
    mc = mod[1, layer].reshape(6, 1024)
    pr = np.concatenate([_fm(mc[4]), _fm(mc[3]), _fm(mc[5]), _fm(ml[4]), _fm(ml[3]), _fm(ml[5]),
                         _fm(inp["ln2_g"][layer]), _fm(inp["ln2_b"][layer])], axis=1)
    cw = np.ascontiguousarray(inp["ffn_conv_w"][layer].T.reshape(44, 128, 3).transpose(1, 0, 2).reshape(128, 132))
    cb = _fm(inp["ffn_conv_b"][layer])
    z = np.zeros((1024, 1), np.float32)
    xc = np.concatenate([z, x1T[:, :LC], z], axis=1)
    xl = np.concatenate([z, x1T[:, LC:], z], axis=1)
    maps = []
    for c in range(NCORE):
        xin = np.ascontiguousarray(np.concatenate([xc[:, c * NCX:c * NCX + NCX + 2], xl[:, c * NLT:c * NLT + NLT + 2]], axis=1))
        mk = np.ones((128, 2 * (NFB + 1)), np.float32)
        if c == 0:
            mk[:, 0] = 0.0
            mk[:, 2] = 0.0
        if c == NCORE - 1:
            mk[:, 1] = 0.0
            mk[:, 2 * NFB + 1] = 0.0
        maps.append({"x1": xin, "mk": mk, "pr": pr, "cw": cw, "cb": cb, "wu": inp["ffn_w_up"][layer],
                     "wd": inp["ffn_w_down"][layer]})
    res = _run("ffn", build_ffn, maps)
    return tok_unshard([r["out"] for r in res])


QH = L // 2
QC = LC // 2
NKT = TT // 128


def build_attn():
    nc = bass.Bass("TRN2", target_bir_lowering=False)
    qT = _dram(nc, "qT", [64, QC + QH], F32)
    kT = _dram(nc, "kT", [64, TT], F32)
    v = _dram(nc, "v", [TT, 64], F32)
    cosk = _dram(nc, "cosk", [64, L], F32)
    sink = _dram(nc, "sink", [64, L], F32)
    cosq = _dram(nc, "cosq", [64, QH], F32)
    sinq = _dram(nc, "sinq", [64, QH], F32)
    cst = _dram(nc, "cst", [64, 66], F32)
    out = _dram(nc, "out", [64, QC + QH], F32, out=True)
    P = Prog(nc)
    S = slice(None)
    ct = P.sb([64, 66], F32)
    ones = P.sb([128, 64], F32)
    KT = P.sb([64, TT], BF16)
    QT = P.sb([64, QC + QH], BF16)
    VA = P.sb([128, NKT, 65], BF16)
    P.dma(ct[:], cst)
    P.memset(ones[:], 1.0)
    vv = v.rearrange("(t p) d -> p t d", p=128)
    for i in range(5):
        P.dma(VA.sub(i, (S, slice(i * 26, (i + 1) * 26), slice(0, 64))), vv[:, i * 26:(i + 1) * 26, :], q="pool")
    P.memset(VA.sub("one", (S, S, slice(64, 65))), 1.0)
    NPB = 3
    xin = [P.sb([64, 512], F32) for _ in range(NPB)]
    cin = [P.sb([64, 512], F32) for _ in range(NPB)]
    sin_ = [P.sb([64, 512], F32) for _ in range(NPB)]
    sqs = [P.sb([64, 512], F32) for _ in range(NPB)]
    rstds = [P.sb([64, 512], F32) for _ in range(NPB)]
    xns = [P.sb([64, 512], F32) for _ in range(NPB)]
    t1s = [P.sb([64, 512], F32) for _ in range(NPB)]
    t2s = [P.sb([64, 512], F32) for _ in range(NPB)]
    pss_l = [P.ps([64, 512], F32) for _ in range(2)]
    psr_l = [P.ps([64, 512], F32) for _ in range(2)]
    pss = pss_l[0]
    psr = psr_l[0]
    preps = []

    def prep_a(idx, src, c0, n, gcol, tabs, dst, d0):
        i = idx % NPB
        x = xin[i]
        P.dma(x[:, 0:n], src[:, c0:c0 + n], q="sp")
        if tabs is not None:
            P.dma(cin[i][:, 0:n], tabs[0][:, tabs[2]:tabs[2] + n], q="act")
            P.dma(sin_[i][:, 0:n], tabs[1][:, tabs[2]:tabs[2] + n], q="act")
        ps = pss_l[idx % 2]
        rstd = rstds[i]
        P.act(sqs[i][:, 0:n], x[:, 0:n], AF.Square)
        P.mm(ps[0:64, 0:n], ones[0:64, :], sqs[i][:, 0:n])
        P.act(rstd[:, 0:n], ps[0:64, 0:n], AF.Sqrt, bias=EPS, scale=1.0 / 64)
        P.gen("dve", lambda e, rstd=rstd, n=n: e.reciprocal(rstd.t[:, 0:n], rstd.t[:, 0:n]), [rstd[:, 0:n]], [rstd[:, 0:n]])

    def prep_b(idx, src, c0, n, gcol, tabs, dst, d0):
        i = idx % NPB
        x = xin[i]
        rstd = rstds[i]
        if tabs is None:
            P.stt(dst[:, d0:d0 + n], x[:, 0:n], ct[:, gcol:gcol + 1], rstd[:, 0:n], ALU.mult, ALU.mult)
            return
        xn, t1, t2 = xns[i], t1s[i], t2s[i]
        pr_ = psr_l[idx % 2]
        P.stt(xn[:, 0:n], x[:, 0:n], ct[:, gcol:gcol + 1], rstd[:, 0:n], ALU.mult, ALU.mult)
        P.mm(pr_[0:64, 0:n], ct[:, 0:64], xn[:, 0:n])
        P.tt(t1[:, 0:n], xn[:, 0:n], cin[i][:, 0:n], ALU.mult, eng="pool")
        P.tt(t2[:, 0:n], pr_[0:64, 0:n], sin_[i][:, 0:n], ALU.mult)
        P.tt(dst[:, d0:d0 + n], t1[:, 0:n], t2[:, 0:n], ALU.add, eng="pool")

    preps.append((kT, 0, LC, 65, None, KT, 0))
    for i in range(L // 512):
        preps.append((kT, LC + 512 * i, 512, 65, (cosk, sink, 512 * i), KT, LC + 512 * i))
    preps.append((qT, 0, QC, 64, None, QT, 0))
    for i in range(QH // 512):
        preps.append((qT, QC + 512 * i, 512, 64, (cosq, sinq, 512 * i), QT, QC + 512 * i))
    for t_ in range(len(preps) + 1):
        if t_ < len(preps):
            prep_a(t_, *preps[t_])
        if t_ >= 1:
            prep_b(t_ - 1, *preps[t_ - 1])

    psS = [P.ps([128, 512], F32) for _ in range(2)] + [pss_l[0], pss_l[1]]
    psO = [P.ps([128, 512], F32) for _ in range(2)]
    psB = psr
    NS = len(psS)
    LOOK = 2
    pT = [P.sb([128, 512], BF16) for _ in range(NS)]
    osb = [P.sb([65, 512], F32) for _ in range(2)]
    yo = [P.sb([64, 512], F32) for _ in range(2)]
    blocks = [(0, QC, 0, 2)] + [(QC + 512 * i, 512, 0, NKT) for i in range(QH // 512)]
    work = []
    for bi, (q0, n, k0, k1) in enumerate(blocks):
        for kt in range(k0, k1):
            work.append((bi, q0, n, kt, kt == k0, kt == k1 - 1))

    def emit_s(w, si):
        bi, q0, n, kt, first, last = w
        P.mm(psS[si % NS][:, 0:n], KT[:, kt * 128:(kt + 1) * 128], QT[:, q0:q0 + n])

    for si in range(min(LOOK, len(work))):
        emit_s(work[si], si)
    for si, w in enumerate(work):
        bi, q0, n, kt, first, last = w
        if si + LOOK < len(work):
            emit_s(work[si + LOOK], si + LOOK)
        ps = psS[si % NS]
        pt = pT[si % NS]
        po = psO[bi % 2]
        P.act(pt[:, 0:n], ps[:, 0:n], AF.Exp, scale=0.125)
        P.mm(po[0:65, 0:n], VA.all((S, kt, slice(0, 65))), pt[:, 0:n], start=first, stop=last)
        if last:
            ob = osb[bi % 2]
            y = yo[bi % 2]
            P.copy(ob[:, 0:n], po[0:65, 0:n], eng="dve")
            P.gen("dve", lambda e, ob=ob, n=n: e.reciprocal(ob.t[64:65, 0:n], ob.t[64:65, 0:n]), [ob[64:65, 0:n]], [ob[64:65, 0:n]])
            P.mm(psB[0:64, 0:n], ones[64:65, :], ob[64:65, 0:n])
            P.tt(y[:, 0:n], ob[0:64, 0:n], psB[0:64, 0:n], ALU.mult)
            P.dma(out[:, q0:q0 + n], y[:, 0:n], q="sp")
    P.finish()
    return nc


def rope_tables():
    rows = L // 64
    row = np.repeat(np.arange(rows, dtype=np.float32), 64)
    col = np.tile(np.arange(64, dtype=np.float32), rows)
    inv = (np.float32(10000.0) ** (-np.arange(16, dtype=np.float32) / np.float32(16))).astype(np.float32)
    ang = np.concatenate([row[:, None] * inv, col[:, None] * inv], axis=-1).astype(np.float32)
    cos = np.cos(ang).astype(np.float32).T
    sin = np.sin(ang).astype(np.float32).T
    return (np.ascontiguousarray(np.concatenate([cos, cos], axis=0)),
            np.ascontiguousarray(np.concatenate([sin, sin], axis=0)))


def run_attn(proj, layer, inp):
    cosf, sinf = rope_tables()
    RT = np.zeros((64, 64), np.float32)
    for dp in range(32):
        RT[dp + 32, dp] = -1.0
        RT[dp, dp + 32] = 1.0
    cst = np.concatenate([RT, inp["attn_q_norm"][layer][:, None], inp["attn_k_norm"][layer][:, None]], axis=1)
    cst = np.ascontiguousarray(cst.astype(np.float32))
    maps = []
    for c in range(NCORE):
        g, h, half = c // 4, c // 2, c % 2
        qrows = proj[2320 + 64 * h:2320 + 64 * (h + 1)]
        qT = np.ascontiguousarray(np.concatenate([qrows[:, half * QC:(half + 1) * QC],
                                                  qrows[:, LC + half * QH:LC + (half + 1) * QH]], axis=1))
        kT = np.ascontiguousarray(proj[2576 + 64 * g:2576 + 64 * (g + 1)])
        v = np.ascontiguousarray(proj[2704 + 64 * g:2704 + 64 * (g + 1)].T)
        maps.append({"qT": qT, "kT": kT, "v": v, "cosk": cosf, "sink": sinf,
                     "cosq": np.ascontiguousarray(cosf[:, half * QH:(half + 1) * QH]),
                     "sinq": np.ascontiguousarray(sinf[:, half * QH:(half + 1) * QH]), "cst": cst})
    res = _run("attn", build_attn, maps)
    y = np.zeros((256, TT), np.float32)
    for c in range(NCORE):
        h, half = c // 2, c % 2
        o = res[c]["out"]
        y[64 * h:64 * (h + 1), half * QC:(half + 1) * QC] = o[:, :QC]
        y[64 * h:64 * (h + 1), LC + half * QH:LC + (half + 1) * QH] = o[:, QC:]
    return y


NCH = TT // 128
SSD_W = LC + 2 + L + 2
SB = 1024
SSD_STAGE = 9
SSD_BLKS = [(0, LC, 0)] + [(LC + 2 + SB * b, SB, LC + SB * b) for b in range(L // SB)]


def build_ssd():
    nc = bass.Bass("TRN2", target_bir_lowering=False)
    xr = _dram(nc, "xr", [2, 64, SSD_W], F32)
    br = _dram(nc, "br", [2, 128, SSD_W], F32)
    cr = _dram(nc, "cr", [2, 128, SSD_W], F32)
    dtr = _dram(nc, "dtr", [2, 128, NCH], F32)
    scl = _dram(nc, "scl", [2, 128, 4], F32)
    cwx = _dram(nc, "cwx", [2, 64, 4], F32)
    cwb = _dram(nc, "cwb", [2, 128, 4], F32)
    cwc = _dram(nc, "cwc", [2, 128, 4], F32)
    cst = _dram(nc, "cst", [128, 384], F32)
    out = _dram(nc, "out", [2, TT, 64], F32, out=True)
    P = Prog(nc)
    S = slice(None)
    ct = P.sb([128, 384], F32)
    ones = P.sb([128, 128], F32)
    identb = P.sb([128, 128], BF16)
    P.dma(ct[:], cst)
    P.memset(ones[:], 1.0)
    P.copy(identb[:], ct[:, 256:384])
    tri = ct[:, 0:128]
    negm = ct[:, 128:256]
    ident = ct[:, 256:384]
    ps_big = P.ps([128, 512], F32)
    ps_trb = P.ps([128, 128], BF16)
    ps_trx = P.ps([128, 64], F32)
    ps_scs = [P.ps([128, 128], F32), ps_big]
    ps_segs = [P.ps([128, 128], F32), P.ps([128, 128], F32)]
    ps_ys = P.ps([128, 128], F32)
    ps_i = P.ps([128, 64], F32)
    negmb = P.sb([128, 128], BF16)
    P.copy(negmb[:], negm)
    W2 = SB + 2

    class Job:
        pass

    def mk_job(j):
        J = Job()
        J.j = j
        for nm in ("dtt", "e1", "dt", "dta", "acum", "nacum", "ea", "dch", "wv"):
            setattr(J, nm, P.sb([128, NCH], F32))
        J.sc4 = P.sb([128, 4], F32)
        J.at = P.sb([128, 1], F32)
        J.cx = P.sb([64, 4], F32)
        J.cb = P.sb([128, 4], F32)
        J.cc = P.sb([128, 4], F32)
        J.xraw = P.sb([64, W2], F32)
        J.braw = P.sb([128, W2], F32)
        J.craw = P.sb([128, W2], F32)
        J.tx = P.sb([64, SB], F32)
        J.tb = P.sb([128, SB], F32)
        J.tc_ = P.sb([128, SB], F32)
        J.xT = P.sb([64, SB], F32)
        J.BT = P.sb([128, SB], BF16)
        J.CT = P.sb([128, SB], BF16)
        J.Bc = P.sb([128, 128], BF16)
        J.xdt = P.sb([128, 64], BF16)
        J.xw = P.sb([128, 64], BF16)
        J.xD = P.sb([128, 64], F32)
        J.dg = P.sb([128, 128], F32)
        J.dec = P.sb([128, 128], F32)
        J.MT = P.sb([128, 128], BF16)
        J.yi = P.sb([128, 64], F32)
        J.H = P.sb([128, 64], F32)
        J.Hb = P.sb([128, 64], BF16)
        J.ybuf = [P.sb([128, SB // 128, 64], F32) for _ in range(2)]
        J.yb_i = 0
        P.dma(J.dtt[:], dtr[j])
        P.dma(J.sc4[:], scl[j])
        P.dma(J.cx[:], cwx[j])
        P.dma(J.cb[:], cwb[j])
        P.dma(J.cc[:], cwc[j])
        P.act(J.e1[:], J.dtt[:], AF.Exp, bias=J.sc4[:, 0:1])
        P.act(J.dt[:], J.e1[:], AF.Ln, bias=1.0)
        P.act(J.at[:], J.sc4[:, 1:2], AF.Exp)
        P.ts(J.at[:], J.at[:], -1.0, None, ALU.mult)
        P.ts(J.dta[:], J.dt[:], J.at[:, 0:1], None, ALU.mult)
        P.mm(ps_big[:, 0:NCH], tri, J.dta[:])
        P.copy(J.acum[:], ps_big[:, 0:NCH])
        P.ts(J.nacum[:], J.acum[:], -1.0, None, ALU.mult)
        P.act(J.ea[:], J.acum[:], AF.Exp)
        P.mm(ps_big[:, 256:256 + NCH], ones[:], J.dta[:])
        P.act(J.dch[:], ps_big[:, 256:256 + NCH], AF.Exp)
        P.tt(J.wv[:], ps_big[:, 256:256 + NCH], J.acum[:], ALU.subtract)
        P.act(J.wv[:], J.wv[:], AF.Exp)
        P.tt(J.wv[:], J.wv[:], J.dt[:], ALU.mult)
        P.memset(J.H[:], 0.0)
        P.memset(J.Hb[:], 0.0)
        return J

    def conv_block(J, c0, n):
        j = J.j
        P.dma(J.xraw[:, 0:n + 2], xr[j, :, c0:c0 + n + 2], q="sp")
        P.dma(J.braw[:, 0:n + 2], br[j, :, c0:c0 + n + 2], q="act")
        P.dma(J.craw[:, 0:n + 2], cr[j, :, c0:c0 + n + 2], q="sp")
        for (raw, tmp, w4, dst) in ((J.xraw, J.tx, J.cx, J.xT), (J.braw, J.tb, J.cb, J.BT), (J.craw, J.tc_, J.cc, J.CT)):
            P.ts(tmp[:, 0:n], raw[:, 1:n + 1], w4[:, 1:2], w4[:, 3:4], ALU.mult, ALU.add)
            P.stt(tmp[:, 0:n], raw[:, 0:n], w4[:, 0:1], tmp[:, 0:n], ALU.mult, ALU.add)
            P.stt(tmp[:, 0:n], raw[:, 2:n + 2], w4[:, 2:3], tmp[:, 0:n], ALU.mult, ALU.add)
            P.act(dst[:, 0:n], tmp[:, 0:n], AF.Silu)

    def chunk_a(J, yb, ci, c):
        cs = slice(ci * 128, (ci + 1) * 128)
        ps_sc = ps_scs[J.j]
        ps_seg = ps_segs[J.j]
        P.tr(ps_trb[:, 0:128], J.BT[:, cs], identb[:])
        P.copy(J.Bc[:], ps_trb[:, 0:128], eng="act")
        P.tr(ps_trx[:, 0:64], J.xT[:, cs], ident[0:64, 0:64])
        P.ts(J.xdt[:], ps_trx[:, 0:64], J.dt[:, c:c + 1], None, ALU.mult)
        P.ts(J.xw[:], ps_trx[:, 0:64], J.wv[:, c:c + 1], None, ALU.mult)
        P.ts(J.xD[:], ps_trx[:, 0:64], J.sc4[:, 2:3], None, ALU.mult)
        P.mm(ps_sc[:, 0:128], J.BT[:, cs], J.CT[:, cs])
        P.ts(J.dg[:], ident, J.acum[:, c:c + 1], 0.0, ALU.mult, ALU.add, eng="pool")
        P.mm(ps_seg[:, 0:128], ones[:], J.dg[:], start=True, stop=False)
        P.mm(ps_seg[:, 0:128], identb[:], negmb[:], start=False, stop=True)
        P.act(J.dec[:], ps_seg[:, 0:128], AF.Exp, bias=J.nacum[:, c:c + 1])
        P.tt(J.MT[:], ps_sc[:, 0:128], J.dec[:], ALU.mult)

    def chunk_b(J, yb, ci, c):
        cs = slice(ci * 128, (ci + 1) * 128)
        ps_y = ps_ys.sub("y", (S, slice(0, 64)))
        ps_s = ps_ys.sub("s", (S, slice(64, 128)))
        P.mm(ps_y, J.MT[:], J.xdt[:])
        P.mm(ps_i[:, 0:64], J.CT[:, cs], J.Hb[:])
        P.mm(ps_s, J.Bc[:], J.xw[:])
        P.act(J.yi[:], ps_y, AF.Identity)
        P.tt(J.yi[:], J.yi[:], J.xD[:], ALU.add, eng="pool")
        P.stt(yb[:, ci, :], ps_i[:, 0:64], J.ea[:, c:c + 1], J.yi[:], ALU.mult, ALU.add)
        P.stt(J.H[:], J.H[:], J.dch[:, c:c + 1], ps_s, ALU.mult, ALU.add)
        P.copy(J.Hb[:], J.H[:], eng="act")

    jobs = [mk_job(0), mk_job(1)]
    for (c0, n, t0) in SSD_BLKS:
        ybs = []
        for J in jobs:
            conv_block(J, c0, n)
            ybs.append(J.ybuf[J.yb_i % 2])
            J.yb_i += 1
        for ci in range(n // 128):
            for J, yb in zip(jobs, ybs):
                chunk_a(J, yb, ci, t0 // 128 + ci)
            for J, yb in zip(jobs, ybs):
                chunk_b(J, yb, ci, t0 // 128 + ci)
        for J, yb in zip(jobs, ybs):
            P.dma(out[J.j, t0:t0 + n, :].rearrange("(c p) d -> p c d", p=128), yb[:, 0:n // 128, :], q="act")
    P.finish()
    return nc


def _flipseq(a):
    return np.concatenate([a[..., :LC][..., ::-1], a[..., LC:][..., ::-1]], axis=-1)


def _padseq(a):
    z = np.zeros(a.shape[:-1] + (1,), np.float32)
    return np.concatenate([z, a[..., :LC], z, z, a[..., LC:], z], axis=-1)


def ssd_consts():
    s = np.arange(128)
    tri = (s[:, None] <= s[None, :]).astype(np.float32)
    negm = np.where(s[:, None] > s[None, :], -30000.0, 0.0).astype(np.float32)
    return np.ascontiguousarray(np.concatenate([tri, negm, np.eye(128, dtype=np.float32)], axis=1))


def run_ssd(proj, layer, inp):
    cst = ssd_consts()
    cw = inp["ssd_conv_w"][layer]
    cbias = inp["ssd_conv_b"][layer]
    maps = []
    for hd in range(NCORE):
        g = hd // 4
        rows = {"x": slice(1280 + 64 * hd, 1280 + 64 * (hd + 1)),
                "b": slice(1792 + 128 * g, 1792 + 128 * (g + 1)),
                "c": slice(2048 + 128 * g, 2048 + 128 * (g + 1))}
        chs = {"x": slice(64 * hd, 64 * (hd + 1)), "b": slice(512 + 128 * g, 512 + 128 * (g + 1)),
               "c": slice(768 + 128 * g, 768 + 128 * (g + 1))}
        m = {}
        for nm, key in (("xr", "x"), ("br", "b"), ("cr", "c")):
            a = proj[rows[key]]
            m[nm] = np.ascontiguousarray(np.stack([_padseq(a), _padseq(_flipseq(a))]))
        for nm, key in (("cwx", "x"), ("cwb", "b"), ("cwc", "c")):
            w = cw[:, chs[key]]
            b = cbias[chs[key]]
            f = np.stack([w[0], w[1], w[2], b], axis=1)
            bk = np.stack([w[2], w[1], w[0], b], axis=1)
            m[nm] = np.ascontiguousarray(np.stack([f, bk]).astype(np.float32))
        dts = []
        scl = []
        for dr in range(2):
            raw = proj[2304 + dr * 8 + hd]
            if dr == 1:
                raw = _flipseq(raw)
            dts.append(raw.reshape(NCH, 128).T)
            s4 = np.zeros((128, 4), np.float32)
            s4[:, 0] = inp["ssd_dt_bias"][layer][dr, hd]
            s4[:, 1] = inp["ssd_a_log"][layer][dr, hd]
            s4[:, 2] = inp["ssd_d"][layer][hd] if dr == 0 else 0.0
            scl.append(s4)
        m["dtr"] = np.ascontiguousarray(np.stack(dts).astype(np.float32))
        m["scl"] = np.ascontiguousarray(np.stack(scl))
        m["cst"] = cst
        maps.append(m)
    res = _run("ssd", build_ssd, maps)
    yf = np.concatenate([res[hd]["out"][0].T for hd in range(NCORE)], axis=0)
    yb = np.concatenate([_flipseq(res[hd]["out"][1].T) for hd in range(NCORE)], axis=0)
    return np.ascontiguousarray(yf), np.ascontiguousarray(yb)


HN = 2 * L
HCH = 32
HY_SKIP = {}
PI = float(np.pi)


def hyena_consts():
    n1 = np.arange(128)
    f1 = np.arange(128)
    n2 = np.arange(256)
    f2 = np.arange(256)
    a1 = 2 * np.pi * np.outer(n1, f1) / 128
    D1 = np.concatenate([np.cos(a1), -np.sin(a1)], axis=1)
    at = 2 * np.pi * np.outer(n2, f1) / HN
    TwC, TwS = np.cos(at), np.sin(at)
    a3 = 2 * np.pi * np.outer(n2, f2) / 256
    C3, S3 = np.cos(a3), np.sin(a3)
    E1 = np.concatenate([C3.T, S3.T], axis=1)
    E2 = np.concatenate([-S3.T, C3.T], axis=1)
    F1c = np.cos(a1.T)[:, :64] / HN
    F1s = -np.sin(a1.T)[:, :64] / HN
    c = {}
    c["d1"] = np.stack([D1[0:64], D1[64:128]])
    c["tw"] = np.stack([np.stack([np.concatenate([TwC[h * 128:(h + 1) * 128]] * 2, axis=1),
                                  np.concatenate([TwS[h * 128:(h + 1) * 128]] * 2, axis=1)]) for h in range(2)])
    c["tw"] = c["tw"].transpose(2, 0, 1, 3)
    c["c3"] = np.stack([np.stack([C3[h * 128:(h + 1) * 128], S3[h * 128:(h + 1) * 128], -S3[h * 128:(h + 1) * 128]])
                        for h in range(2)]).transpose(2, 0, 1, 3)
    c["e"] = np.stack([np.stack([E1[g * 128:(g + 1) * 128], E2[g * 128:(g + 1) * 128]]) for g in range(2)]
                      ).transpose(2, 0, 1, 3)
    c["tw2"] = np.stack([np.concatenate([TwC.T, TwC.T], axis=1), np.concatenate([TwS.T, TwS.T], axis=1)]
                        ).transpose(1, 0, 2)
    c["f1"] = np.stack([F1c, F1s]).transpose(1, 0, 2)
    return {k: np.ascontiguousarray(v.astype(np.float32)) for k, v in c.items()}


def _hy_feats(n, pos):
    t = np.linspace(0.0, 1.0, n, dtype=np.float32)
    w = ((2.0 * np.pi / n) * np.arange(n, dtype=np.float32)).astype(np.float32)
    f = np.linspace(1e-4, 15, 16, dtype=np.float32)[None, :]
    tt_ = t[pos][:, None]
    ww = w[pos][:, None]
    return np.concatenate([tt_, np.cos(f * ww), -np.sin(f * ww)], axis=-1).astype(np.float32)


def _hy_deltas():
    mn = np.log(1e-2) / 1.5
    mx = np.log(1e-2) / 0.3
    return np.abs(np.linspace(mn, mx, 256, dtype=np.float32))


def hyena_tables():
    n = np.arange(HN)
    t = np.where(n < L, n, HN - n)
    t = np.where(n == L, 0, t)
    feats = _hy_feats(L, t)
    featsP = feats.reshape(128, 256, 33).transpose(1, 0, 2).reshape(HN, 33).T
    tl = np.linspace(0.0, 1.0, L, dtype=np.float32)
    win = np.exp(-tl[t][:, None] * _hy_deltas()[None, :]).astype(np.float32)
    win[L] = 0.0
    win = win.reshape(2, 64, 256, 256)
    return np.ascontiguousarray(featsP.astype(np.float32)), win


def build_hyena(with_ctx):
    nc = bass.Bass("TRN2", target_bir_lowering=False)
    raw = _dram(nc, "raw", [3, HCH, 64, 258], F32)
    cwl = _dram(nc, "cwl", [64, 3 * HCH * 4], F32)
    skp = _dram(nc, "skp", [64, 2 * HCH], F32)
    w1 = _dram(nc, "w1", [33, 64], F32)
    w2 = _dram(nc, "w2", [64, 64], F32)
    w3s = _dram(nc, "w3s", [64, 4 * HCH], F32)
    fb = _dram(nc, "fb", [64, 3], F32)
    featsP = _dram(nc, "featsP", [33, HN], F32)
    win = _dram(nc, "win", [2, 64, 256, HCH], F32)
    d1 = _dram(nc, "d1", [2, 64, 256], F32)
    tw = _dram(nc, "tw", [128, 2, 2, 256], F32)
    c3 = _dram(nc, "c3", [128, 2, 3, 256], F32)
    ee = _dram(nc, "e", [128, 2, 2, 512], F32)
    tw2 = _dram(nc, "tw2", [128, 2, 512], F32)
    f1 = _dram(nc, "f1", [128, 2, 64], F32)
    out = _dram(nc, "out", [HCH, L], F32, out=True)
    if with_ctx:
        rawc = _dram(nc, "rawc", [3, HCH, 258], F32)
        cwc = _dram(nc, "cwc", [HCH, 12], F32)
        skc = _dram(nc, "skc", [HCH, 2], F32)
        featsC = _dram(nc, "featsC", [33, 256], F32)
        winC = _dram(nc, "winC", [HCH, 256], F32)
        outc = _dram(nc, "outc", [HCH, 256], F32, out=True)
    P = Prog(nc)
    S = slice(None)
    cw = P.sb([64, 3 * HCH * 4], F32)
    sk = P.sb([64, 2 * HCH], F32)
    W1 = P.sb([33, 64], F32)
    W2 = P.sb([64, 64], BF16)
    W3 = P.sb([64, 4 * HCH], BF16)
    FB = P.sb([64, 3], F32)
    FBB = P.sb([64, 2], F32)
    D1 = P.sb([64, 2, 256], BF16)
    TW = P.sb([128, 2, 2, 256], F32)
    C3 = P.sb([128, 2, 3, 256], BF16)
    EE = P.sb([128, 2, 2, 512], BF16)
    TW2 = P.sb([128, 2, 512], F32)
    F1 = P.sb([128, 2, 64], BF16)
    P.dma(cw[:], cwl)
    P.dma(sk[:], skp)
    P.dma(W1[:], w1)
    P.dma(FB[:], fb)
    P.dma(TW[:], tw)
    P.dma(TW2[:], tw2)
    P.dma(W2[:], w2, q="pool")
    P.dma(W3[:], w3s, q="pool")
    P.dma(D1[:], d1.rearrange("a p f -> p a f"), q="pool")
    P.dma(C3[:], c3, q="pool")
    P.dma(EE[:], ee, q="pool")
    P.dma(F1[:], f1, q="pool")
    P.ts(FBB[:], FB[:, 1:3], FB[:, 0:1], None, ALU.mult)
    banks = [P.ps([128, 512], F32) for _ in range(8)]

    NB3 = 3
    fts = [P.sb([33, 512], F32) for _ in range(NB3)]
    aas = [P.sb([64, 512], F32) for _ in range(NB3)]
    m1s = [P.sb([64, 512], F32) for _ in range(NB3)]
    m2s = [P.sb([64, 512], F32) for _ in range(NB3)]
    h1s = [P.sb([64, 512], BF16) for _ in range(NB3)]
    Gs = [P.sb([64, 512], BF16) for _ in range(NB3)]
    wt = [P.sb([64, 2, 32, HCH], F32)] * 2

    def wrap(i, n):
        a, m1, m2 = aas[i], m1s[i], m2s[i]
        P.ts(m2[:, 0:n], a[:, 0:n], PI, -1.0, ALU.is_gt, ALU.mult)
        P.stt(m1[:, 0:n], a[:, 0:n], -PI, m2[:, 0:n], ALU.is_lt, ALU.add)
        P.stt(a[:, 0:n], m1[:, 0:n], 2 * PI, a[:, 0:n], ALU.mult, ALU.add)

    def mlp_s1(i, ft, n, bank):
        P.mm(bank[0:64, 0:n], W1[:], ft)
        P.act(aas[i][:, 0:n], bank[0:64, 0:n], AF.Identity, scale=FB[:, 0:1], bias=FBB[:, 0:1])

    def mlp_s2(i, n, bank):
        wrap(i, n)
        P.act(h1s[i][:, 0:n], aas[i][:, 0:n], AF.Sin)
        P.mm(bank[0:64, 0:n], W2[:], h1s[i][:, 0:n])
        P.act(aas[i][:, 0:n], bank[0:64, 0:n], AF.Identity, scale=FB[:, 0:1], bias=FBB[:, 1:2])

    def mlp_s3(i, n, G):
        wrap(i, n)
        P.act(G[:, 0:n], aas[i][:, 0:n], AF.Sin)

    def mlp(ft, n, h1_unused, G):
        mlp_s1(0, ft, n, banks[0])
        mlp_s2(0, n, banks[1])
        mlp_s3(0, n, G)

    kf = P.sb([64, HCH, 256], BF16)
    kb = P.sb([64, HCH, 256], BF16)
    KH = P.sb([128, 2, 2, HCH * 128], BF16)
    z = P.sb([64, HCH, 256], F32)
    Bt = [P.sb([128, 2, 2, 512], BF16) for _ in range(2)]
    t12 = P.sb([128, 512], F32)
    t34 = P.sb([128, 512], F32)
    rawt = [P.sb([64, 258], F32) for _ in range(3)]
    vin = [P.sb([64, 4, 256], F32) for _ in range(2)]
    vb = [P.sb([64, 256], BF16) for _ in range(2)]
    Zt = P.sb([128, 2, 2, 512], BF16)
    pa = t12
    pb = t34
    Dt = [P.sb([128, 2, 512], BF16) for _ in range(2)]
    gate = [P.sb([64, 256], F32) for _ in range(2)]
    tq = [P.sb([64, 256], F32) for _ in range(2)]
    ot = [P.sb([64, 256], F32) for _ in range(2)]
    cnt = {"raw": 0, "g": 0, "vb": 0, "o": 0}

    def conv_row(w, ch, dst):
        r = rawt[cnt["raw"] % 3]
        cnt["raw"] += 1
        P.dma(r[:], raw[w, ch], q="sp" if cnt["raw"] % 2 == 0 else "act")
        o = (w * HCH + ch) * 4
        P.ts(dst, r[:, 1:257], cw[:, o + 1:o + 2], cw[:, o + 3:o + 4], ALU.mult, ALU.add)
        P.stt(dst, r[:, 0:256], cw[:, o:o + 1], dst, ALU.mult, ALU.add)
        P.stt(dst, r[:, 2:258], cw[:, o + 2:o + 3], dst, ALU.mult, ALU.add)

    def twiddle_fwd(psA, h, btile, ch4):
        P.tt(t12[:, 0:256], psA[:, 0:256], TW[:, h, 0, :], ALU.mult)
        P.tt(t34[:, 0:256], psA[:, 0:256], TW[:, h, 1, :], ALU.mult)
        cs = slice(ch4 * 128, (ch4 + 1) * 128)
        P.tt(btile.sub((h, 0, ch4), (S, h, 0, cs)), t12[:, 0:128], t34[:, 128:256], ALU.add)
        P.tt(btile.sub((h, 1, ch4), (S, h, 1, cs)), t12[:, 128:256], t34[:, 0:128], ALU.subtract, eng="pool")

    def step3(btile, evac):
        ball = _AllView(btile)
        for g in range(2):
            gs = slice(g * 128, (g + 1) * 128)
            for ri in range(2):
                ps = banks[2 + g * 2 + ri]
                terms = []
                for h in range(2):
                    if ri == 0:
                        terms += [(0, h, 0), (1, h, 1)]
                    else:
                        terms += [(0, h, 1), (2, h, 0)]
                for ti, (m, h, bri) in enumerate(terms):
                    P.mm(ps[:, :], C3[:, h, m, gs], ball[:, h, bri, :], start=(ti == 0), stop=(ti == 3))
                evac(g, ri, ps)

    for o in range(2):
        def fg_s1(blk):
            i = blk % NB3
            P.dma(fts[i][:], featsP[:, blk * 512:(blk + 1) * 512], q="sp")
            mlp_s1(i, fts[i][:], 512, banks[blk % 2])

        def fg_s2(blk):
            mlp_s2(blk % NB3, 512, banks[4 + blk % 2])

        def fg_s3(blk):
            i = blk % NB3
            G = Gs[i]
            mlp_s3(i, 512, G)
            w_ = wt[(blk // 8) % 2]
            if blk % 8 == 0:
                for half in range(2):
                    P.dma(w_.sub(half, (S, half)), win[half, :, blk * 4:blk * 4 + 32, :], q="act")
            for half in range(2):
                pk = banks[2 + half + 4 * (blk % 2)]
                wc = (o * 2 + half) * HCH
                for q in range(4):
                    P.mm(pk[0:64, q * HCH:(q + 1) * HCH], G[:, q * 128 + half * 64:q * 128 + half * 64 + 64],
                         W3[:, wc:wc + HCH])
                kt = kf if half == 0 else kb
                q0 = (blk % 8) * 4
                src = V(pk.t[0:64, 0:4 * HCH].rearrange("p (q c) -> p c q", q=4), [pk.trk], pk.bank)
                wv_ = V(w_.t[:, half, q0:q0 + 4, :].rearrange("p q c -> p c q"), [w_.subs[half]])
                P.tt(kt.sub(blk, (S, S, slice(blk * 4, blk * 4 + 4))), src, wv_, ALU.mult)

        nblk = 64 if not HY_SKIP.get('filt') else 0
        for t_ in range(nblk + 2):
            if t_ < nblk:
                fg_s1(t_)
            if 0 <= t_ - 1 < nblk:
                fg_s2(t_ - 1)
            if 0 <= t_ - 2 < nblk:
                fg_s3(t_ - 2)
        kfa = _AllView(kf)
        kba = _AllView(kb)
        for cg in range(HCH // 4 if not HY_SKIP.get('fdft') else 0):
            bt = Bt[cg % 2]
            for ch4 in range(4):
                ch = cg * 4 + ch4
                for h in range(2):
                    psA = banks[h]
                    hs = slice(h * 128, (h + 1) * 128)
                    P.mm(psA[:, 0:256], kfa[:, ch, hs], D1[:, 0, :], start=True, stop=False)
                    P.mm(psA[:, 0:256], kba[:, ch, hs], D1[:, 1, :], start=False, stop=True)
                    twiddle_fwd(psA, h, bt, ch4)

            def evac_k(g, ri, ps, cg=cg):
                P.copy(KH.sub((g, ri, cg), (S, g, ri, slice(cg * 512, (cg + 1) * 512))), ps[:, :], eng="act")
            step3(bt, evac_k)
        for cg in range(HCH // 4):
            bt = Bt[cg % 2]
            vi = vin[cg % 2]
            for ch4 in range(4):
                ch = cg * 4 + ch4
                if o == 0:
                    conv_row(0, ch, vi.sub(ch4, (S, ch4, S)))
                    src = vi.sub(ch4, (S, ch4, S))
                else:
                    src = z.sub(ch, (S, ch, S))
                vbt = vb[cnt["vb"] % 2]
                cnt["vb"] += 1
                P.copy(vbt[:], src, eng="act")
                for h in range(2):
                    psA = banks[h]
                    P.mm(psA[:, 0:256], vbt[:, h * 128:(h + 1) * 128], D1[:, 0, :])
                    twiddle_fwd(psA, h, bt, ch4)

            def evac_z(g, ri, ps, cg=cg):
                if ri == 1:
                    xr = banks[2 + g * 2]
                    xi = banks[2 + g * 2 + 1]
                    cs = slice(cg * 512, (cg + 1) * 512)
                    kr = KH.sub((g, 0, cg), (S, g, 0, cs))
                    ki = KH.sub((g, 1, cg), (S, g, 1, cs))
                    P.tt(pa[:], xr[:, :], kr, ALU.mult)
                    P.tt(pb[:], xi[:, :], ki, ALU.mult)
                    P.tt(Zt.sub((g, 0), (S, g, 0, S)), pa[:], pb[:], ALU.subtract, eng="pool")
                    P.tt(pa[:], xr[:, :], ki, ALU.mult)
                    P.tt(pb[:], xi[:, :], kr, ALU.mult)
                    P.tt(Zt.sub((g, 1), (S, g, 1, S)), pa[:], pb[:], ALU.add, eng="pool")
            step3(bt, evac_z)
            for ch4 in range(4):
                ch = cg * 4 + ch4
                cs = slice(ch4 * 128, (ch4 + 1) * 128)
                psC = banks[6]
                ti = 0
                for g in range(2):
                    for ri in range(2):
                        P.mm(psC[:, :], Zt.sub((g, ri), (S, g, ri, cs)), EE[:, g, ri, :], start=(ti == 0), stop=(ti == 3))
                        ti += 1
                dt_ = Dt[(ch4 // 2) % 2]
                c2 = ch4 % 2
                P.tt(t12[:], psC[:, :], TW2[:, 0, :], ALU.mult)
                P.tt(t34[:], psC[:, :], TW2[:, 1, :], ALU.mult)
                P.tt(dt_.sub((0, c2), (S, 0, slice(c2 * 256, (c2 + 1) * 256))), t12[:, 0:256], t34[:, 256:512], ALU.subtract)
                P.tt(dt_.sub((1, c2), (S, 1, slice(c2 * 256, (c2 + 1) * 256))), t34[:, 0:256], t12[:, 256:512], ALU.add,
                     eng="pool")
                if c2 == 1:
                    psY = banks[7]
                    da = _AllView(dt_)
                    P.mm(psY[0:64, :], F1[:, 0, :], da[:, 0, :], start=True, stop=False)
                    P.mm(psY[0:64, :], F1[:, 1, :], da[:, 1, :], start=False, stop=True)
                    for cc2 in range(2):
                        chx = ch - 1 + cc2
                        c4x = ch4 - 1 + cc2
                        gt = gate[cnt["g"] % 2]
                        tqq = tq[cnt["g"] % 2]
                        cnt["g"] += 1
                        conv_row(1 + o, chx, gt[:])
                        vsrc = vi.sub(c4x, (S, c4x, S)) if o == 0 else z.sub(chx, (S, chx, S))
                        so = o * HCH + chx
                        P.stt(tqq[:], vsrc, sk[:, so:so + 1], psY[0:64, cc2 * 256:(cc2 + 1) * 256], ALU.mult, ALU.add)
                        if o == 0:
                            P.tt(z.sub(chx, (S, chx, S)), tqq[:], gt[:], ALU.mult, eng="pool")
                        else:
                            oo = ot[cnt["o"] % 2]
                            cnt["o"] += 1
                            P.tt(oo[:], tqq[:], gt[:], ALU.mult, eng="pool")
                            P.dma(out[chx].rearrange("(a b) -> a b", b=256), oo[:], q="sp")
    if with_ctx:
        ftc = P.sb([33, 256], F32)
        wc_ = P.sb([HCH, 256], F32)
        cwc_t = P.sb([HCH, 12], F32)
        skc_t = P.sb([HCH, 2], F32)
        hfb = P.sb([HCH, 4, 256], F32)
        rc = P.sb([HCH, 3, 258], F32)
        class _Sl:
            def __init__(self, b):
                self.b = b
                self.t = b.t[0:HCH, 0:256]
                self.trk = b.trk

            def __getitem__(self, idx):
                return V(self.t[idx], [self.b.trk])
        u3 = [_Sl(m2s[0]), _Sl(m2s[1]), _Sl(m2s[2])]
        accs = [_Sl(aas[0]), _Sl(aas[1]), _Sl(aas[2]), _Sl(m1s[0])]
        zc = _Sl(m1s[1])
        Gc = P.sb([64, 256], BF16)
        P.dma(ftc[:], featsC)
        P.dma(wc_[:], winC)
        P.dma(cwc_t[:], cwc)
        P.dma(skc_t[:], skc)
        P.dma(rc[:], rawc.rearrange("w c n -> c w n"))
        mlp_s1(0, ftc[:], 256, banks[0])
        mlp_s2(0, 256, banks[1])
        mlp_s3(0, 256, Gc)
        for od in range(4):
            P.mm(banks[2][0:HCH, 0:256], W3[:, od * HCH:(od + 1) * HCH], Gc[:])
            P.tt(hfb.sub(od, (S, od, S)), banks[2][0:HCH, 0:256], wc_[:], ALU.mult)
        for w in range(3):
            dst = u3[w][:, :]
            P.ts(dst, rc[:, w, 1:257], cwc_t[:, w * 4 + 1:w * 4 + 2], cwc_t[:, w * 4 + 3:w * 4 + 4], ALU.mult, ALU.add)
            P.stt(dst, rc[:, w, 0:256], cwc_t[:, w * 4:w * 4 + 1], dst, ALU.mult, ALU.add)
            P.stt(dst, rc[:, w, 2:258], cwc_t[:, w * 4 + 2:w * 4 + 3], dst, ALU.mult, ALU.add)
        for o in range(2):
            vv_ = u3[0] if o == 0 else zc
            P.ts(accs[0][:, :], vv_[:, :], skc_t[:, o:o + 1], None, ALU.mult)
            for a_ in accs[1:]:
                P.memset(a_[:, :], 0.0)
            ai = 0
            hf_ = hfb.sub(o * 2, (S, o * 2, S))
            hb_ = hfb.sub(o * 2 + 1, (S, o * 2 + 1, S))
            for dd in range(256):
                a_ = accs[ai % 4]
                ai += 1
                P.stt(a_[:, dd:256], vv_[:, 0:256 - dd], hf_[:, dd:dd + 1], a_[:, dd:256], ALU.mult, ALU.add)
                if dd >= 1:
                    a_ = accs[ai % 4]
                    ai += 1
                    P.stt(a_[:, 0:256 - dd], vv_[:, dd:256], hb_[:, dd:dd + 1], a_[:, 0:256 - dd], ALU.mult, ALU.add)
            P.tt(accs[0][:, :], accs[0][:, :], accs[1][:, :], ALU.add)
            P.tt(accs[2][:, :], accs[2][:, :], accs[3][:, :], ALU.add, eng="pool")
            P.tt(accs[0][:, :], accs[0][:, :], accs[2][:, :], ALU.add)
            P.tt(zc[:, :], accs[0][:, :], u3[1 + o][:, :], ALU.mult)
        P.dma(outc, zc[:, :])
    P.finish()
    return nc


def run_hyena(proj, layer, inp, with_ctx):
    cs = hyena_consts()
    featsP, win = hyena_tables()
    cwf = inp["hy_conv_w"][layer]
    cbf = inp["hy_conv_b"][layer]
    w3 = inp["hy_ffn_w3"][layer].reshape(64, 2, 2, 256)
    fb = np.stack([inp["hy_freq"][layer], inp["hy_ffn_b1"][layer], inp["hy_ffn_b2"][layer]], axis=1).astype(np.float32)
    z1 = np.zeros((1,), np.float32)
    if with_ctx:
        featsC = np.ascontiguousarray(_hy_feats(LC, np.arange(LC)).T)
        tlc = np.linspace(0.0, 1.0, LC, dtype=np.float32)
        winC_all = np.exp(-tlc[None, :] * _hy_deltas()[:, None]).astype(np.float32)
    maps = []
    for c in range(NCORE):
        chs = np.arange(HCH * c, HCH * (c + 1))
        rows = np.stack([proj[w * 256 + chs] for w in range(3)])
        lat = rows[:, :, LC:]
        pad = np.concatenate([np.zeros((3, HCH, 1), np.float32), lat, np.zeros((3, HCH, 257), np.float32)], axis=2)
        idx = (np.arange(64) * 256)[:, None] + np.arange(258)[None, :]
        raw = np.ascontiguousarray(pad[:, :, idx])
        cw4 = np.stack([np.stack([cwf[0, w * 256 + chs], cwf[1, w * 256 + chs], cwf[2, w * 256 + chs],
                                  cbf[w * 256 + chs]], axis=1) for w in range(3)])
        m = {"raw": raw,
             "cwl": np.ascontiguousarray(np.broadcast_to(cw4.reshape(1, -1), (64, 3 * HCH * 4))).astype(np.float32),
             "skp": np.ascontiguousarray(np.broadcast_to(inp["hy_bias"][layer][:, chs].reshape(1, -1), (64, 2 * HCH))).astype(np.float32),
             "w1": inp["hy_ffn_w1"][layer], "w2": inp["hy_ffn_w2"][layer],
             "w3s": np.ascontiguousarray(w3[:, :, :, chs].reshape(64, 4 * HCH)), "fb": fb,
             "featsP": featsP, "win": np.ascontiguousarray(win[:, :, :, chs]),
             "d1": cs["d1"], "tw": cs["tw"], "c3": cs["c3"], "e": cs["e"], "tw2": cs["tw2"], "f1": cs["f1"]}
        if with_ctx:
            cr = rows[:, :, :LC]
            m["rawc"] = np.ascontiguousarray(np.concatenate([np.zeros((3, HCH, 1), np.float32), cr,
                                                             np.zeros((3, HCH, 1), np.float32)], axis=2))
            m["cwc"] = np.ascontiguousarray(cw4.transpose(1, 0, 2).reshape(HCH, 12).astype(np.float32))
            m["skc"] = np.ascontiguousarray(inp["hy_bias"][layer][:, chs].T.astype(np.float32))
            m["featsC"] = featsC
            m["winC"] = np.ascontiguousarray(winC_all[chs])
        maps.append(m)
    key = "hyena_ctx" if with_ctx else "hyena"
    res = _run(key, lambda: build_hyena(with_ctx), maps)
    y = np.zeros((256, TT), np.float32)
    for c in range(NCORE):
        y[HCH * c:HCH * (c + 1), LC:] = res[c]["out"]
        if with_ctx:
            y[HCH * c:HCH * (c + 1), :LC] = res[c]["outc"]
    return y


def kernel(**inp):
    inp = {k: np.asarray(v) for k, v in inp.items()}
    mod = run_mod(inp)
    xT_lat = np.ascontiguousarray(inp["x"][0].T)
    xT_ctx = np.ascontiguousarray(inp["ctx"][0].T)
    for layer in range(2):
        proj = run_inproj(xT_ctx, xT_lat, mod, layer, inp["w_in"][layer])
        hy = run_hyena(proj, layer, inp, with_ctx=(layer == 0))
        yf, yb = run_ssd(proj, layer, inp)
        at = run_attn(proj, layer, inp)
        mix_rows = np.ascontiguousarray(np.concatenate([hy, yf, yb, proj[768:1280], at], axis=0))
        x1T = run_post1(mix_rows, xT_ctx, xT_lat, mod, layer, inp)
        x2T = run_ffn(x1T, mod, layer, inp)
        xT_ctx = np.ascontiguousarray(x2T[:, :LC])
        xT_lat = np.ascontiguousarray(x2T[:, LC:])
    return np.ascontiguousarray(xT_lat.T)[None].astype(np.float32)
```
